# Optimizing a Trainium2 kernel written in Bass

```python
import jax
import jax.numpy as jnp
from jax import lax
import numpy as np


D_MODEL = 1024
BATCH = 8
SEQ = 4096
DEPTH = 1

CHUNK = 64
N_MEM = 256
D_MIX = D_MODEL
D_RNN = D_MIX // 2
D_ATT = D_MIX - D_RNN
N_RNN_BLOCKS = 8
RNN_BLOCK = D_RNN // N_RNN_BLOCKS
CONV_WIDTH = 4
RG_LRU_C = 8.0
N_ATT_HEADS = 8
ATT_HEAD_DIM = D_ATT // N_ATT_HEADS
LEFT_CHUNKS = 8
BAND = (LEFT_CHUNKS + 1) * CHUNK
REL_CLIP = 128
N_XHEADS = 4
XHEAD_DIM = D_MODEL // N_XHEADS
N_GROUPS = 4
EXPERTS_PER_GROUP = 8
N_EXPERTS = N_GROUPS * EXPERTS_PER_GROUP
TOP_K = 2
D_EXPERT = D_MODEL // 2
MOE_BLOCK = 128
D_IN_PROJ = 2 * D_RNN + 3 * D_ATT
EPS = 1e-6
NEG_INF = -1e30

kernel_name = 'hymba_style_streaming_hybrid_block'


def rms_norm(x, w):
    xf = x.astype(jnp.float32)
    y = xf * lax.rsqrt(jnp.mean(xf * xf, axis=-1, keepdims=True) + EPS)
    return (y * w.astype(jnp.float32)).astype(x.dtype)


def _linear_recurrence_combine(left, right):
    a1, b1 = left
    a2, b2 = right
    return a1 * a2, a2 * b1 + b2


def recurrent_group(xr, xg, conv_w, conv_b, w_a, b_a, w_x, b_x, lam):
    b, s, _ = xr.shape
    xc = lax.conv_general_dilated(
        xr, conv_w[:, None, :].astype(xr.dtype), window_strides=(1,),
        padding=[(CONV_WIDTH - 1, 0)], dimension_numbers=('NWC', 'WIO', 'NWC'),
        feature_group_count=D_RNN) + conv_b
    xb = xc.reshape(b, s, N_RNN_BLOCKS, RNN_BLOCK)
    r = jax.nn.sigmoid((jnp.einsum('bsnc,ncd->bsnd', xb, w_a).reshape(b, s, D_RNN) + b_a).astype(jnp.float32))
    i = jax.nn.sigmoid((jnp.einsum('bsnc,ncd->bsnd', xb, w_x).reshape(b, s, D_RNN) + b_x).astype(jnp.float32))
    log_a = -RG_LRU_C * r * jax.nn.softplus(-lam.astype(jnp.float32))
    a = jnp.exp(log_a)
    u = jnp.sqrt(-jnp.expm1(2.0 * log_a)) * (i * xc.astype(jnp.float32))
    _, h = lax.associative_scan(_linear_recurrence_combine, (a, u), axis=1)
    return (jax.nn.gelu(xg.astype(jnp.float32)) * h).astype(xr.dtype)


def rel_bias_band(rel_bias):
    qi = np.arange(CHUNK)[:, None]
    kj = np.arange(BAND)[None, :]
    dist = LEFT_CHUNKS * CHUNK + qi - kj
    idx = np.clip(dist, -REL_CLIP, REL_CLIP) + REL_CLIP
    return rel_bias[:, idx]


def chunk_band_attention(q, k, v, rel_bias):
    b, s, _ = q.shape
    nc = s // CHUNK
    qc = q.reshape(b, nc, CHUNK, N_ATT_HEADS, ATT_HEAD_DIM)
    kc = k.reshape(b, nc, CHUNK, N_ATT_HEADS, ATT_HEAD_DIM)
    vc = v.reshape(b, nc, CHUNK, N_ATT_HEADS, ATT_HEAD_DIM)
    pad = ((0, 0), (LEFT_CHUNKS, 0), (0, 0), (0, 0), (0, 0))
    kp = jnp.pad(kc, pad)
    vp = jnp.pad(vc, pad)
    kb = jnp.concatenate([kp[:, j:j + nc] for j in range(LEFT_CHUNKS + 1)], axis=2)
    vb = jnp.concatenate([vp[:, j:j + nc] for j in range(LEFT_CHUNKS + 1)], axis=2)
    chunk_id = np.arange(nc)[:, None] - LEFT_CHUNKS + np.arange(LEFT_CHUNKS + 1)[None, :]
    valid = np.repeat(chunk_id >= 0, CHUNK, axis=1)
    scores = jnp.einsum('bnqhd,bnkhd->bhnqk', qc, kb, preferred_element_type=jnp.float32)
    scores = scores * (ATT_HEAD_DIM ** -0.5) + rel_bias_band(rel_bias).astype(jnp.float32)[None, :, None]
    scores = jnp.where(valid[None, None, :, None, :], scores, NEG_INF)
    probs = jax.nn.softmax(scores, axis=-1).astype(v.dtype)
    out = jnp.einsum('bhnqk,bnkhd->bnqhd', probs, vb)
    return out.reshape(b, s, D_ATT)


def memory_cross_attention(h, mem_n, wq, wk, wv, wo):
    b, s, _ = h.shape
    m = mem_n.shape[1]
    q = (h @ wq).reshape(b, s, N_XHEADS, XHEAD_DIM)
    k = (mem_n @ wk).reshape(b, m, N_XHEADS, XHEAD_DIM)
    v = (mem_n @ wv).reshape(b, m, N_XHEADS, XHEAD_DIM)
    scores = jnp.einsum('bshd,bmhd->bhsm', q, k, preferred_element_type=jnp.float32) * (XHEAD_DIM ** -0.5)
    probs = jax.nn.softmax(scores, axis=-1).astype(v.dtype)
    out = jnp.einsum('bhsm,bmhd->bshd', probs, v).reshape(b, s, D_MODEL)
    return out @ wo


def routed_experts(ht, expert_idx, combine_w, w_gate, w_up, w_down):
    t, d = ht.shape
    n_slots = t * TOP_K
    flat_e = expert_idx.reshape(-1)
    flat_tok = jnp.repeat(jnp.arange(t, dtype=jnp.int32), TOP_K)
    order = jnp.argsort(flat_e)
    e_sorted = flat_e[order]
    tok_sorted = flat_tok[order]
    w_sorted = combine_w.reshape(-1)[order]
    counts = jnp.bincount(flat_e, length=N_EXPERTS)
    padded = (counts + MOE_BLOCK - 1) // MOE_BLOCK * MOE_BLOCK
    pad_end = jnp.cumsum(padded)
    pad_start = pad_end - padded
    start = jnp.cumsum(counts) - counts
    dest = pad_start[e_sorted] + (jnp.arange(n_slots) - start[e_sorted])
    n_blocks = (n_slots + MOE_BLOCK - 1) // MOE_BLOCK + N_EXPERTS
    n_rows = n_blocks * MOE_BLOCK
    src_tok = jnp.full((n_rows,), t, dtype=jnp.int32).at[dest].set(tok_sorted)
    x_pad = jnp.concatenate([ht, jnp.zeros((1, d), ht.dtype)], axis=0)
    xs = x_pad[src_tok].reshape(n_blocks, MOE_BLOCK, d)
    block_expert = jnp.minimum(
        jnp.searchsorted(pad_end, jnp.arange(n_blocks) * MOE_BLOCK, side='right'), N_EXPERTS - 1)

    def expert_block(args):
        xb, e = args
        g = xb @ w_gate[e]
        u = xb @ w_up[e]
        return (jax.nn.silu(g) * u) @ w_down[e]

    ys = lax.map(expert_block, (xs, block_expert)).reshape(n_rows, d)
    contrib = (ys[dest].astype(jnp.float32) * w_sorted[:, None]).astype(ht.dtype)
    return jnp.zeros((t, d), ht.dtype).at[tok_sorted].add(contrib)


def hierarchical_moe(h, w_group, b_group, w_router, b_router, w_gate, w_up, w_down):
    b, s, d = h.shape
    t = b * s
    ht = h.reshape(t, d)
    g_logits = (ht @ w_group).astype(jnp.float32) + b_group.astype(jnp.float32)
    g_probs = jax.nn.softmax(g_logits, axis=-1)
    g_p, g_idx = lax.top_k(g_probs, 1)
    e_all = jnp.einsum('td,gde->tge', ht, w_router).astype(jnp.float32) + b_router.astype(jnp.float32)
    sel = jnp.broadcast_to(g_idx[:, :, None], (t, 1, EXPERTS_PER_GROUP))
    e_logits = jnp.take_along_axis(e_all, sel, axis=1)[:, 0]
    e_probs = jax.nn.softmax(e_logits, axis=-1)
    e_p, e_idx = lax.top_k(e_probs, TOP_K)
    e_p = e_p / jnp.sum(e_p, axis=-1, keepdims=True)
    combine_w = g_p * e_p
    expert_idx = g_idx * EXPERTS_PER_GROUP + e_idx
    y = routed_experts(ht, expert_idx, combine_w, w_gate, w_up, w_down)
    return y.reshape(b, s, d)


def setup_inputs(seed: int = 0) -> dict:
    key = jax.random.key(seed)
    ks = jax.random.split(key, 32)
    f32 = jnp.float32

    def nrm(k, shape, scale):
        return jax.random.normal(k, shape, f32) * scale

    def gain(k, shape):
        return 1.0 + 0.02 * jax.random.normal(k, shape, f32)

    a0 = jax.random.uniform(ks[10], (DEPTH, D_RNN), f32, 0.9, 0.999)
    lam = jnp.log(a0) - jnp.log1p(-a0)
    return {
        'x': nrm(ks[0], (BATCH, SEQ, D_MODEL), 1.0),
        'mem': nrm(ks[1], (BATCH, N_MEM, D_MODEL), 1.0),
        'ln1_w': gain(ks[2], (DEPTH, D_MODEL)),
        'w_in': nrm(ks[3], (DEPTH, D_MODEL, D_IN_PROJ), D_MODEL ** -0.5),
        'conv_w': nrm(ks[4], (DEPTH, CONV_WIDTH, D_RNN), CONV_WIDTH ** -0.5),
        'conv_b': nrm(ks[5], (DEPTH, D_RNN), 0.02),
        'rnn_wa': nrm(ks[6], (DEPTH, N_RNN_BLOCKS, RNN_BLOCK, RNN_BLOCK), RNN_BLOCK ** -0.5),
        'rnn_ba': nrm(ks[7], (DEPTH, D_RNN), 0.02),
        'rnn_wx': nrm(ks[8], (DEPTH, N_RNN_BLOCKS, RNN_BLOCK, RNN_BLOCK), RNN_BLOCK ** -0.5),
        'rnn_bx': nrm(ks[9], (DEPTH, D_RNN), 0.02),
        'rnn_lambda': lam,
        'rel_bias': nrm(ks[11], (DEPTH, N_ATT_HEADS, 2 * REL_CLIP + 1), 0.2),
        'gn_rnn_w': gain(ks[12], (DEPTH, D_RNN)),
        'gn_att_w': gain(ks[13], (DEPTH, D_ATT)),
        'w_out': nrm(ks[14], (DEPTH, D_MIX, D_MODEL), D_MIX ** -0.5),
        'ln2_w': gain(ks[15], (DEPTH, D_MODEL)),
        'mem_norm_w': gain(ks[16], (D_MODEL,)),
        'xq_w': nrm(ks[17], (DEPTH, D_MODEL, D_MODEL), D_MODEL ** -0.5),
        'xk_w': nrm(ks[18], (DEPTH, D_MODEL, D_MODEL), D_MODEL ** -0.5),
        'xv_w': nrm(ks[19], (DEPTH, D_MODEL, D_MODEL), D_MODEL ** -0.5),
        'xo_w': nrm(ks[20], (DEPTH, D_MODEL, D_MODEL), D_MODEL ** -0.5),
        'ln3_w': gain(ks[21], (DEPTH, D_MODEL)),
        'router_group_w': nrm(ks[22], (DEPTH, D_MODEL, N_GROUPS), D_MODEL ** -0.5),
        'router_group_b': nrm(ks[23], (DEPTH, N_GROUPS), 0.01),
        'router_expert_w': nrm(ks[24], (DEPTH, N_GROUPS, D_MODEL, EXPERTS_PER_GROUP), D_MODEL ** -0.5),
        'router_expert_b': nrm(ks[25], (DEPTH, N_GROUPS, EXPERTS_PER_GROUP), 0.01),
        'expert_gate_w': nrm(ks[26], (DEPTH, N_EXPERTS, D_MODEL, D_EXPERT), D_MODEL ** -0.5),
        'expert_up_w': nrm(ks[27], (DEPTH, N_EXPERTS, D_MODEL, D_EXPERT), D_MODEL ** -0.5),
        'expert_down_w': nrm(ks[28], (DEPTH, N_EXPERTS, D_EXPERT, D_MODEL), D_EXPERT ** -0.5),
        'final_norm_w': gain(ks[29], (D_MODEL,)),
    }


def reference(x, mem, ln1_w, w_in, conv_w, conv_b, rnn_wa, rnn_ba, rnn_wx, rnn_bx, rnn_lambda,
              rel_bias, gn_rnn_w, gn_att_w, w_out, ln2_w, mem_norm_w, xq_w, xk_w, xv_w, xo_w,
              ln3_w, router_group_w, router_group_b, router_expert_w, router_expert_b,
              expert_gate_w, expert_up_w, expert_down_w, final_norm_w):
    mem_n = rms_norm(mem, mem_norm_w)
    for l in range(DEPTH):
        h = rms_norm(x, ln1_w[l])
        proj = h @ w_in[l]
        xr = proj[..., :D_RNN]
        xg = proj[..., D_RNN:2 * D_RNN]
        q = proj[..., 2 * D_RNN:2 * D_RNN + D_ATT]
        k = proj[..., 2 * D_RNN + D_ATT:2 * D_RNN + 2 * D_ATT]
        v = proj[..., 2 * D_RNN + 2 * D_ATT:]
        y_rnn = recurrent_group(xr, xg, conv_w[l], conv_b[l], rnn_wa[l], rnn_ba[l],
                                rnn_wx[l], rnn_bx[l], rnn_lambda[l])
        y_att = chunk_band_attention(q, k, v, rel_bias[l])
        mixed = jnp.concatenate([rms_norm(y_rnn, gn_rnn_w[l]), rms_norm(y_att, gn_att_w[l])], axis=-1)
        x = x + mixed @ w_out[l]
        h = rms_norm(x, ln2_w[l])
        x = x + memory_cross_attention(h, mem_n, xq_w[l], xk_w[l], xv_w[l], xo_w[l])
        h = rms_norm(x, ln3_w[l])
        x = x + hierarchical_moe(h, router_group_w[l], router_group_b[l], router_expert_w[l],
                                 router_expert_b[l], expert_gate_w[l], expert_up_w[l], expert_down_w[l])
    return rms_norm(x, final_norm_w)
```

```python
import numpy as np
from contextlib import ExitStack

import concourse.bass as bass
import concourse.mybir as mybir
from concourse.bass_utils import run_bass_kernel_spmd

F32 = mybir.dt.float32
BF16 = mybir.dt.bfloat16
I32 = mybir.dt.int32
AF = mybir.ActivationFunctionType
ALU = mybir.AluOpType
AX = mybir.AxisListType

S = 4096
D = 1024
NT = 32
GT = 512
NG = 8
TPG = 4
NE = 32
CAP = 512
NSLOT = NE * CAP
NROWS = NSLOT + 1024
TRASH = NSLOT
EPS = 1e-6
NV = 64
GELU_K = 0.7978845608028654


class Res:
    __slots__ = ("w", "r")

    def __init__(self):
        self.w = {}
        self.r = {}


class DSem:
    def __init__(self, sem):
        self.sem = sem
        self.cnt = 0


class TK:
    def __init__(self, nc, stack):
        self.nc = nc
        self.stack = stack
        self.eng = {"pe": nc.tensor, "dve": nc.vector, "act": nc.scalar, "pool": nc.gpsimd, "sp": nc.sync}
        self.esem = {}
        self.ecnt = {}
        self.seen = {}
        for k in self.eng:
            self.esem[k] = stack.enter_context(nc.semaphore("es_" + k))
            self.ecnt[k] = 0
            self.seen[k] = {}
        self.dsems = []
        self.dsd = {}

    def dsem(self, name):
        if name not in self.dsd:
            d = DSem(self.stack.enter_context(self.nc.semaphore("m_" + name)))
            self.dsems.append(d)
            self.dsd[name] = d
        return self.dsd[name]

    @staticmethod
    def _merge(d, tokd):
        for k, (s, v) in tokd.items():
            if v is None or k not in d or d[k][1] < v:
                d[k] = (s, v)

    @staticmethod
    def _flat(rs):
        out = []
        for r in rs:
            if isinstance(r, (list, tuple)):
                out.extend(TK._flat(r))
            else:
                out.append(r)
        return out

    def _deps(self, reads, writes):
        d = {}
        for r in self._flat(reads):
            self._merge(d, r.w)
        for w in self._flat(writes):
            self._merge(d, w.w)
            self._merge(d, w.r)
        return d

    def _wait(self, e, deps):
        E = self.eng[e]
        seen = self.seen[e]
        for k, (s, v) in deps.items():
            if e == "pe" and k == "pe":
                continue
            if v is None:
                sem, val = s.sem, s.cnt
            else:
                sem, val = s, v
            if seen.get(k, 0) < val:
                E.wait_ge(sem, val)
                seen[k] = val

    @staticmethod
    def _update(reads, writes, key, tok):
        for r in TK._flat(reads):
            r.r[key] = tok
        for w in TK._flat(writes):
            w.w = {key: tok}
            w.r = {}

    def op(self, e, fn, r=(), w=()):
        self._wait(e, self._deps(r, w))
        ins = fn(self.eng[e])
        self.ecnt[e] += 1
        ins.then_inc(self.esem[e], 1)
        self._update(r, w, e, (self.esem[e], self.ecnt[e]))

    def ops(self, e, fns, r=(), w=()):
        self._wait(e, self._deps(r, w))
        ins = None
        for fn in fns:
            ins = fn(self.eng[e])
        self.ecnt[e] += 1
        ins.then_inc(self.esem[e], 1)
        self._update(r, w, e, (self.esem[e], self.ecnt[e]))

    def dma(self, q, name, fn, r=(), w=()):
        ds = self.dsem(name)
        self._wait(q, self._deps(r, w))
        ins = fn(self.eng[q])
        ds.cnt += 16
        ins.then_inc(ds.sem, 16)
        self._update(r, w, "d_" + name, (ds, None))

    def barrier(self):
        d = {}
        for k in self.eng:
            if self.ecnt[k] > 0:
                d[k] = (self.esem[k], self.ecnt[k])
        for name, ds in self.dsd.items():
            if ds.cnt > 0:
                d["d_" + name] = (ds.sem, ds.cnt)
        for e in self.eng:
            E = self.eng[e]
            seen = self.seen[e]
            for k, (s, v) in d.items():
                if k == e:
                    continue
                if seen.get(k, 0) < v:
                    E.wait_ge(s, v)
                    seen[k] = v


def build_nc():
    nc = bass.Bass("TRN2", target_bir_lowering=False)

    def din(name, shape, dt=F32):
        return nc.dram_tensor(name, shape, dt, kind="ExternalInput").ap()

    x_d = din("x", [S, D])
    mem_d = din("mem", [256, D])
    vecs_d = din("vecs", [128, NV])
    bm_d = din("bm", [128, 8, 640])
    bd_d = din("bd", [128, 2, 4, 128])
    wr_d = din("wr", [D, 36])
    rb_d = din("rb", [36])
    w_in_d = din("w_in", [D, 2560])
    w_out_d = din("w_out", [D, D])
    wq_d = din("wq", [D, D])
    wk_d = din("wk", [D, D])
    wv_d = din("wv", [D, D])
    wo_d = din("wo", [D, D])
    memw_d = din("memw", [D])
    fin_d = din("fin", [D])
    eg_d = din("eg", [NE, D, 512])
    eu_d = din("eu", [NE, D, 512])
    ed_d = din("ed", [NE, 512, D])
    out_d = nc.dram_tensor("out", [S, D], F32, kind="ExternalOutput").ap()
    x2s_d = nc.dram_tensor("x2s", [S, D], F32, kind="Internal").ap()
    xs_d = nc.dram_tensor("xs", [NROWS, D], BF16, kind="Internal").ap()
    ys_d = nc.dram_tensor("ys", [NSLOT + 1, D], F32, kind="Internal").ap()

    with ExitStack() as st:
        tk = TK(nc, st)

        def sb(name, shape, dt, stack=st):
            return stack.enter_context(nc.sbuf_tensor("s_" + name, shape, dt))

        def psum(name, shape, dt):
            return st.enter_context(nc.psum_tensor(name, shape, dt))

        def V(fn, r=(), w=()):
            tk.op("dve", fn, r, w)

        def A(fn, r=(), w=()):
            tk.op("act", fn, r, w)

        def GP(fn, r=(), w=()):
            tk.op("pool", fn, r, w)

        def PE(fns, r=(), w=()):
            tk.ops("pe", fns, r, w)

        pall = psum("pall", [128, 8 * 512], F32)
        pA = pall[:, 0:1024]
        pB = pall[:, 1024:2048]
        pC = pall[:, 2048:3072]
        pD = pall[:, 3072:3584]
        pE = pall[:, 3584:4096]
        r_pE = Res()
        r_pA = [Res(), Res()]
        r_pB = [Res(), Res()]
        r_pC = [Res(), Res()]
        r_pD = Res()
        SX = [pA[:, 0:640], pB[:, 0:640]]
        r_SX = [r_pA, r_pB]

        identf = sb("identf", [128, 128], F32)
        ident = sb("ident", [128, 128], BF16)
        ones_bf = sb("ones_bf", [128, 128], BF16)
        trif = sb("trif", [128, 128], F32)
        tri_bf = sb("tri_bf", [128, 128], BF16)
        vecs = sb("vecs", [128, NV], F32)
        hb = sb("hb", [128, 8], F32)
        hc = sb("hc", [128, 8], F32)
        spt = sb("spt", [128, 4], F32)
        rb_bc = sb("rb_bc", [128, 36], F32)
        dest_all = sb("dest_all", [128, 2 * NT], I32)
        cw_all = sb("cw_all", [128, 2 * NT], F32)
        cnt = sb("cnt", [128, NE], F32)
        eoff_i = sb("eoff_i", [128, NE], I32)
        eoff = sb("eoff", [128, NE], F32)
        neg05 = sb("neg05", [128, 8], F32)
        qtr = sb("qtr", [128, 1], F32)
        hstate = sb("hstate", [128, 4], F32)
        halo = sb("halo", [128, 4, 3], F32)
        zt = sb("zt", [128, 8], F32)
        r_ident = Res(); r_identf = Res(); r_ones = Res(); r_trif = Res(); r_tri = Res()
        r_vecs = Res(); r_hb = Res(); r_hc = Res(); r_spt = Res()
        r_rb = Res()
        r_dest = [Res() for _ in range(NT)]
        r_cw = [Res() for _ in range(NT)]
        r_cnt = Res(); r_eoffi = Res(); r_eoff = Res(); r_neg = Res()
        r_hstate = [Res() for _ in range(4)]
        r_halo = [Res() for _ in range(4)]
        r_zrow = Res()
        r_x2s = [Res() for _ in range(NT)]
        r_xs = Res()
        r_ys = Res()

        ln1w = vecs[:, 0:8]
        ln2w = vecs[:, 8:16]
        gnrw = vecs[:, 16:20]
        gnaw = vecs[:, 20:24]
        convb = vecs[:, 24:28]
        lam = vecs[:, 36:40]
        ln3w = vecs[:, 56:64]

        tk.dma("sp", "vecs", lambda e: e.dma_start(out=vecs[:], in_=vecs_d), w=[r_vecs])
        tk.dma("sp", "rb", lambda e: e.dma_start(out=rb_bc[:], in_=rb_d.partition_broadcast(128)), w=[r_rb])
        GP(lambda e: e.memset(identf[:], 0.0), w=[r_identf])
        GP(lambda e: e.affine_select(out=identf[:], in_=identf[:], pattern=[[-1, 128]], compare_op=ALU.not_equal,
                                     fill=1.0, base=0, channel_multiplier=1), r=[r_identf], w=[r_identf])
        V(lambda e: e.tensor_copy(out=ident[:], in_=identf[:]), r=[r_identf], w=[r_ident])
        GP(lambda e: e.memset(trif[:], 1.0), w=[r_trif])
        GP(lambda e: e.affine_select(out=trif[:], in_=trif[:], pattern=[[1, 128]], compare_op=ALU.is_gt,
                                     fill=0.0, base=0, channel_multiplier=-1), r=[r_trif], w=[r_trif])
        V(lambda e: e.tensor_copy(out=tri_bf[:], in_=trif[:]), r=[r_trif], w=[r_tri])
        V(lambda e: e.memset(ones_bf[:], 1.0), w=[r_ones])
        GP(lambda e: e.iota(eoff_i[:], pattern=[[CAP, NE]], base=0, channel_multiplier=0), w=[r_eoffi])
        V(lambda e: e.tensor_copy(out=eoff[:], in_=eoff_i[:]), r=[r_eoffi], w=[r_eoff])
        V(lambda e: e.memset(neg05[:], -0.5), w=[r_neg])
        V(lambda e: e.memset(qtr[:], 0.25), w=[r_neg])
        V(lambda e: e.memset(cnt[:], 0.0), w=[r_cnt])
        V(lambda e: e.memset(hstate[:], 0.0), w=r_hstate)
        V(lambda e: e.memset(halo[:], 0.0), w=r_halo)
        V(lambda e: e.memset(zt[:], 0.0), w=[r_zrow])
        tk.dma("sp", "zt", lambda e: e.dma_start(out=ys_d[TRASH].rearrange("(p f) -> p f", p=128), in_=zt[:]), r=[r_zrow], w=[Res()])
        V(lambda e: e.tensor_scalar(out=hb[:], in0=vecs[:, 28:36], scalar1=0.5, scalar2=None, op0=ALU.mult), r=[r_vecs], w=[r_hb])
        A(lambda e: e.activation(out=spt[:], in_=lam, func=AF.Exp, scale=-1.0), r=[r_vecs], w=[r_spt])
        A(lambda e: e.activation(out=spt[:], in_=spt[:], func=AF.Ln, bias=1.0), r=[r_spt], w=[r_spt])
        V(lambda e: e.tensor_scalar(out=hc[:, 0:4], in0=spt[:], scalar1=-4.0, scalar2=None, op0=ALU.mult), r=[r_spt], w=[r_hc])
        V(lambda e: e.tensor_scalar(out=hc[:, 4:8], in0=spt[:], scalar1=-8.0, scalar2=None, op0=ALU.mult), r=[r_spt], w=[r_hc])

        def rstd_op(src_ap, n, dim, tmp_ap, out_ap, r_src, r_tmp, r_out):
            V(lambda e: e.tensor_scalar(out=tmp_ap, in0=src_ap, scalar1=1.0 / dim, scalar2=EPS, op0=ALU.mult, op1=ALU.add),
              r=r_src, w=[r_tmp])
            GP(lambda e: e.tensor_tensor(out=out_ap, in0=tmp_ap, in1=neg05[:, 0:n], op=ALU.pow), r=[r_tmp, r_neg], w=[r_out])

        with ExitStack() as sa:
            w_in = sb("w_in", [128, 8, 2560], BF16, sa)
            w_out = sb("w_out", [128, 8, D], BF16, sa)
            wq = sb("wq", [128, 8, D], BF16, sa)
            wo = sb("wo", [128, 8, D], BF16, sa)
            bd = sb("bd", [128, 2, 4, 128], BF16, sa)
            wr = sb("wr", [128, 8, 36], BF16, sa)
            bm = sb("bm", [128, 8, 640], BF16, sa)
            kTm = sb("kTm", [128, 8, 256], BF16, sa)
            vm = sb("vm", [128, 2, D], BF16, sa)
            kring = sb("kring", [128, 4, 1024], BF16, sa)
            vring = sb("vring", [128, 8, 8, 65], BF16, sa)
            r_w_in = [Res(), Res()]; r_w_out = Res(); r_wq = Res(); r_wo = Res(); r_bd = Res(); r_wr = Res(); r_bm = Res()
            r_kTm = Res(); r_vm = Res()
            r_kring = [[Res() for _ in range(4)] for _ in range(2)]
            r_vring = [Res() for _ in range(8)]

            def wview(dram):
                return dram.rearrange("(k p) n -> p k n", p=128)

            def mm_transpose(dst, src_fn, nblk, r, w):
                PE([(lambda e, b=b: e.matmul(dst[:, b * 128:(b + 1) * 128], lhsT=src_fn(b), rhs=ident[:], start=True, stop=True))
                    for b in range(nblk)], r=list(r) + [r_ident], w=w)


            zt2 = sb("zt2", [128, D], BF16, sa); r_zt2 = Res(); r_xsz = Res()
            V(lambda e: e.memset(zt2[:], 0.0), w=[r_zt2])

            tk.dma("pool", "w_in0", lambda e: e.dma_start(out=w_in[:, :, 0:1280], in_=wview(w_in_d)[:, :, 0:1280]), w=[r_w_in[0]])
            tk.dma("pool", "w_in1", lambda e: e.dma_start(out=w_in[:, :, 1280:2560], in_=wview(w_in_d)[:, :, 1280:2560]), w=[r_w_in[1]])

            with ExitStack() as s0:
                wk = sb("wk", [128, 8, D], BF16, s0)
                wv = sb("wv", [128, 8, D], BF16, s0)
                memt = sb("memt", [128, 2, D], F32, s0)
                memn = sb("memn", [128, 2, D], BF16, s0)
                memT = sb("memT", [128, 8, 256], BF16, s0)
                memw_bc = sb("memw_bc", [128, D], F32, s0)
                junk0 = sb("junk0", [128, D], F32, s0)
                mss = sb("mss", [128, 2], F32, s0)
                mtmp = sb("mtmp", [128, 2], F32, s0)
                mrs = sb("mrs", [128, 2], F32, s0)
                r_wk = Res(); r_wv = Res(); r_memt = Res(); r_memn = Res(); r_memT = Res(); r_memw = Res()
                r_junk0 = Res(); r_mss = Res(); r_mtmp = Res(); r_mrs = Res()
                tk.dma("sp", "memt", lambda e: e.dma_start(out=memt[:], in_=mem_d.rearrange("(t p) d -> p t d", p=128)), w=[r_memt])
                tk.dma("sp", "memw", lambda e: e.dma_start(out=memw_bc[:], in_=memw_d.partition_broadcast(128)), w=[r_memw])
                tk.dma("pool", "wk", lambda e: e.dma_start(out=wk[:], in_=wview(wk_d)), w=[r_wk])
                tk.dma("pool", "wv", lambda e: e.dma_start(out=wv[:], in_=wview(wv_d)), w=[r_wv])
                for t in range(2):
                    A(lambda e: e.activation(out=junk0[:], in_=memt[:, t, :], func=AF.Square, accum_out=mss[:, t:t + 1]),
                      r=[r_memt], w=[r_junk0, r_mss])
                rstd_op(mss[:], 2, D, mtmp[:], mrs[:], [r_mss], r_mtmp, r_mrs)
                for t in range(2):
                    V(lambda e: e.scalar_tensor_tensor(out=memn[:, t, :], in0=memt[:, t, :], scalar=mrs[:, t:t + 1], in1=memw_bc[:],
                                                       op0=ALU.mult, op1=ALU.mult), r=[r_memt, r_mrs, r_memw], w=[r_memn])
                    mm_transpose(pC, lambda b: memn[:, t, b * 128:(b + 1) * 128], 8, [r_memn], r_pC)
                    V(lambda e: e.tensor_copy(out=memT[:, :, t * 128:(t + 1) * 128], in_=pC.rearrange("p (k t) -> p k t", k=8)),
                      r=r_pC, w=[r_memT])
                for fo in range(8):
                    bank = fo % 2
                    PE([(lambda e, kc=kc: e.matmul(pA[:, bank * 512:bank * 512 + 256], lhsT=wk[:, kc, fo * 128:(fo + 1) * 128],
                                                   rhs=memT[:, kc, :], start=(kc == 0), stop=(kc == 7))) for kc in range(8)],
                       r=[r_wk, r_memT], w=[r_pA[bank]])
                    A(lambda e: e.activation(out=kTm[:, fo, :], in_=pA[:, bank * 512:bank * 512 + 256], func=AF.Copy),
                      r=[r_pA[bank]], w=[r_kTm])
                for mt in range(2):
                    for half in range(2):
                        PE([(lambda e, kc=kc: e.matmul(pB[:, half * 512:(half + 1) * 512], lhsT=memT[:, kc, mt * 128:(mt + 1) * 128],
                                                       rhs=wv[:, kc, half * 512:(half + 1) * 512], start=(kc == 0), stop=(kc == 7)))
                            for kc in range(8)], r=[r_wv, r_memT], w=[r_pB[half]])
                        A(lambda e: e.activation(out=vm[:, mt, half * 512:(half + 1) * 512], in_=pB[:, half * 512:(half + 1) * 512],
                                                 func=AF.Copy), r=[r_pB[half]], w=[r_vm])
                tk.barrier()

            tk.dma("pool", "w_out", lambda e: e.dma_start(out=w_out[:], in_=wview(w_out_d)), w=[r_w_out])
            tk.dma("pool", "wq", lambda e: e.dma_start(out=wq[:], in_=wview(wq_d)), w=[r_wq])
            tk.dma("pool", "wo", lambda e: e.dma_start(out=wo[:], in_=wview(wo_d)), w=[r_wo])
            tk.dma("pool", "bd", lambda e: e.dma_start(out=bd[:], in_=bd_d), w=[r_bd])
            tk.dma("pool", "wr", lambda e: e.dma_start(out=wr[:], in_=wview(wr_d)), w=[r_wr])
            tk.dma("pool", "bm", lambda e: e.dma_start(out=bm[:], in_=bm_d), w=[r_bm])
            V(lambda e: e.memset(vring[:].rearrange("p a b c -> p (a b c)"), 1.0), w=r_vring)
            xs_z = xs_d.rearrange("(p r) d -> p r d", p=128)
            for zi in range(NROWS // 128):
                tk.dma("act", "xsz", lambda e: e.dma_start(out=xs_z[:, zi, :], in_=zt2[:]), r=[r_zt2], w=[r_xsz])

            xt = [sb("xt%d" % j, [128, D], F32, sa) for j in range(TPG)]
            r_xt = [Res() for _ in range(TPG)]
            xn = [sb("xn0", [128, D], BF16, sa)] * 2
            r_xn = [Res()] * 2
            junk = xn[0]; r_junk = r_xn[0]
            hT = sb("hT", [128, 8, GT], BF16, sa)
            r_hT = [Res() for _ in range(TPG)]
            mix = sb("mix", [128, 12, GT], BF16, sa)
            r_mix = [Res() for _ in range(12)]
            qT = mix[:, 0:8, :]
            r_qT = r_mix[0:8]
            yrT = mix[:, 8:12, :]
            r_yrT = r_mix[8:12]
            yaT = sb("yaT", [128, 4, GT], BF16, sa)
            r_yaT = [Res() for _ in range(TPG)]
            qxT = mix[:, 0:8, :]
            r_qxT = r_mix[0:8]
            ssq = sb("ssq", [128, 4], F32, sa); r_ssq = Res()
            stmp = sb("stmp", [128, 8], F32, sa); r_stmp = Res()
            rstd = sb("rstd", [128, 4], F32, sa); r_rstd = Res()
            ssqr = sb("ssqr", [128, 4], F32, sa); r_ssqr = Res()
            ssqa = sb("ssqa", [128, 4], F32, sa); r_ssqa = Res()
            rstdg = sb("rstdg", [128, 8], F32, sa); r_rstdg = Res()
            xrp = sb("xrp", [128, GT + 3], F32, sa); r_xrp = Res()
            xc = sb("xc", [128, GT], F32, sa); r_xc = Res()
            xcb = sb("xcb", [128, GT], BF16, sa); r_xcb = Res()
            tr_ = sb("tr_", [128, GT], F32, sa); r_tr = Res()
            ti_ = sb("ti_", [128, GT], F32, sa); r_ti = Res()
            av = sb("av", [128, GT], F32, sa); r_av = Res()
            vv = sb("vv", [128, GT], F32, sa); r_vv = Res()
            hs = sb("hs", [128, GT], F32, sa); r_hs = Res()
            xgs = sb("xgs", [128, GT], F32, sa); r_xgs = Res()
            g1 = sb("g1", [128, GT], F32, sa); r_g1 = Res()
            g2 = sb("g2", [128, GT], F32, sa); r_g2 = Res()
            a2 = hs; r_a2 = r_hs
            gl = av; r_gl = r_av
            yraw = vv; r_yraw = r_vv
            ysq = sb("ysq", [128, GT], BF16, sa); r_ysq = Res()
            Eb = [sb("Eb%d" % k, [128, 640], BF16, sa) for k in range(2)]
            r_Eb = [Res(), Res()]
            rden8 = sb("rden8", [128, 8], F32, sa); r_rden8 = Res()
            ex = [sb("ex0", [128, 2, GT], BF16, sa)] * 2
            r_ex = [Res()] * 2
            rdn = tr_; r_rdn = r_tr
            h3 = [sb("h3_%d" % k, [128, D], BF16, sa) for k in range(2)]
            r_h3 = [Res(), Res()]
            h3T = sb("h3T", [128, 8, 128], BF16, sa); r_h3T = Res()
            yat = h3[0][:].bitcast(F32); r_yat = r_h3[0]
            yab = Eb[0][:, 0:512]; r_yab = r_Eb[0]
            rt = sb("rt", [128, 772], F32, sa)
            r_rt = Res()
            Ab4 = xcb[:, 0:4 * NE].rearrange("p (j e) -> p j e", j=4); r_Ab = r_xcb

            def rtv(a, b, **kw):
                v = rt[:, a:b]
                return v.rearrange(kw.pop("pat"), **kw) if kw else v

            lg4 = rtv(0, 144, pat="p (j c) -> p j c", j=4)
            gmax4 = rt[:, 144:148]
            gsum4 = rt[:, 148:152]
            gp4 = rt[:, 152:156]
            m1_4 = rt[:, 156:160]
            m2_4 = rt[:, 160:164]
            dd4 = rt[:, 164:168]
            edd4 = rt[:, 168:172]
            w1_4 = rt[:, 172:176]
            w2_4 = rt[:, 176:180]
            gsh4 = rtv(180, 196, pat="p (j c) -> p j c", j=4)
            gex4 = rtv(196, 212, pat="p (j c) -> p j c", j=4)
            goh4 = rtv(212, 228, pat="p (j c) -> p j c", j=4)
            el4 = rtv(228, 260, pat="p (j c) -> p j c", j=4)
            oh1_4 = rtv(260, 292, pat="p (j c) -> p j c", j=4)
            el2_4 = rtv(292, 324, pat="p (j c) -> p j c", j=4)
            oh2_4 = rtv(324, 356, pat="p (j c) -> p j c", j=4)
            pk4 = rtv(356, 364, pat="p (j k) -> p j k", j=4)
            ek4 = rtv(364, 372, pat="p (j k) -> p j k", j=4)
            ok4 = rtv(372, 380, pat="p (j k) -> p j k", j=4)
            sd4 = rtv(380, 388, pat="p (j k) -> p j k", j=4)
            prod4 = rtv(388, 516, pat="p (j g e) -> p j g e", j=4, g=4)
            A1_4 = rtv(516, 644, pat="p (j c) -> p j c", j=4)
            A2_4 = rtv(644, 772, pat="p (j c) -> p j c", j=4)
            posf4 = rtv(0, 128, pat="p (j c) -> p j c", j=4)
            prodE = rtv(388, 516, pat="p (j c) -> p j c", j=4)

            def load_x(g):
                for j in range(TPG):
                    i = g * TPG + j
                    tk.dma("sp", "x%d" % j, lambda e: e.dma_start(out=xt[j][:], in_=x_d[i * 128:(i + 1) * 128, :]), w=[r_xt[j]])

            def norm_stats(j):
                A(lambda e: e.activation(out=junk[:], in_=xt[j][:], func=AF.Square, accum_out=ssq[:, j:j + 1]),
                  r=[r_xt[j]], w=[r_junk, r_ssq])

            def norm_to_hT(j, lnw):
                k = j % 2
                A(lambda e: e.activation(out=xn[k][:], in_=xt[j][:], func=AF.Copy, scale=rstd[:, j:j + 1]),
                  r=[r_xt[j], r_rstd], w=[r_xn[k]])
                pq, r_pq = (pB, r_pB) if j % 2 == 0 else (pC, r_pC)
                mm_transpose(pq, lambda b: xn[k][:, b * 128:(b + 1) * 128], 8, [r_xn[k]], r_pq)
                V(lambda e: e.tensor_tensor(out=hT[:, :, j * 128:(j + 1) * 128], in0=pq.rearrange("p (k t) -> p k t", k=8),
                                            in1=lnw.unsqueeze(2).broadcast_to([128, 8, 128]), op=ALU.mult),
                  r=r_pq + [r_vecs], w=[r_hT[j]])

            def proj_fm(wt, r_w, col0, bank_ap, r_bank):
                PE([(lambda e, kc=kc: e.matmul(bank_ap, lhsT=wt[:, kc, col0:col0 + 128], rhs=hT[:, kc, :],
                                               start=(kc == 0), stop=(kc == 7))) for kc in range(8)],
                   r=list(r_w) + r_hT, w=[r_bank])

            def rnn_steps(g, c):
                C0 = pD
                rC0 = r_pD
                cw0 = 40 + c * 4
                M_ = []
                G_ = []
                T_ = []
                G_.append(lambda: proj_fm(w_in, r_w_in, 512 + c * 128, C0, rC0))
                G_.append(lambda: A(lambda e: e.activation(out=xgs[:], in_=C0, func=AF.Copy), r=[rC0], w=[r_xgs]))
                G_.append(lambda: A(lambda e: e.activation(out=g1[:], in_=xgs[:], func=AF.Square), r=[r_xgs], w=[r_g1]))
                G_.append(lambda: GP(lambda e: e.tensor_scalar(out=g1[:], in0=g1[:], scalar1=0.044715, scalar2=1.0, op0=ALU.mult, op1=ALU.add),
                                     r=[r_g1], w=[r_g1]))
                G_.append(lambda: GP(lambda e: e.tensor_tensor(out=g1[:], in0=xgs[:], in1=g1[:], op=ALU.mult), r=[r_xgs, r_g1], w=[r_g1]))
                G_.append(lambda: A(lambda e: e.activation(out=g2[:], in_=g1[:], func=AF.Tanh, scale=GELU_K), r=[r_g1], w=[r_g2]))
                G_.append(lambda: V(lambda e: e.scalar_tensor_tensor(out=g2[:], in0=g2[:], scalar=1.0, in1=xgs[:], op0=ALU.add, op1=ALU.mult),
                                    r=[r_g2, r_xgs], w=[r_g2]))
                M_.append(lambda: proj_fm(w_in, r_w_in, c * 128, C0, rC0))
                M_.append(lambda: A(lambda e: e.activation(out=xrp[:, 3:GT + 3], in_=C0, func=AF.Copy), r=[rC0], w=[r_xrp]))
                M_.append(lambda: V(lambda e: e.tensor_copy(out=xrp[:, 0:3], in_=halo[:, c, :]), r=[r_halo[c]], w=[r_xrp]))
                M_.append(lambda: V(lambda e: e.tensor_scalar(out=xc[:], in0=xrp[:, 3:GT + 3], scalar1=vecs[:, cw0 + 3:cw0 + 4],
                                                               scalar2=convb[:, c:c + 1], op0=ALU.mult, op1=ALU.add),
                                    r=[r_xrp, r_vecs], w=[r_xc]))
                for jj in range(3):
                    M_.append(lambda jj=jj: V(lambda e: e.scalar_tensor_tensor(out=xc[:], in0=xrp[:, jj:jj + GT],
                                                                               scalar=vecs[:, cw0 + jj:cw0 + jj + 1], in1=xc[:],
                                                                               op0=ALU.mult, op1=ALU.add),
                                              r=[r_xrp, r_vecs, r_xc], w=[r_xc]))
                M_.append(lambda: V(lambda e: e.tensor_copy(out=halo[:, c, :], in_=xrp[:, GT:GT + 3]), r=[r_xrp], w=[r_halo[c]]))
                M_.append(lambda: A(lambda e: e.activation(out=xcb[:], in_=xc[:], func=AF.Copy), r=[r_xc], w=[r_xcb]))
                M_.append(lambda: PE([lambda e: e.matmul(C0, lhsT=bd[:, 0, c, :], rhs=xcb[:], start=True, stop=True)],
                                     r=[r_bd, r_xcb], w=[rC0]))
                M_.append(lambda: A(lambda e: e.activation(out=tr_[:], in_=C0, func=AF.Tanh, scale=0.5, bias=hb[:, c:c + 1]),
                                    r=[rC0, r_hb], w=[r_tr]))
                M_.append(lambda: PE([lambda e: e.matmul(C0, lhsT=bd[:, 1, c, :], rhs=xcb[:], start=True, stop=True)],
                                     r=[r_bd, r_xcb], w=[rC0]))
                M_.append(lambda: A(lambda e: e.activation(out=ti_[:], in_=C0, func=AF.Tanh, scale=0.5, bias=hb[:, 4 + c:5 + c]),
                                    r=[rC0, r_hb], w=[r_ti]))
                M_.append(lambda: A(lambda e: e.activation(out=av[:], in_=tr_[:], func=AF.Exp, scale=hc[:, c:c + 1], bias=hc[:, c:c + 1]),
                                    r=[r_tr, r_hc], w=[r_av]))
                M_.append(lambda: A(lambda e: e.activation(out=a2[:], in_=tr_[:], func=AF.Exp, scale=hc[:, 4 + c:5 + c],
                                                           bias=hc[:, 4 + c:5 + c]), r=[r_tr, r_hc], w=[r_a2]))
                M_.append(lambda: V(lambda e: e.scalar_tensor_tensor(out=vv[:], in0=ti_[:], scalar=1.0, in1=xc[:], op0=ALU.add, op1=ALU.mult),
                                    r=[r_ti, r_xc], w=[r_vv]))
                M_.append(lambda: A(lambda e: e.activation(out=a2[:], in_=a2[:], func=AF.Sqrt, scale=-0.25, bias=qtr[:, 0:1]),
                                    r=[r_a2, r_neg], w=[r_a2]))
                M_.append(lambda: V(lambda e: e.tensor_tensor(out=vv[:], in0=vv[:], in1=a2[:], op=ALU.mult), r=[r_vv, r_a2], w=[r_vv]))
                M_.append(lambda: V(lambda e: e.tensor_tensor_scan(out=hs[:], data0=av[:], data1=vv[:], initial=hstate[:, c:c + 1],
                                                                    op0=ALU.mult, op1=ALU.add), r=[r_av, r_vv, r_hstate[c]], w=[r_hs]))
                M_.append(lambda: V(lambda e: e.tensor_copy(out=hstate[:, c:c + 1], in_=hs[:, GT - 1:GT]), r=[r_hs], w=[r_hstate[c]]))
                T_.append(lambda: V(lambda e: e.scalar_tensor_tensor(out=yraw[:], in0=g2[:], scalar=0.5, in1=hs[:], op0=ALU.mult, op1=ALU.mult),
                                    r=[r_g2, r_hs], w=[r_yraw]))
                T_.append(lambda: A(lambda e: e.activation(out=yrT[:, c, :], in_=yraw[:], func=AF.Copy, scale=gnrw[:, c:c + 1]),
                                    r=[r_yraw, r_vecs], w=[r_yrT[c]]))
                T_.append(lambda: A(lambda e: e.activation(out=ysq[:], in_=yraw[:], func=AF.Square), r=[r_yraw], w=[r_ysq]))
                T_.append(lambda: PE([(lambda e, j=j: e.matmul(C0[:, j:j + 1], lhsT=ysq[:, j * 128:(j + 1) * 128], rhs=ones_bf[:, 0:1],
                                                               start=True, stop=True)) for j in range(TPG)], r=[r_ysq, r_ones], w=[rC0]))
                if c == 0:
                    T_.append(lambda: V(lambda e: e.tensor_copy(out=ssqr[:], in_=C0[:, 0:4]), r=[rC0], w=[r_ssqr]))
                else:
                    T_.append(lambda: V(lambda e: e.tensor_tensor(out=ssqr[:], in0=C0[:, 0:4], in1=ssqr[:], op=ALU.add),
                                        r=[rC0, r_ssqr], w=[r_ssqr]))
                out_ = []
                gi = 0
                for mi, m_ in enumerate(M_):
                    out_.append(m_)
                    if mi % 3 == 1 and gi < len(G_):
                        out_.append(G_[gi]); gi += 1
                out_.extend(G_[gi:])
                out_.extend(T_)
                return out_

            def att_tile(g, j, steps):
                i = g * TPG + j
                ms = [m for m in range(5) if i - m >= 0]
                nb = len(ms)
                pBv = pC.rearrange("p (b x) -> p b x", b=2)[:, :, 0:260].rearrange("p b (h d) -> p b h d", d=65)
                kstep = 3

                def emit_S(h):
                    ch = h // 2
                    bi = h % 2
                    fns = []
                    for m in ms:
                        slot = (i - m) % 8
                        fns.append(lambda e, m=m: e.matmul(SX[bi][:, m * 128:(m + 1) * 128], lhsT=ident[:],
                                                           rhs=bm[:, h, m * 128:(m + 1) * 128], start=True, stop=False))
                        fns.append(lambda e, m=m, slot=slot: e.matmul(
                            SX[bi][:, m * 128:(m + 1) * 128], lhsT=kring[:, ch, slot * 128:(slot + 1) * 128],
                            rhs=qT[:, h, j * 128:(j + 1) * 128], start=False, stop=True))
                    PE(fns, r=[r_kring[0][ch], r_kring[1][ch], r_qT[h], r_bm, r_ident], w=[r_SX[bi]])

                emit_S(0)
                for h in range(8):
                    bi = h % 2
                    if h + 1 < 8:
                        emit_S(h + 1)
                    A(lambda e: e.activation(out=Eb[bi][:, 0:nb * 128], in_=SX[bi][:, 0:nb * 128], func=AF.Exp),
                      r=[r_SX[bi]], w=[r_Eb[bi]])
                    fns = []
                    for idx, m in enumerate(ms):
                        slot = (i - m) % 8
                        fns.append(lambda e, m=m, slot=slot, idx=idx: e.matmul(
                            pC[:, (h // 4) * 512 + (h % 4) * 65:(h // 4) * 512 + (h % 4) * 65 + 65],
                            lhsT=Eb[bi][:, m * 128:(m + 1) * 128], rhs=vring[:, slot, h, :], start=(idx == 0), stop=(idx == nb - 1)))
                    PE(fns, r=[r_Eb[bi]] + r_vring, w=[r_pC[h // 4]])
                    for _ in range(kstep):
                        if steps:
                            steps.pop(0)()
                if j == TPG - 1:
                    while steps:
                        steps.pop(0)()
                V(lambda e: e.reciprocal(out=rden8[:].rearrange("p (b h) -> p b h", b=2), in_=pBv[:, :, :, 64]),
                  r=r_pC, w=[r_rden8])
                V(lambda e: e.tensor_tensor(out=yat[:].rearrange("p (b h d) -> p b h d", b=2, h=4), in0=pBv[:, :, :, 0:64],
                                            in1=rden8[:].rearrange("p (b h) -> p b h", b=2).unsqueeze(3).broadcast_to([128, 2, 4, 64]),
                                            op=ALU.mult), r=r_pC + [r_rden8], w=[r_yat])
                A(lambda e: e.activation(out=junk[:, 0:512], in_=yat[:], func=AF.Square, accum_out=ssqa[:, j:j + 1]),
                  r=[r_yat], w=[r_junk, r_ssqa])
                A(lambda e: e.activation(out=yab[:], in_=yat[:], func=AF.Copy), r=[r_yat], w=[r_yab])
                mm_transpose(pE, lambda b: yab[:, b * 128:(b + 1) * 128], 4, [r_yab], [r_pE])
                V(lambda e: e.tensor_tensor(out=yaT[:, :, j * 128:(j + 1) * 128], in0=pE.rearrange("p (k t) -> p k t", k=4),
                                            in1=gnaw.unsqueeze(2).broadcast_to([128, 4, 128]), op=ALU.mult),
                  r=[r_pE, r_vecs], w=[r_yaT[j]])

            def out_proj(j):
                for half in range(2):
                    PE([(lambda e, c=c: e.matmul(pA[:, half * 512:(half + 1) * 512], lhsT=yrT[:, c, j * 128:(j + 1) * 128],
                                                 rhs=w_out[:, c, half * 512:(half + 1) * 512], start=(c == 0), stop=(c == 3)))
                        for c in range(4)], r=r_yrT + [r_w_out], w=[r_pA[half]])
                for half in range(2):
                    PE([(lambda e, c=c: e.matmul(pB[:, half * 512:(half + 1) * 512], lhsT=yaT[:, c, j * 128:(j + 1) * 128],
                                                 rhs=w_out[:, 4 + c, half * 512:(half + 1) * 512], start=(c == 0), stop=(c == 3)))
                        for c in range(4)], r=[r_yaT[j], r_w_out], w=[r_pB[half]])
                V(lambda e: e.scalar_tensor_tensor(out=xt[j][:], in0=pA[:], scalar=rstdg[:, j:j + 1], in1=xt[j][:],
                                                   op0=ALU.mult, op1=ALU.add), r=r_pA + [r_rstdg, r_xt[j]], w=[r_xt[j]])
                V(lambda e: e.scalar_tensor_tensor(out=xt[j][:], in0=pB[:], scalar=rstdg[:, 4 + j:5 + j], in1=xt[j][:],
                                                   op0=ALU.mult, op1=ALU.add), r=r_pB + [r_rstdg, r_xt[j]], w=[r_xt[j]])

            def cross_attn():
                for hh in range(4):
                    k = hh % 2
                    for mt in range(2):
                        PE([(lambda e, dc=dc: e.matmul(pB[:, mt * 512:(mt + 1) * 512], lhsT=kTm[:, 2 * hh + dc, mt * 128:(mt + 1) * 128],
                                                       rhs=qxT[:, 2 * hh + dc, :], start=(dc == 0), stop=(dc == 1))) for dc in range(2)],
                           r=[r_kTm, r_qxT[2 * hh], r_qxT[2 * hh + 1]], w=[r_pB[mt]])
                        A(lambda e: e.activation(out=ex[k][:, mt, :], in_=pB[:, mt * 512:(mt + 1) * 512], func=AF.Exp, scale=0.0625),
                          r=[r_pB[mt]], w=[r_ex[k]])
                    PE([(lambda e, mt=mt: e.matmul(pD[:], lhsT=ones_bf[:], rhs=ex[k][:, mt, :], start=(mt == 0), stop=(mt == 1)))
                        for mt in range(2)], r=[r_ones, r_ex[k]], w=[r_pD])
                    V(lambda e: e.reciprocal(out=rdn[:], in_=pD[:]), r=[r_pD], w=[r_rdn])
                    for dc in range(2):
                        PE([(lambda e, mt=mt: e.matmul(pC[:, dc * 512:(dc + 1) * 512],
                                                       lhsT=vm[:, mt, (2 * hh + dc) * 128:(2 * hh + dc + 1) * 128],
                                                       rhs=ex[k][:, mt, :], start=(mt == 0), stop=(mt == 1))) for mt in range(2)],
                           r=[r_vm, r_ex[k]], w=[r_pC[dc]])
                        V(lambda e: e.tensor_tensor(out=hT[:, 2 * hh + dc, :], in0=pC[:, dc * 512:(dc + 1) * 512], in1=rdn[:], op=ALU.mult),
                          r=[r_pC[dc], r_rdn], w=r_hT)

            def wo_proj(j):
                pw, r_pw = (pA, r_pA) if j % 2 == 0 else (pB, r_pB)
                for half in range(2):
                    PE([(lambda e, kc=kc: e.matmul(pw[:, half * 512:(half + 1) * 512], lhsT=hT[:, kc, j * 128:(j + 1) * 128],
                                                   rhs=wo[:, kc, half * 512:(half + 1) * 512], start=(kc == 0), stop=(kc == 7)))
                        for kc in range(8)], r=r_hT + [r_wo], w=[r_pw[half]])
                V(lambda e: e.tensor_tensor(out=xt[j][:], in0=pw, in1=xt[j][:], op=ALU.add), r=r_pw + [r_xt[j]], w=[r_xt[j]])

            def route_group(g):
                R = [r_rt]
                h3r = [mix[:, 2 * j:2 * j + 2, :].rearrange("p a t -> p (a t)") for j in range(TPG)]
                r_h3r = [[r_mix[2 * j], r_mix[2 * j + 1]] for j in range(TPG)]
                for j in range(TPG):
                    i = g * TPG + j
                    A(lambda e: e.activation(out=h3r[j], in_=xt[j][:], func=AF.Copy, scale=rstd[:, j:j + 1]),
                      r=[r_xt[j], r_rstd], w=r_h3r[j])
                    tk.dma("sp", "x2st%d" % j, lambda e: e.dma_start(out=x2s_d[i * 128:(i + 1) * 128, :], in_=xt[j][:]), r=[r_xt[j]], w=[r_x2s[i]])
                    pq, r_pq = (pB, r_pB) if j % 2 == 0 else (pC, r_pC)
                    mm_transpose(pq, lambda b: h3r[j][:, b * 128:(b + 1) * 128], 8, r_h3r[j], r_pq)
                    V(lambda e: e.tensor_tensor(out=h3T[:], in0=pq.rearrange("p (k t) -> p k t", k=8),
                                                in1=ln3w.unsqueeze(2).broadcast_to([128, 8, 128]), op=ALU.mult), r=r_pq + [r_vecs], w=[r_h3T])
                    PE([(lambda e, kc=kc: e.matmul(pD[:, j * 64:j * 64 + 36], lhsT=h3T[:, kc, :], rhs=wr[:, kc, :], start=(kc == 0), stop=(kc == 7)))
                        for kc in range(8)], r=[r_h3T, r_wr], w=[r_pD])
                pDl = pD[:, 0:256].rearrange("p (j c) -> p j c", j=4)[:, :, 0:36]
                V(lambda e: e.tensor_tensor(out=lg4, in0=pDl, in1=rb_bc[:].unsqueeze(1).broadcast_to([128, 4, 36]), op=ALU.add),
                  r=[r_pD, r_rb], w=R)
                lgG = lg4[:, :, 0:4]
                lgE = lg4[:, :, 4:36].rearrange("p j (g e) -> p j g e", g=4)
                V(lambda e: e.tensor_reduce(out=gmax4, in_=lgG, axis=AX.X, op=ALU.max), r=R, w=R)
                V(lambda e: e.tensor_tensor(out=gsh4, in0=lgG, in1=gmax4.unsqueeze(2).broadcast_to([128, 4, 4]), op=ALU.subtract), r=R, w=R)
                A(lambda e: e.activation(out=gex4, in_=gsh4, func=AF.Exp), r=R, w=R)
                V(lambda e: e.tensor_reduce(out=gsum4, in_=gex4, axis=AX.X, op=ALU.add), r=R, w=R)
                V(lambda e: e.reciprocal(out=gp4, in_=gsum4), r=R, w=R)
                V(lambda e: e.tensor_tensor(out=goh4, in0=lgG, in1=gmax4.unsqueeze(2).broadcast_to([128, 4, 4]), op=ALU.is_ge), r=R, w=R)
                V(lambda e: e.tensor_tensor(out=prod4, in0=lgE, in1=goh4.unsqueeze(3).broadcast_to([128, 4, 4, 8]), op=ALU.mult), r=R, w=R)
                V(lambda e: e.tensor_reduce(out=el4, in_=prod4.rearrange("p j g e -> p j e g"), axis=AX.X, op=ALU.add), r=R, w=R)
                V(lambda e: e.tensor_reduce(out=m1_4, in_=el4, axis=AX.X, op=ALU.max), r=R, w=R)
                V(lambda e: e.tensor_tensor(out=oh1_4, in0=el4, in1=m1_4.unsqueeze(2).broadcast_to([128, 4, 8]), op=ALU.is_ge), r=R, w=R)
                V(lambda e: e.scalar_tensor_tensor(out=el2_4, in0=oh1_4, scalar=-1e30, in1=el4, op0=ALU.mult, op1=ALU.add), r=R, w=R)
                V(lambda e: e.tensor_reduce(out=m2_4, in_=el2_4, axis=AX.X, op=ALU.max), r=R, w=R)
                V(lambda e: e.tensor_tensor(out=oh2_4, in0=el2_4, in1=m2_4.unsqueeze(2).broadcast_to([128, 4, 8]), op=ALU.is_ge), r=R, w=R)
                V(lambda e: e.tensor_tensor(out=dd4, in0=m2_4, in1=m1_4, op=ALU.subtract), r=R, w=R)
                A(lambda e: e.activation(out=edd4, in_=dd4, func=AF.Exp), r=R, w=R)
                V(lambda e: e.tensor_scalar(out=w1_4, in0=edd4, scalar1=1.0, scalar2=None, op0=ALU.add), r=R, w=R)
                V(lambda e: e.reciprocal(out=w1_4, in_=w1_4), r=R, w=R)
                V(lambda e: e.tensor_tensor(out=w2_4, in0=edd4, in1=w1_4, op=ALU.mult), r=R, w=R)
                cwv = cw_all[:, 8 * g:8 * g + 8].rearrange("p (j k) -> p j k", k=2)
                r_cwg = [r_cw[g * TPG + j] for j in range(TPG)]
                V(lambda e: e.tensor_tensor(out=cwv[:, :, 0], in0=w1_4, in1=gp4, op=ALU.mult), r=R, w=r_cwg)
                V(lambda e: e.tensor_tensor(out=cwv[:, :, 1], in0=w2_4, in1=gp4, op=ALU.mult), r=R + r_cwg, w=r_cwg)
                gohb = goh4.unsqueeze(3).broadcast_to([128, 4, 4, 8])
                V(lambda e: e.tensor_tensor(out=A1_4.rearrange("p j (g e) -> p j g e", g=4), in0=gohb,
                                            in1=oh1_4.unsqueeze(2).broadcast_to([128, 4, 4, 8]), op=ALU.mult), r=R, w=R)
                V(lambda e: e.tensor_tensor(out=A2_4.rearrange("p j (g e) -> p j g e", g=4), in0=gohb,
                                            in1=oh2_4.unsqueeze(2).broadcast_to([128, 4, 4, 8]), op=ALU.mult), r=R, w=R)
                V(lambda e: e.tensor_tensor(out=Ab4[:], in0=A1_4, in1=A2_4, op=ALU.add), r=R, w=[r_Ab])
                for j in range(TPG):
                    fns = [lambda e: e.matmul(pD[:, 256 + j * 32:256 + (j + 1) * 32], lhsT=tri_bf[:], rhs=Ab4[:, j, :], start=True, stop=(j == 0))]
                    for jp in range(j):
                        fns.append(lambda e, jp=jp: e.matmul(pD[:, 256 + j * 32:256 + (j + 1) * 32], lhsT=ones_bf[:], rhs=Ab4[:, jp, :],
                                                             start=False, stop=(jp == j - 1)))
                    PE(fns, r=[r_tri, r_ones, r_Ab], w=[r_pD])
                PE([(lambda e, j=j: e.matmul(pD[:, 384:416], lhsT=ones_bf[:], rhs=Ab4[:, j, :], start=(j == 0), stop=(j == TPG - 1)))
                    for j in range(TPG)], r=[r_ones, r_Ab], w=[r_pD])
                V(lambda e: e.tensor_tensor(out=posf4, in0=pD[:, 256:384].rearrange("p (j e) -> p j e", j=4),
                                            in1=cnt[:].unsqueeze(1).broadcast_to([128, 4, NE]), op=ALU.add), r=[r_pD, r_cnt], w=R)
                V(lambda e: e.tensor_tensor(out=cnt[:], in0=pD[:, 384:416], in1=cnt[:], op=ALU.add), r=[r_pD, r_cnt], w=[r_cnt])
                eoffb = eoff[:].unsqueeze(1).broadcast_to([128, 4, NE])
                for kk, Af in enumerate((A1_4, A2_4)):
                    V(lambda e: e.tensor_tensor(out=prodE, in0=Af, in1=posf4, op=ALU.mult), r=R, w=R)
                    V(lambda e: e.tensor_reduce(out=pk4[:, :, kk], in_=prodE, axis=AX.X, op=ALU.add), r=R, w=R)
                    V(lambda e: e.tensor_tensor(out=prodE, in0=Af, in1=eoffb, op=ALU.mult), r=R + [r_eoff], w=R)
                    V(lambda e: e.tensor_reduce(out=ek4[:, :, kk], in_=prodE, axis=AX.X, op=ALU.add), r=R, w=R)
                V(lambda e: e.tensor_scalar(out=ok4, in0=pk4, scalar1=float(CAP), scalar2=None, op0=ALU.is_lt), r=R, w=R)
                V(lambda e: e.tensor_tensor(out=sd4, in0=pk4, in1=ek4, op=ALU.add), r=R, w=R)
                V(lambda e: e.scalar_tensor_tensor(out=sd4, in0=sd4, scalar=-float(TRASH), in1=ok4, op0=ALU.add, op1=ALU.mult), r=R, w=R)
                V(lambda e: e.tensor_scalar(out=sd4, in0=sd4, scalar1=float(TRASH), scalar2=None, op0=ALU.add), r=R, w=R)
                r_dg = [r_dest[g * TPG + j] for j in range(TPG)]
                V(lambda e: e.tensor_copy(out=dest_all[:, 8 * g:8 * g + 8], in_=sd4.rearrange("p j k -> p (j k)")), r=R, w=r_dg)
                def do_scatter():
                    for j in range(TPG):
                        i = g * TPG + j
                        for kk in range(2):
                            tk.dma("pool", "sc%d" % j, lambda e: e.indirect_dma_start(
                                out=xs_d, out_offset=bass.IndirectOffsetOnAxis(ap=dest_all[:, 2 * i + kk:2 * i + kk + 1], axis=0),
                                in_=h3r[j], in_offset=None), r=r_h3r[j] + [r_dest[i], r_xsz], w=[Res()])
                return do_scatter

            pending = None
            for g in range(NG):
                par = g % 2
                load_x(g)
                for j in range(TPG):
                    norm_stats(j)
                rstd_op(ssq[:], 4, D, stmp[:, 0:4], rstd[:], [r_ssq], r_stmp, r_rstd)
                if pending is not None:
                    pending()
                for j in range(TPG):
                    norm_to_hT(j, ln1w)
                rnn_all = []
                for c_ in range(4):
                    rnn_all.extend(rnn_steps(g, c_))

                def pump(n):
                    for _ in range(n):
                        if rnn_all:
                            rnn_all.pop(0)()

                GP(lambda e: e.memset(mix[:, 0:8, :].rearrange("p a t -> p (a t)"), 0.0), w=r_mix[0:8])
                for c in range(4):
                    proj_fm(w_in, r_w_in, 1024 + c * 128, pA[:, (c % 2) * 512:(c % 2 + 1) * 512], r_pA[c % 2])
                    for hh_ in range(2):
                        A(lambda e: e.activation(out=qT[hh_ * 64:(hh_ + 1) * 64, 2 * c + hh_, :],
                                                 in_=pA[hh_ * 64:(hh_ + 1) * 64, (c % 2) * 512:(c % 2 + 1) * 512], func=AF.Copy, scale=0.125),
                          r=[r_pA[c % 2]], w=[r_qT[2 * c + hh_]])
                    pump(3)
                for c in range(4):
                    proj_fm(w_in, r_w_in, 1536 + c * 128, pA[:, (c % 2) * 512:(c % 2 + 1) * 512], r_pA[c % 2])
                    A(lambda e: e.activation(out=kring[:, c, par * 512:(par + 1) * 512], in_=pA[:, (c % 2) * 512:(c % 2 + 1) * 512],
                                             func=AF.Copy), r=[r_pA[c % 2]], w=[r_kring[par][c]])
                    pump(3)
                for j in range(TPG):
                    slot = (g * TPG + j) % 8
                    PE([(lambda e, kc=kc: e.matmul(pC[:, (j % 2) * 512:(j % 2 + 1) * 512], lhsT=hT[:, kc, j * 128:(j + 1) * 128],
                                                   rhs=w_in[:, kc, 2048:2560], start=(kc == 0), stop=(kc == 7))) for kc in range(8)],
                       r=r_w_in + [r_hT[j]], w=[r_pC[j % 2]])
                    V(lambda e: e.tensor_copy(out=vring[:, slot, :, 0:64],
                                              in_=pC[:, (j % 2) * 512:(j % 2 + 1) * 512].rearrange("p (h d) -> p h d", h=8)),
                      r=[r_pC[j % 2]], w=[r_vring[slot]])
                    pump(3)
                for j in range(TPG):
                    att_tile(g, j, rnn_all)
                V(lambda e: e.tensor_copy(out=stmp[:, 0:4], in_=ssqr[:]), r=[r_ssqr], w=[r_stmp])
                V(lambda e: e.tensor_copy(out=stmp[:, 4:8], in_=ssqa[:]), r=[r_ssqa], w=[r_stmp])
                V(lambda e: e.tensor_scalar(out=stmp[:], in0=stmp[:], scalar1=1.0 / 512, scalar2=EPS, op0=ALU.mult, op1=ALU.add),
                  r=[r_stmp], w=[r_stmp])
                GP(lambda e: e.tensor_tensor(out=rstdg[:], in0=stmp[:], in1=neg05[:], op=ALU.pow), r=[r_stmp, r_neg], w=[r_rstdg])
                for j in range(TPG):
                    out_proj(j)
                    norm_stats(j)
                rstd_op(ssq[:], 4, D, stmp[:, 0:4], rstd[:], [r_ssq], r_stmp, r_rstd)
                for j in range(TPG):
                    norm_to_hT(j, ln2w)
                for fo in range(8):
                    proj_fm(wq, [r_wq], fo * 128, pA[:, (fo % 2) * 512:(fo % 2 + 1) * 512], r_pA[fo % 2])
                    A(lambda e: e.activation(out=qxT[:, fo, :], in_=pA[:, (fo % 2) * 512:(fo % 2 + 1) * 512], func=AF.Copy),
                      r=[r_pA[fo % 2]], w=[r_qxT[fo]])
                cross_attn()
                for j in range(TPG):
                    wo_proj(j)
                    norm_stats(j)
                rstd_op(ssq[:], 4, D, stmp[:, 0:4], rstd[:], [r_ssq], r_stmp, r_rstd)
                pending = route_group(g)
            pending()
            tk.barrier()

        with ExitStack() as sbk:
            NB = 3
            wg = [sb("wg%d" % k, [128, 8, 512], BF16, sbk) for k in range(NB)]
            wu = [sb("wu%d" % k, [128, 8, 512], BF16, sbk) for k in range(NB)]
            wd = [sb("wd%d" % k, [128, 4, D], BF16, sbk) for k in range(NB)]
            r_wg = [Res() for _ in range(NB)]; r_wu = [Res() for _ in range(NB)]; r_wd = [Res() for _ in range(NB)]
            xsl = [sb("xsl%d" % k, [128, CAP // 128, D], BF16, sbk) for k in range(NB)]
            r_xsl = [Res() for _ in range(NB)]
            xsT = sb("xsT", [128, 8, CAP], BF16, sbk); r_xsT = Res()
            sg = [sb("sg%d" % k, [128, CAP], F32, sbk) for k in range(2)]
            r_sg = [Res(), Res()]
            aT = sb("aT", [128, 4, CAP], BF16, sbk)
            r_aT = [Res() for _ in range(4)]
            yb = [sb("yb%d" % k, [128, D], F32, sbk) for k in range(2)]
            r_yb = [Res(), Res()]

            def load_expert(e_):
                k = e_ % NB
                tk.dma("pool", "ewg%d" % k, lambda e: e.dma_start(out=wg[k][:], in_=eg_d[e_].rearrange("(k p) n -> p k n", p=128)), w=[r_wg[k]])
                tk.dma("pool", "ewu%d" % k, lambda e: e.dma_start(out=wu[k][:], in_=eu_d[e_].rearrange("(k p) n -> p k n", p=128)), w=[r_wu[k]])
                tk.dma("pool", "ewd%d" % k, lambda e: e.dma_start(out=wd[k][:], in_=ed_d[e_].rearrange("(k p) n -> p k n", p=128)), w=[r_wd[k]])
                tk.dma("sp", "xsl%d" % k, lambda e: e.dma_start(out=xsl[k][:], in_=xs_d[e_ * CAP:(e_ + 1) * CAP, :].rearrange("(t p) d -> p t d", p=128)),
                       w=[r_xsl[k]])

            xsT2 = [xsT, sb("xsT_b", [128, 8, CAP], BF16, sbk)]
            r_xsT2 = [r_xsT, Res()]

            def tr_steps(e_):
                k = e_ % NB
                xo, r_xo = xsT2[e_ % 2], r_xsT2[e_ % 2]
                st_ = []
                n = 0
                for t in range(CAP // 128):
                    for hf in range(2):
                        pq, r_pq = (pD, r_pD) if n % 2 == 0 else (pE, r_pE)
                        n += 1
                        st_.append(lambda t=t, hf=hf, pq=pq, r_pq=r_pq: mm_transpose(
                            pq, lambda b: xsl[k][:, t, (hf * 4 + b) * 128:(hf * 4 + b + 1) * 128], 4, [r_xsl[k]], [r_pq]))
                        st_.append(lambda t=t, hf=hf, pq=pq, r_pq=r_pq: V(lambda e: e.tensor_tensor(
                            out=xo[:, hf * 4:(hf + 1) * 4, t * 128:(t + 1) * 128], in0=pq.rearrange("p (k t) -> p k t", k=4),
                            in1=ln3w[:, hf * 4:(hf + 1) * 4].unsqueeze(2).broadcast_to([128, 4, 128]), op=ALU.mult),
                            r=[r_pq, r_vecs], w=[r_xo]))
                return st_

            def main_steps(e_):
                k = e_ % NB
                xo, r_xo = xsT2[e_ % 2], r_xsT2[e_ % 2]
                st_ = []
                for fc in range(4):
                    b2 = fc % 2
                    st_.append(lambda fc=fc, b2=b2: PE([(lambda e, kc=kc: e.matmul(pA[:, b2 * 512:b2 * 512 + CAP], lhsT=wg[k][:, kc, fc * 128:(fc + 1) * 128],
                                                                                 rhs=xo[:, kc, :], start=(kc == 0), stop=(kc == 7))) for kc in range(8)],
                                                       r=[r_wg[k], r_xo], w=[r_pA[b2]]))
                    st_.append(lambda fc=fc, b2=b2: PE([(lambda e, kc=kc: e.matmul(pB[:, b2 * 512:b2 * 512 + CAP], lhsT=wu[k][:, kc, fc * 128:(fc + 1) * 128],
                                                                                 rhs=xo[:, kc, :], start=(kc == 0), stop=(kc == 7))) for kc in range(8)],
                                                       r=[r_wu[k], r_xo], w=[r_pB[b2]]))
                    st_.append(lambda b2=b2: A(lambda e: e.activation(out=sg[b2][:], in_=pA[:, b2 * 512:b2 * 512 + CAP], func=AF.Silu),
                                               r=[r_pA[b2]], w=[r_sg[b2]]))
                    st_.append(lambda fc=fc, b2=b2: V(lambda e: e.tensor_tensor(out=aT[:, fc, :], in0=pB[:, b2 * 512:b2 * 512 + CAP], in1=sg[b2][:], op=ALU.mult),
                                                      r=[r_pB[b2], r_sg[b2]], w=[r_aT[fc]]))
                for t in range(CAP // 128):
                    yk = (e_ * (CAP // 128) + t) % 2
                    for half in range(2):
                        st_.append(lambda t=t, half=half: PE([(lambda e, fc=fc: e.matmul(pC[:, half * 512:(half + 1) * 512], lhsT=aT[:, fc, t * 128:(t + 1) * 128],
                                                                                         rhs=wd[k][:, fc, half * 512:(half + 1) * 512], start=(fc == 0), stop=(fc == 3)))
                                                              for fc in range(4)], r=r_aT + [r_wd[k]], w=[r_pC[half]]))
                    st_.append(lambda yk=yk: A(lambda e: e.activation(out=yb[yk][:, 0:512], in_=pC[:, 0:512], func=AF.Copy), r=[r_pC[0], r_yb[yk]], w=[r_yb[yk]]))
                    st_.append(lambda yk=yk: V(lambda e: e.tensor_copy(out=yb[yk][:, 512:1024], in_=pC[:, 512:1024]), r=[r_pC[1], r_yb[yk]], w=[r_yb[yk]]))
                    row0 = e_ * CAP + t * 128
                    st_.append(lambda yk=yk, row0=row0: tk.dma("sp", "ys%d" % yk, lambda e: e.dma_start(out=ys_d[row0:row0 + 128, :], in_=yb[yk][:]),
                                                               r=[r_yb[yk]], w=[Res()]))
                return st_

            load_expert(0)
            load_expert(1)
            for f_ in tr_steps(0):
                f_()
            for e_ in range(NE):
                if e_ + 2 < NE:
                    load_expert(e_ + 2)
                ms_ = main_steps(e_)
                ts_ = tr_steps(e_ + 1) if e_ + 1 < NE else []
                while ms_ or ts_:
                    for _ in range(2):
                        if ms_:
                            ms_.pop(0)()
                    if ts_:
                        ts_.pop(0)()
            tk.barrier()

        with ExitStack() as sc:
            NBC = 4
            x2t = [sb("x2t%d" % k, [128, D], F32, sc) for k in range(NBC)]
            y1t = [sb("y1t%d" % k, [128, D], F32, sc) for k in range(NBC)]
            y2t = [sb("y2t%d" % k, [128, D], F32, sc) for k in range(NBC)]
            r_x2t = [Res() for _ in range(NBC)]; r_y1t = [Res() for _ in range(NBC)]; r_y2t = [Res() for _ in range(NBC)]
            junkc = [sb("junkc%d" % k, [128, D], BF16, sc) for k in range(2)]; r_junkc = [Res(), Res()]
            fin_bc = sb("fin_bc", [128, D], F32, sc); r_fin = Res()
            tk.dma("sp", "fin", lambda e: e.dma_start(out=fin_bc[:], in_=fin_d.partition_broadcast(128)), w=[r_fin])
            fss = sb("fss", [128, NBC], F32, sc); r_fss = [Res() for _ in range(NBC)]
            ftmp = sb("ftmp", [128, NBC], F32, sc); r_ftmp = [Res() for _ in range(NBC)]
            frs = sb("frs", [128, NBC], F32, sc); r_frs = [Res() for _ in range(NBC)]

            def load_c(i):
                k = i % NBC
                tk.dma("sp", "cx%d" % k, lambda e: e.dma_start(out=x2t[k][:], in_=x2s_d[i * 128:(i + 1) * 128, :]), r=[r_x2s[i]], w=[r_x2t[k]])
                tk.dma("pool", "cy1%d" % k, lambda e: e.indirect_dma_start(
                    out=y1t[k][:], out_offset=None, in_=ys_d, in_offset=bass.IndirectOffsetOnAxis(ap=dest_all[:, 2 * i:2 * i + 1], axis=0)),
                    r=[r_dest[i]], w=[r_y1t[k]])
                tk.dma("pool", "cy2%d" % k, lambda e: e.indirect_dma_start(
                    out=y2t[k][:], out_offset=None, in_=ys_d, in_offset=bass.IndirectOffsetOnAxis(ap=dest_all[:, 2 * i + 1:2 * i + 2], axis=0)),
                    r=[r_dest[i]], w=[r_y2t[k]])

            def c_steps(i):
                k = i % NBC
                jk = i % 2
                st_ = []
                st_.append(lambda: V(lambda e: e.scalar_tensor_tensor(out=x2t[k][:], in0=y1t[k][:], scalar=cw_all[:, 2 * i:2 * i + 1], in1=x2t[k][:],
                                                                      op0=ALU.mult, op1=ALU.add), r=[r_y1t[k], r_cw[i], r_x2t[k]], w=[r_x2t[k]]))
                st_.append(lambda: V(lambda e: e.scalar_tensor_tensor(out=x2t[k][:], in0=y2t[k][:], scalar=cw_all[:, 2 * i + 1:2 * i + 2], in1=x2t[k][:],
                                                                      op0=ALU.mult, op1=ALU.add), r=[r_y2t[k], r_cw[i], r_x2t[k]], w=[r_x2t[k]]))
                st_.append(lambda: A(lambda e: e.activation(out=junkc[jk][:], in_=x2t[k][:], func=AF.Square, accum_out=fss[:, k:k + 1]),
                                     r=[r_x2t[k]], w=[r_junkc[jk], r_fss[k]]))
                st_.append(lambda: V(lambda e: e.tensor_scalar(out=ftmp[:, k:k + 1], in0=fss[:, k:k + 1], scalar1=1.0 / D, scalar2=EPS,
                                                               op0=ALU.mult, op1=ALU.add), r=[r_fss[k]], w=[r_ftmp[k]]))
                st_.append(lambda: GP(lambda e: e.tensor_tensor(out=frs[:, k:k + 1], in0=ftmp[:, k:k + 1], in1=neg05[:, 0:1], op=ALU.pow),
                                      r=[r_ftmp[k], r_neg], w=[r_frs[k]]))
                st_.append(lambda: A(lambda e: e.activation(out=y2t[k][:], in_=x2t[k][:], func=AF.Copy, scale=frs[:, k:k + 1]),
                                     r=[r_x2t[k], r_frs[k]], w=[r_y2t[k]]))
                st_.append(lambda: V(lambda e: e.tensor_tensor(out=y1t[k][:], in0=y2t[k][:], in1=fin_bc[:], op=ALU.mult),
                                     r=[r_y2t[k], r_fin], w=[r_y1t[k]]))
                st_.append(lambda: tk.dma("sp", "out%d" % k, lambda e: e.dma_start(out=out_d[i * 128:(i + 1) * 128, :], in_=y1t[k][:]), r=[r_y1t[k]]))
                return st_

            load_c(0)
            load_c(1)
            for i in range(0, NT, 2):
                for t_ in (i + 2, i + 3):
                    if t_ < NT:
                        load_c(t_)
                a_ = c_steps(i)
                b_ = c_steps(i + 1)
                while a_ or b_:
                    if a_:
                        a_.pop(0)()
                    if b_:
                        b_.pop(0)()
            for k in range(NBC):
                ds = tk.dsem("out%d" % k)
                nc.sync.wait_ge(ds.sem, ds.cnt)
    return nc


def _prep_shared(inp):
    f = np.float32
    vecs = np.zeros((128, NV), f)

    def pc(v, n):
        return np.ascontiguousarray(np.asarray(v, f).reshape(n, 128).T)

    vecs[:, 0:8] = pc(inp["ln1_w"][0], 8)
    vecs[:, 8:16] = pc(inp["ln2_w"][0], 8)
    vecs[:, 16:20] = pc(inp["gn_rnn_w"][0], 4)
    vecs[:, 20:24] = pc(inp["gn_att_w"][0], 4)
    vecs[:, 24:28] = pc(inp["conv_b"][0], 4)
    vecs[:, 28:32] = pc(inp["rnn_ba"][0], 4)
    vecs[:, 32:36] = pc(inp["rnn_bx"][0], 4)
    vecs[:, 36:40] = pc(inp["rnn_lambda"][0], 4)
    vecs[:, 56:64] = pc(inp["ln3_w"][0], 8)
    cwt = np.asarray(inp["conv_w"][0], f)
    for c in range(4):
        for j in range(4):
            vecs[:, 40 + c * 4 + j] = cwt[j, c * 128:(c + 1) * 128]
    bd = np.zeros((128, 2, 4, 128), f)
    for gi, name in enumerate(("rnn_wa", "rnn_wx")):
        w = np.asarray(inp[name][0], f)
        for c in range(4):
            for b in range(2):
                bd[b * 64:(b + 1) * 64, gi, c, b * 64:(b + 1) * 64] = w[2 * c + b]
    rb = np.asarray(inp["rel_bias"][0], f)
    kk = np.arange(128)[:, None]
    qq = np.arange(640)[None, :]
    idx = np.clip(qq - kk, -128, 128) + 128
    valid = np.where(kk < 64, (qq < 576), (qq >= 64))
    bm = np.empty((128, 8, 640), f)
    for h in range(8):
        bm[:, h, :] = np.where(valid, rb[h][idx], f(-30000.0))
    wr = np.concatenate([np.asarray(inp["router_group_w"][0], f),
                         np.asarray(inp["router_expert_w"][0], f).transpose(1, 0, 2).reshape(D, 32)], axis=1)
    rbias = np.concatenate([np.asarray(inp["router_group_b"][0], f), np.asarray(inp["router_expert_b"][0], f).reshape(32)])
    shared = {
        "vecs": vecs, "bm": bm, "bd": bd, "wr": np.ascontiguousarray(wr), "rb": np.ascontiguousarray(rbias),
        "w_in": np.ascontiguousarray(inp["w_in"][0], f), "w_out": np.ascontiguousarray(inp["w_out"][0], f),
        "wq": np.ascontiguousarray(inp["xq_w"][0], f), "wk": np.ascontiguousarray(inp["xk_w"][0], f),
        "wv": np.ascontiguousarray(inp["xv_w"][0], f), "wo": np.ascontiguousarray(inp["xo_w"][0], f),
        "memw": np.ascontiguousarray(inp["mem_norm_w"], f),
        "fin": np.ascontiguousarray(inp["final_norm_w"], f),
        "eg": np.ascontiguousarray(inp["expert_gate_w"][0], f), "eu": np.ascontiguousarray(inp["expert_up_w"][0], f),
        "ed": np.ascontiguousarray(inp["expert_down_w"][0], f),
    }
    return shared


def kernel(**inputs):
    inp = {k: np.asarray(v) for k, v in inputs.items()}
    shared = _prep_shared(inp)
    nc = build_nc()
    in_maps = []
    for b in range(8):
        m = dict(shared)
        m["x"] = np.ascontiguousarray(inp["x"][b], np.float32)
        m["mem"] = np.ascontiguousarray(inp["mem"][b], np.float32)
        in_maps.append(m)
    res = run_bass_kernel_spmd(nc, in_maps, core_ids=list(range(8)))
    return np.stack([np.asarray(r["out"], np.float32) for r in res.results], axis=0)
```

```python
import numpy as np
from contextlib import ExitStack

import concourse.bass as bass
import concourse.mybir as mybir
from concourse.bass_utils import run_bass_kernel_spmd

F32 = mybir.dt.float32
BF16 = mybir.dt.bfloat16
I32 = mybir.dt.int32
AF = mybir.ActivationFunctionType
ALU = mybir.AluOpType
AX = mybir.AxisListType

S = 4096
D = 1024
NT = 32
GT = 512
NG = 8
TPG = 4
NE = 32
CAP = 512
NSLOT = NE * CAP
NROWS = NSLOT + 1024
TRASH = NSLOT
EPS = 1e-6
NV = 64
GELU_K = 0.7978845608028654


class Res:
    __slots__ = ("w", "r")

    def __init__(self):
        self.w = {}
        self.r = {}


class DSem:
    def __init__(self, sem):
        self.sem = sem
        self.cnt = 0


class TK:
    def __init__(self, nc, stack):
        self.nc = nc
        self.stack = stack
        self.eng = {"pe": nc.tensor, "dve": nc.vector, "act": nc.scalar, "pool": nc.gpsimd, "sp": nc.sync}
        self.esem = {}
        self.ecnt = {}
        self.seen = {}
        for k in self.eng:
            self.esem[k] = stack.enter_context(nc.semaphore("es_" + k))
            self.ecnt[k] = 0
            self.seen[k] = {}
        self.dsems = []
        self.dsd = {}

    def dsem(self, name):
        if name not in self.dsd:
            d = DSem(self.stack.enter_context(self.nc.semaphore("m_" + name)))
            self.dsems.append(d)
            self.dsd[name] = d
        return self.dsd[name]

    @staticmethod
    def _merge(d, tokd):
        for k, (s, v) in tokd.items():
            if v is None or k not in d or d[k][1] < v:
                d[k] = (s, v)

    @staticmethod
    def _flat(rs):
        out = []
        for r in rs:
            if isinstance(r, (list, tuple)):
                out.extend(TK._flat(r))
            else:
                out.append(r)
        return out

    def _deps(self, reads, writes):
        d = {}
        for r in self._flat(reads):
            self._merge(d, r.w)
        for w in self._flat(writes):
            self._merge(d, w.w)
            self._merge(d, w.r)
        return d

    def _wait(self, e, deps):
        E = self.eng[e]
        seen = self.seen[e]
        for k, (s, v) in deps.items():
            if e == "pe" and k == "pe":
                continue
            if v is None:
                sem, val = s.sem, s.cnt
            else:
                sem, val = s, v
            if seen.get(k, 0) < val:
                E.wait_ge(sem, val)
                seen[k] = val

    @staticmethod
    def _update(reads, writes, key, tok):
        for r in TK._flat(reads):
            r.r[key] = tok
        for w in TK._flat(writes):
            w.w = {key: tok}
            w.r = {}

    def op(self, e, fn, r=(), w=()):
        self._wait(e, self._deps(r, w))
        ins = fn(self.eng[e])
        self.ecnt[e] += 1
        ins.then_inc(self.esem[e], 1)
        self._update(r, w, e, (self.esem[e], self.ecnt[e]))

    def ops(self, e, fns, r=(), w=()):
        self._wait(e, self._deps(r, w))
        ins = None
        for fn in fns:
            ins = fn(self.eng[e])
        self.ecnt[e] += 1
        ins.then_inc(self.esem[e], 1)
        self._update(r, w, e, (self.esem[e], self.ecnt[e]))

    def dma(self, q, name, fn, r=(), w=()):
        ds = self.dsem(name)
        self._wait(q, self._deps(r, w))
        ins = fn(self.eng[q])
        ds.cnt += 16
        ins.then_inc(ds.sem, 16)
        self._update(r, w, "d_" + name, (ds, None))

    def barrier(self):
        d = {}
        for k in self.eng:
            if self.ecnt[k] > 0:
                d[k] = (self.esem[k], self.ecnt[k])
        for name, ds in self.dsd.items():
            if ds.cnt > 0:
                d["d_" + name] = (ds.sem, ds.cnt)
        for e in self.eng:
            E = self.eng[e]
            seen = self.seen[e]
            for k, (s, v) in d.items():
                if k == e:
                    continue
                if seen.get(k, 0) < v:
                    E.wait_ge(s, v)
                    seen[k] = v


def build_nc():
    nc = bass.Bass("TRN2", target_bir_lowering=False)

    def din(name, shape, dt=F32):
        return nc.dram_tensor(name, shape, dt, kind="ExternalInput").ap()

    x_d = din("x", [S, D])
    mem_d = din("mem", [256, D])
    vecs_d = din("vecs", [128, NV])
    bm_d = din("bm", [128, 8, 640])
    bd_d = din("bd", [128, 2, 4, 128])
    wr_d = din("wr", [D, 36])
    rb_d = din("rb", [36])
    w_in_d = din("w_in", [D, 2560])
    w_out_d = din("w_out", [D, D])
    wq_d = din("wq", [D, D])
    wk_d = din("wk", [D, D])
    wv_d = din("wv", [D, D])
    wo_d = din("wo", [D, D])
    memw_d = din("memw", [D])
    fin_d = din("fin", [D])
    eg_d = din("eg", [NE, D, 512])
    eu_d = din("eu", [NE, D, 512])
    ed_d = din("ed", [NE, 512, D])
    out_d = nc.dram_tensor("out", [S, D], F32, kind="ExternalOutput").ap()
    x2s_d = nc.dram_tensor("x2s", [S, D], F32, kind="Internal").ap()
    xs_d = nc.dram_tensor("xs", [NROWS, D], BF16, kind="Internal").ap()
    ys_d = nc.dram_tensor("ys", [NSLOT + 1, D], F32, kind="Internal").ap()

    with ExitStack() as st:
        tk = TK(nc, st)

        def sb(name, shape, dt, stack=st):
            return stack.enter_context(nc.sbuf_tensor("s_" + name, shape, dt))

        def psum(name, shape, dt):
            return st.enter_context(nc.psum_tensor(name, shape, dt))

        def V(fn, r=(), w=()):
            tk.op("dve", fn, r, w)

        def A(fn, r=(), w=()):
            tk.op("act", fn, r, w)

        def GP(fn, r=(), w=()):
            tk.op("pool", fn, r, w)

        def PE(fns, r=(), w=()):
            tk.ops("pe", fns, r, w)

        pall = psum("pall", [128, 8 * 512], F32)
        pA = pall[:, 0:1024]
        pB = pall[:, 1024:2048]
        pC = pall[:, 2048:3072]
        pD = pall[:, 3072:3584]
        pE = pall[:, 3584:4096]
        r_pE = Res()
        r_pA = [Res(), Res()]
        r_pB = [Res(), Res()]
        r_pC = [Res(), Res()]
        r_pD = Res()
        SX = [pA[:, 0:640], pB[:, 0:640]]
        r_SX = [r_pA, r_pB]

        identf = sb("identf", [128, 128], F32)
        ident = sb("ident", [128, 128], BF16)
        ones_bf = sb("ones_bf", [128, 128], BF16)
        trif = sb("trif", [128, 128], F32)
        tri_bf = sb("tri_bf", [128, 128], BF16)
        vecs = sb("vecs", [128, NV], F32)
        hb = sb("hb", [128, 8], F32)
        hc = sb("hc", [128, 8], F32)
        spt = sb("spt", [128, 4], F32)
        rb_bc = sb("rb_bc", [128, 36], F32)
        dest_all = sb("dest_all", [128, 2 * NT], I32)
        cw_all = sb("cw_all", [128, 2 * NT], F32)
        cnt = sb("cnt", [128, NE], F32)
        eoff_i = sb("eoff_i", [128, NE], I32)
        eoff = sb("eoff", [128, NE], F32)
        neg05 = sb("neg05", [128, 8], F32)
        qtr = sb("qtr", [128, 1], F32)
        hstate = sb("hstate", [128, 4], F32)
        halo = sb("halo", [128, 4, 3], F32)
        zt = sb("zt", [128, 8], F32)
        r_ident = Res(); r_identf = Res(); r_ones = Res(); r_trif = Res(); r_tri = Res()
        r_vecs = Res(); r_hb = Res(); r_hc = Res(); r_spt = Res()
        r_rb = Res()
        r_dest = [Res() for _ in range(NT)]
        r_cw = [Res() for _ in range(NT)]
        r_cnt = Res(); r_eoffi = Res(); r_eoff = Res(); r_neg = Res()
        r_hstate = [Res() for _ in range(4)]
        r_halo = [Res() for _ in range(4)]
        r_zrow = Res()
        r_x2s = [Res() for _ in range(NT)]
        r_xs = Res()
        r_ys = Res()

        ln1w = vecs[:, 0:8]
        ln2w = vecs[:, 8:16]
        gnrw = vecs[:, 16:20]
        gnaw = vecs[:, 20:24]
        convb = vecs[:, 24:28]
        lam = vecs[:, 36:40]
        ln3w = vecs[:, 56:64]

        tk.dma("sp", "vecs", lambda e: e.dma_start(out=vecs[:], in_=vecs_d), w=[r_vecs])
        tk.dma("sp", "rb", lambda e: e.dma_start(out=rb_bc[:], in_=rb_d.partition_broadcast(128)), w=[r_rb])
        GP(lambda e: e.memset(identf[:], 0.0), w=[r_identf])
        GP(lambda e: e.affine_select(out=identf[:], in_=identf[:], pattern=[[-1, 128]], compare_op=ALU.not_equal,
                                     fill=1.0, base=0, channel_multiplier=1), r=[r_identf], w=[r_identf])
        V(lambda e: e.tensor_copy(out=ident[:], in_=identf[:]), r=[r_identf], w=[r_ident])
        GP(lambda e: e.memset(trif[:], 1.0), w=[r_trif])
        GP(lambda e: e.affine_select(out=trif[:], in_=trif[:], pattern=[[1, 128]], compare_op=ALU.is_gt,
                                     fill=0.0, base=0, channel_multiplier=-1), r=[r_trif], w=[r_trif])
        V(lambda e: e.tensor_copy(out=tri_bf[:], in_=trif[:]), r=[r_trif], w=[r_tri])
        V(lambda e: e.memset(ones_bf[:], 1.0), w=[r_ones])
        GP(lambda e: e.iota(eoff_i[:], pattern=[[CAP, NE]], base=0, channel_multiplier=0), w=[r_eoffi])
        V(lambda e: e.tensor_copy(out=eoff[:], in_=eoff_i[:]), r=[r_eoffi], w=[r_eoff])
        V(lambda e: e.memset(neg05[:], -0.5), w=[r_neg])
        V(lambda e: e.memset(qtr[:], 0.25), w=[r_neg])
        V(lambda e: e.memset(cnt[:], 0.0), w=[r_cnt])
        V(lambda e: e.memset(hstate[:], 0.0), w=r_hstate)
        V(lambda e: e.memset(halo[:], 0.0), w=r_halo)
        V(lambda e: e.memset(zt[:], 0.0), w=[r_zrow])
        tk.dma("sp", "zt", lambda e: e.dma_start(out=ys_d[TRASH].rearrange("(p f) -> p f", p=128), in_=zt[:]), r=[r_zrow], w=[Res()])
        V(lambda e: e.tensor_scalar(out=hb[:], in0=vecs[:, 28:36], scalar1=0.5, scalar2=None, op0=ALU.mult), r=[r_vecs], w=[r_hb])
        A(lambda e: e.activation(out=spt[:], in_=lam, func=AF.Exp, scale=-1.0), r=[r_vecs], w=[r_spt])
        A(lambda e: e.activation(out=spt[:], in_=spt[:], func=AF.Ln, bias=1.0), r=[r_spt], w=[r_spt])
        V(lambda e: e.tensor_scalar(out=hc[:, 0:4], in0=spt[:], scalar1=-4.0, scalar2=None, op0=ALU.mult), r=[r_spt], w=[r_hc])
        V(lambda e: e.tensor_scalar(out=hc[:, 4:8], in0=spt[:], scalar1=-8.0, scalar2=None, op0=ALU.mult), r=[r_spt], w=[r_hc])

        def rstd_op(src_ap, n, dim, tmp_ap, out_ap, r_src, r_tmp, r_out):
            V(lambda e: e.tensor_scalar(out=tmp_ap, in0=src_ap, scalar1=1.0 / dim, scalar2=EPS, op0=ALU.mult, op1=ALU.add),
              r=r_src, w=[r_tmp])
            GP(lambda e: e.tensor_tensor(out=out_ap, in0=tmp_ap, in1=neg05[:, 0:n], op=ALU.pow), r=[r_tmp, r_neg], w=[r_out])

        with ExitStack() as sa:
            w_in = sb("w_in", [128, 8, 2560], BF16, sa)
            w_out = sb("w_out", [128, 8, D], BF16, sa)
            wq = sb("wq", [128, 8, D], BF16, sa)
            wo = sb("wo", [128, 8, D], BF16, sa)
            bd = sb("bd", [128, 2, 4, 128], BF16, sa)
            wr = sb("wr", [128, 8, 36], BF16, sa)
            bm = sb("bm", [128, 8, 640], BF16, sa)
            kTm = sb("kTm", [128, 8, 256], BF16, sa)
            vm = sb("vm", [128, 2, D], BF16, sa)
            kring = sb("kring", [128, 4, 1024], BF16, sa)
            vring = sb("vring", [128, 8, 8, 65], BF16, sa)
            r_w_in = [Res(), Res()]; r_w_out = Res(); r_wq = Res(); r_wo = Res(); r_bd = Res(); r_wr = Res(); r_bm = Res()
            r_kTm = Res(); r_vm = Res()
            r_kring = [[Res() for _ in range(4)] for _ in range(2)]
            r_vring = [Res() for _ in range(8)]

            def wview(dram):
                return dram.rearrange("(k p) n -> p k n", p=128)

            def mm_transpose(dst, src_fn, nblk, r, w):
                PE([(lambda e, b=b: e.matmul(dst[:, b * 128:(b + 1) * 128], lhsT=src_fn(b), rhs=ident[:], start=True, stop=True))
                    for b in range(nblk)], r=list(r) + [r_ident], w=w)


            zt2 = sb("zt2", [128, D], BF16, sa); r_zt2 = Res(); r_xsz = Res()
            V(lambda e: e.memset(zt2[:], 0.0), w=[r_zt2])

            tk.dma("pool", "w_in0", lambda e: e.dma_start(out=w_in[:, :, 0:1280], in_=wview(w_in_d)[:, :, 0:1280]), w=[r_w_in[0]])
            tk.dma("pool", "w_in1", lambda e: e.dma_start(out=w_in[:, :, 1280:2560], in_=wview(w_in_d)[:, :, 1280:2560]), w=[r_w_in[1]])

            with ExitStack() as s0:
                wk = sb("wk", [128, 8, D], BF16, s0)
                wv = sb("wv", [128, 8, D], BF16, s0)
                memt = sb("memt", [128, 2, D], F32, s0)
                memn = sb("memn", [128, 2, D], BF16, s0)
                memT = sb("memT", [128, 8, 256], BF16, s0)
                memw_bc = sb("memw_bc", [128, D], F32, s0)
                junk0 = sb("junk0", [128, D], F32, s0)
                mss = sb("mss", [128, 2], F32, s0)
                mtmp = sb("mtmp", [128, 2], F32, s0)
                mrs = sb("mrs", [128, 2], F32, s0)
                r_wk = Res(); r_wv = Res(); r_memt = Res(); r_memn = Res(); r_memT = Res(); r_memw = Res()
                r_junk0 = Res(); r_mss = Res(); r_mtmp = Res(); r_mrs = Res()
                tk.dma("sp", "memt", lambda e: e.dma_start(out=memt[:], in_=mem_d.rearrange("(t p) d -> p t d", p=128)), w=[r_memt])
                tk.dma("sp", "memw", lambda e: e.dma_start(out=memw_bc[:], in_=memw_d.partition_broadcast(128)), w=[r_memw])
                tk.dma("pool", "wk", lambda e: e.dma_start(out=wk[:], in_=wview(wk_d)), w=[r_wk])
                tk.dma("pool", "wv", lambda e: e.dma_start(out=wv[:], in_=wview(wv_d)), w=[r_wv])
                for t in range(2):
                    A(lambda e: e.activation(out=junk0[:], in_=memt[:, t, :], func=AF.Square, accum_out=mss[:, t:t + 1]),
                      r=[r_memt], w=[r_junk0, r_mss])
                rstd_op(mss[:], 2, D, mtmp[:], mrs[:], [r_mss], r_mtmp, r_mrs)
                for t in range(2):
                    V(lambda e: e.scalar_tensor_tensor(out=memn[:, t, :], in0=memt[:, t, :], scalar=mrs[:, t:t + 1], in1=memw_bc[:],
                                                       op0=ALU.mult, op1=ALU.mult), r=[r_memt, r_mrs, r_memw], w=[r_memn])
                    mm_transpose(pC, lambda b: memn[:, t, b * 128:(b + 1) * 128], 8, [r_memn], r_pC)
                    V(lambda e: e.tensor_copy(out=memT[:, :, t * 128:(t + 1) * 128], in_=pC.rearrange("p (k t) -> p k t", k=8)),
                      r=r_pC, w=[r_memT])
                for fo in range(8):
                    bank = fo % 2
                    PE([(lambda e, kc=kc: e.matmul(pA[:, bank * 512:bank * 512 + 256], lhsT=wk[:, kc, fo * 128:(fo + 1) * 128],
                                                   rhs=memT[:, kc, :], start=(kc == 0), stop=(kc == 7))) for kc in range(8)],
                       r=[r_wk, r_memT], w=[r_pA[bank]])
                    A(lambda e: e.activation(out=kTm[:, fo, :], in_=pA[:, bank * 512:bank * 512 + 256], func=AF.Copy),
                      r=[r_pA[bank]], w=[r_kTm])
                for mt in range(2):
                    for half in range(2):
                        PE([(lambda e, kc=kc: e.matmul(pB[:, half * 512:(half + 1) * 512], lhsT=memT[:, kc, mt * 128:(mt + 1) * 128],
                                                       rhs=wv[:, kc, half * 512:(half + 1) * 512], start=(kc == 0), stop=(kc == 7)))
                            for kc in range(8)], r=[r_wv, r_memT], w=[r_pB[half]])
                        A(lambda e: e.activation(out=vm[:, mt, half * 512:(half + 1) * 512], in_=pB[:, half * 512:(half + 1) * 512],
                                                 func=AF.Copy), r=[r_pB[half]], w=[r_vm])
                tk.barrier()

            tk.dma("pool", "w_out", lambda e: e.dma_start(out=w_out[:], in_=wview(w_out_d)), w=[r_w_out])
            tk.dma("pool", "wq", lambda e: e.dma_start(out=wq[:], in_=wview(wq_d)), w=[r_wq])
            tk.dma("pool", "wo", lambda e: e.dma_start(out=wo[:], in_=wview(wo_d)), w=[r_wo])
            tk.dma("pool", "bd", lambda e: e.dma_start(out=bd[:], in_=bd_d), w=[r_bd])
            tk.dma("pool", "wr", lambda e: e.dma_start(out=wr[:], in_=wview(wr_d)), w=[r_wr])
            tk.dma("pool", "bm", lambda e: e.dma_start(out=bm[:], in_=bm_d), w=[r_bm])
            V(lambda e: e.memset(vring[:].rearrange("p a b c -> p (a b c)"), 1.0), w=r_vring)
            xs_z = xs_d.rearrange("(p r) d -> p r d", p=128)
            for zi in range(NROWS // 128):
                tk.dma("act", "xsz", lambda e: e.dma_start(out=xs_z[:, zi, :], in_=zt2[:]), r=[r_zt2], w=[r_xsz])

            xt = [sb("xt%d" % j, [128, D], F32, sa) for j in range(TPG)]
            r_xt = [Res() for _ in range(TPG)]
            xn = [sb("xn0", [128, D], BF16, sa)] * 2
            r_xn = [Res()] * 2
            junk = xn[0]; r_junk = r_xn[0]
            hT = sb("hT", [128, 8, GT], BF16, sa)
            r_hT = [Res() for _ in range(TPG)]
            mix = sb("mix", [128, 12, GT], BF16, sa)
            r_mix = [Res() for _ in range(12)]
            qT = mix[:, 0:8, :]
            r_qT = r_mix[0:8]
            yrT = mix[:, 8:12, :]
            r_yrT = r_mix[8:12]
            yaT = sb("yaT", [128, 4, GT], BF16, sa)
            r_yaT = [Res() for _ in range(TPG)]
            qxT = mix[:, 0:8, :]
            r_qxT = r_mix[0:8]
            ssq = sb("ssq", [128, 4], F32, sa); r_ssq = Res()
            stmp = sb("stmp", [128, 8], F32, sa); r_stmp = Res()
            rstd = sb("rstd", [128, 4], F32, sa); r_rstd = Res()
            ssqr = sb("ssqr", [128, 4], F32, sa); r_ssqr = Res()
            ssqa = sb("ssqa", [128, 4], F32, sa); r_ssqa = Res()
            rstdg = sb("rstdg", [128, 8], F32, sa); r_rstdg = Res()
            xrp = sb("xrp", [128, GT + 3], F32, sa); r_xrp = Res()
            xc = sb("xc", [128, GT], F32, sa); r_xc = Res()
            xcb = sb("xcb", [128, GT], BF16, sa); r_xcb = Res()
            tr_ = sb("tr_", [128, GT], F32, sa); r_tr = Res()
            ti_ = sb("ti_", [128, GT], F32, sa); r_ti = Res()
            av = sb("av", [128, GT], F32, sa); r_av = Res()
            vv = sb("vv", [128, GT], F32, sa); r_vv = Res()
            hs = sb("hs", [128, GT], F32, sa); r_hs = Res()
            xgs = sb("xgs", [128, GT], F32, sa); r_xgs = Res()
            g1 = sb("g1", [128, GT], F32, sa); r_g1 = Res()
            g2 = sb("g2", [128, GT], F32, sa); r_g2 = Res()
            a2 = hs; r_a2 = r_hs
            gl = av; r_gl = r_av
            yraw = vv; r_yraw = r_vv
            ysq = sb("ysq", [128, GT], BF16, sa); r_ysq = Res()
            Eb = [sb("Eb%d" % k, [128, 640], BF16, sa) for k in range(2)]
            r_Eb = [Res(), Res()]
            rden8 = sb("rden8", [128, 8], F32, sa); r_rden8 = Res()
            ex = [sb("ex0", [128, 2, GT], BF16, sa)] * 2
            r_ex = [Res()] * 2
            rdn = tr_; r_rdn = r_tr
            h3 = [sb("h3_%d" % k, [128, D], BF16, sa) for k in range(2)]
            r_h3 = [Res(), Res()]
            h3T = sb("h3T", [128, 8, 128], BF16, sa); r_h3T = Res()
            yat = h3[0][:].bitcast(F32); r_yat = r_h3[0]
            yab = Eb[0][:, 0:512]; r_yab = r_Eb[0]
            rt = sb("rt", [128, 772], F32, sa)
            r_rt = Res()
            Ab4 = xcb[:, 0:4 * NE].rearrange("p (j e) -> p j e", j=4); r_Ab = r_xcb

            def rtv(a, b, **kw):
                v = rt[:, a:b]
                return v.rearrange(kw.pop("pat"), **kw) if kw else v

            lg4 = rtv(0, 144, pat="p (j c) -> p j c", j=4)
            gmax4 = rt[:, 144:148]
            gsum4 = rt[:, 148:152]
            gp4 = rt[:, 152:156]
            m1_4 = rt[:, 156:160]
            m2_4 = rt[:, 160:164]
            dd4 = rt[:, 164:168]
            edd4 = rt[:, 168:172]
            w1_4 = rt[:, 172:176]
            w2_4 = rt[:, 176:180]
            gsh4 = rtv(180, 196, pat="p (j c) -> p j c", j=4)
            gex4 = rtv(196, 212, pat="p (j c) -> p j c", j=4)
            goh4 = rtv(212, 228, pat="p (j c) -> p j c", j=4)
            el4 = rtv(228, 260, pat="p (j c) -> p j c", j=4)
            oh1_4 = rtv(260, 292, pat="p (j c) -> p j c", j=4)
            el2_4 = rtv(292, 324, pat="p (j c) -> p j c", j=4)
            oh2_4 = rtv(324, 356, pat="p (j c) -> p j c", j=4)
            pk4 = rtv(356, 364, pat="p (j k) -> p j k", j=4)
            ek4 = rtv(364, 372, pat="p (j k) -> p j k", j=4)
            ok4 = rtv(372, 380, pat="p (j k) -> p j k", j=4)
            sd4 = rtv(380, 388, pat="p (j k) -> p j k", j=4)
            prod4 = rtv(388, 516, pat="p (j g e) -> p j g e", j=4, g=4)
            A1_4 = rtv(516, 644, pat="p (j c) -> p j c", j=4)
            A2_4 = rtv(644, 772, pat="p (j c) -> p j c", j=4)
            posf4 = rtv(0, 128, pat="p (j c) -> p j c", j=4)
            prodE = rtv(388, 516, pat="p (j c) -> p j c", j=4)

            def load_x(g):
                for j in range(TPG):
                    i = g * TPG + j
                    tk.dma("sp", "x%d" % j, lambda e: e.dma_start(out=xt[j][:], in_=x_d[i * 128:(i + 1) * 128, :]), w=[r_xt[j]])

            def norm_stats(j):
                A(lambda e: e.activation(out=junk[:], in_=xt[j][:], func=AF.Square, accum_out=ssq[:, j:j + 1]),
                  r=[r_xt[j]], w=[r_junk, r_ssq])

            def norm_to_hT(j, lnw):
                k = j % 2
                A(lambda e: e.activation(out=xn[k][:], in_=xt[j][:], func=AF.Copy, scale=rstd[:, j:j + 1]),
                  r=[r_xt[j], r_rstd], w=[r_xn[k]])
                pq, r_pq = (pB, r_pB) if j % 2 == 0 else (pC, r_pC)
                mm_transpose(pq, lambda b: xn[k][:, b * 128:(b + 1) * 128], 8, [r_xn[k]], r_pq)
                V(lambda e: e.tensor_tensor(out=hT[:, :, j * 128:(j + 1) * 128], in0=pq.rearrange("p (k t) -> p k t", k=8),
                                            in1=lnw.unsqueeze(2).broadcast_to([128, 8, 128]), op=ALU.mult),
                  r=r_pq + [r_vecs], w=[r_hT[j]])

            def proj_fm(wt, r_w, col0, bank_ap, r_bank):
                PE([(lambda e, kc=kc: e.matmul(bank_ap, lhsT=wt[:, kc, col0:col0 + 128], rhs=hT[:, kc, :],
                                               start=(kc == 0), stop=(kc == 7))) for kc in range(8)],
                   r=list(r_w) + r_hT, w=[r_bank])

            def rnn_steps(g, c):
                C0 = pD
                rC0 = r_pD
                cw0 = 40 + c * 4
                M_ = []
                G_ = []
                T_ = []
                G_.append(lambda: proj_fm(w_in, r_w_in, 512 + c * 128, C0, rC0))
                G_.append(lambda: V(lambda e: e.tensor_copy(out=xgs[:], in_=C0), r=[rC0], w=[r_xgs]))
                G_.append(lambda: A(lambda e: e.activation(out=g1[:], in_=xgs[:], func=AF.Square), r=[r_xgs], w=[r_g1]))
                G_.append(lambda: GP(lambda e: e.tensor_scalar(out=g1[:], in0=g1[:], scalar1=0.044715, scalar2=1.0, op0=ALU.mult, op1=ALU.add),
                                     r=[r_g1], w=[r_g1]))
                G_.append(lambda: GP(lambda e: e.tensor_tensor(out=g1[:], in0=xgs[:], in1=g1[:], op=ALU.mult), r=[r_xgs, r_g1], w=[r_g1]))
                G_.append(lambda: A(lambda e: e.activation(out=g2[:], in_=g1[:], func=AF.Tanh, scale=GELU_K), r=[r_g1], w=[r_g2]))
                G_.append(lambda: V(lambda e: e.scalar_tensor_tensor(out=g2[:], in0=g2[:], scalar=1.0, in1=xgs[:], op0=ALU.add, op1=ALU.mult),
                                    r=[r_g2, r_xgs], w=[r_g2]))
                M_.append(lambda: proj_fm(w_in, r_w_in, c * 128, C0, rC0))
                M_.append(lambda: V(lambda e: e.tensor_copy(out=xrp[:, 3:GT + 3], in_=C0), r=[rC0], w=[r_xrp]))
                M_.append(lambda: V(lambda e: e.tensor_copy(out=xrp[:, 0:3], in_=halo[:, c, :]), r=[r_halo[c]], w=[r_xrp]))
                M_.append(lambda: V(lambda e: e.tensor_scalar(out=xc[:], in0=xrp[:, 3:GT + 3], scalar1=vecs[:, cw0 + 3:cw0 + 4],
                                                               scalar2=convb[:, c:c + 1], op0=ALU.mult, op1=ALU.add),
                                    r=[r_xrp, r_vecs], w=[r_xc]))
                for jj in range(3):
                    M_.append(lambda jj=jj: V(lambda e: e.scalar_tensor_tensor(out=xc[:], in0=xrp[:, jj:jj + GT],
                                                                               scalar=vecs[:, cw0 + jj:cw0 + jj + 1], in1=xc[:],
                                                                               op0=ALU.mult, op1=ALU.add),
                                              r=[r_xrp, r_vecs, r_xc], w=[r_xc]))
                M_.append(lambda: V(lambda e: e.tensor_copy(out=halo[:, c, :], in_=xrp[:, GT:GT + 3]), r=[r_xrp], w=[r_halo[c]]))
                M_.append(lambda: GP(lambda e: e.tensor_copy(out=xcb[:], in_=xc[:]), r=[r_xc], w=[r_xcb]))
                M_.append(lambda: PE([lambda e: e.matmul(C0, lhsT=bd[:, 0, c, :], rhs=xcb[:], start=True, stop=True)],
                                     r=[r_bd, r_xcb], w=[rC0]))
                M_.append(lambda: A(lambda e: e.activation(out=tr_[:], in_=C0, func=AF.Tanh, scale=0.5, bias=hb[:, c:c + 1]),
                                    r=[rC0, r_hb], w=[r_tr]))
                M_.append(lambda: PE([lambda e: e.matmul(C0, lhsT=bd[:, 1, c, :], rhs=xcb[:], start=True, stop=True)],
                                     r=[r_bd, r_xcb], w=[rC0]))
                M_.append(lambda: A(lambda e: e.activation(out=ti_[:], in_=C0, func=AF.Tanh, scale=0.5, bias=hb[:, 4 + c:5 + c]),
                                    r=[rC0, r_hb], w=[r_ti]))
                M_.append(lambda: A(lambda e: e.activation(out=av[:], in_=tr_[:], func=AF.Exp, scale=hc[:, c:c + 1], bias=hc[:, c:c + 1]),
                                    r=[r_tr, r_hc], w=[r_av]))
                M_.append(lambda: A(lambda e: e.activation(out=a2[:], in_=tr_[:], func=AF.Exp, scale=hc[:, 4 + c:5 + c],
                                                           bias=hc[:, 4 + c:5 + c]), r=[r_tr, r_hc], w=[r_a2]))
                M_.append(lambda: V(lambda e: e.scalar_tensor_tensor(out=vv[:], in0=ti_[:], scalar=1.0, in1=xc[:], op0=ALU.add, op1=ALU.mult),
                                    r=[r_ti, r_xc], w=[r_vv]))
                M_.append(lambda: A(lambda e: e.activation(out=a2[:], in_=a2[:], func=AF.Sqrt, scale=-0.25, bias=qtr[:, 0:1]),
                                    r=[r_a2, r_neg], w=[r_a2]))
                M_.append(lambda: V(lambda e: e.tensor_tensor(out=vv[:], in0=vv[:], in1=a2[:], op=ALU.mult), r=[r_vv, r_a2], w=[r_vv]))
                M_.append(lambda: V(lambda e: e.tensor_tensor_scan(out=hs[:], data0=av[:], data1=vv[:], initial=hstate[:, c:c + 1],
                                                                    op0=ALU.mult, op1=ALU.add), r=[r_av, r_vv, r_hstate[c]], w=[r_hs]))
                M_.append(lambda: V(lambda e: e.tensor_copy(out=hstate[:, c:c + 1], in_=hs[:, GT - 1:GT]), r=[r_hs], w=[r_hstate[c]]))
                T_.append(lambda: V(lambda e: e.scalar_tensor_tensor(out=yraw[:], in0=g2[:], scalar=0.5, in1=hs[:], op0=ALU.mult, op1=ALU.mult),
                                    r=[r_g2, r_hs], w=[r_yraw]))
                T_.append(lambda: V(lambda e: e.tensor_scalar(out=yrT[:, c, :], in0=yraw[:], scalar1=gnrw[:, c:c + 1], scalar2=None, op0=ALU.mult),
                                    r=[r_yraw, r_vecs], w=[r_yrT[c]]))
                T_.append(lambda: GP(lambda e: e.tensor_tensor(out=ysq[:], in0=yraw[:], in1=yraw[:], op=ALU.mult), r=[r_yraw], w=[r_ysq]))
                T_.append(lambda: PE([(lambda e, j=j: e.matmul(C0[:, j:j + 1], lhsT=ysq[:, j * 128:(j + 1) * 128], rhs=ones_bf[:, 0:1],
                                                               start=True, stop=True)) for j in range(TPG)], r=[r_ysq, r_ones], w=[rC0]))
                if c == 0:
                    T_.append(lambda: V(lambda e: e.tensor_copy(out=ssqr[:], in_=C0[:, 0:4]), r=[rC0], w=[r_ssqr]))
                else:
                    T_.append(lambda: V(lambda e: e.tensor_tensor(out=ssqr[:], in0=C0[:, 0:4], in1=ssqr[:], op=ALU.add),
                                        r=[rC0, r_ssqr], w=[r_ssqr]))
                out_ = []
                gi = 0
                for mi, m_ in enumerate(M_):
                    out_.append(m_)
                    if mi % 3 == 1 and gi < len(G_):
                        out_.append(G_[gi]); gi += 1
                out_.extend(G_[gi:])
                out_.extend(T_)
                return out_

            def att_tile(g, j, steps):
                i = g * TPG + j
                ms = [m for m in range(5) if i - m >= 0]
                nb = len(ms)
                pBv = pC.rearrange("p (b x) -> p b x", b=2)[:, :, 0:260].rearrange("p b (h d) -> p b h d", d=65)
                kstep = 3

                def emit_S(h):
                    ch = h // 2
                    bi = h % 2
                    fns = []
                    for m in ms:
                        slot = (i - m) % 8
                        fns.append(lambda e, m=m: e.matmul(SX[bi][:, m * 128:(m + 1) * 128], lhsT=ident[:],
                                                           rhs=bm[:, h, m * 128:(m + 1) * 128], start=True, stop=False))
                        fns.append(lambda e, m=m, slot=slot: e.matmul(
                            SX[bi][:, m * 128:(m + 1) * 128], lhsT=kring[:, ch, slot * 128:(slot + 1) * 128],
                            rhs=qT[:, h, j * 128:(j + 1) * 128], start=False, stop=True))
                    PE(fns, r=[r_kring[0][ch], r_kring[1][ch], r_qT[h], r_bm, r_ident], w=[r_SX[bi]])

                emit_S(0)
                for h in range(8):
                    bi = h % 2
                    if h + 1 < 8:
                        emit_S(h + 1)
                    A(lambda e: e.activation(out=Eb[bi][:, 0:nb * 128], in_=SX[bi][:, 0:nb * 128], func=AF.Exp),
                      r=[r_SX[bi]], w=[r_Eb[bi]])
                    fns = []
                    for idx, m in enumerate(ms):
                        slot = (i - m) % 8
                        fns.append(lambda e, m=m, slot=slot, idx=idx: e.matmul(
                            pC[:, (h // 4) * 512 + (h % 4) * 65:(h // 4) * 512 + (h % 4) * 65 + 65],
                            lhsT=Eb[bi][:, m * 128:(m + 1) * 128], rhs=vring[:, slot, h, :], start=(idx == 0), stop=(idx == nb - 1)))
                    PE(fns, r=[r_Eb[bi]] + r_vring, w=[r_pC[h // 4]])
                    for _ in range(kstep):
                        if steps:
                            steps.pop(0)()
                if j == TPG - 1:
                    while steps:
                        steps.pop(0)()
                V(lambda e: e.reciprocal(out=rden8[:].rearrange("p (b h) -> p b h", b=2), in_=pBv[:, :, :, 64]),
                  r=r_pC, w=[r_rden8])
                V(lambda e: e.tensor_tensor(out=yat[:].rearrange("p (b h d) -> p b h d", b=2, h=4), in0=pBv[:, :, :, 0:64],
                                            in1=rden8[:].rearrange("p (b h) -> p b h", b=2).unsqueeze(3).broadcast_to([128, 2, 4, 64]),
                                            op=ALU.mult), r=r_pC + [r_rden8], w=[r_yat])
                A(lambda e: e.activation(out=junk[:, 0:512], in_=yat[:], func=AF.Square, accum_out=ssqa[:, j:j + 1]),
                  r=[r_yat], w=[r_junk, r_ssqa])
                A(lambda e: e.activation(out=yab[:], in_=yat[:], func=AF.Copy), r=[r_yat], w=[r_yab])
                mm_transpose(pE, lambda b: yab[:, b * 128:(b + 1) * 128], 4, [r_yab], [r_pE])
                V(lambda e: e.tensor_tensor(out=yaT[:, :, j * 128:(j + 1) * 128], in0=pE.rearrange("p (k t) -> p k t", k=4),
                                            in1=gnaw.unsqueeze(2).broadcast_to([128, 4, 128]), op=ALU.mult),
                  r=[r_pE, r_vecs], w=[r_yaT[j]])

            def out_proj(j):
                for half in range(2):
                    PE([(lambda e, c=c: e.matmul(pA[:, half * 512:(half + 1) * 512], lhsT=yrT[:, c, j * 128:(j + 1) * 128],
                                                 rhs=w_out[:, c, half * 512:(half + 1) * 512], start=(c == 0), stop=(c == 3)))
                        for c in range(4)], r=r_yrT + [r_w_out], w=[r_pA[half]])
                for half in range(2):
                    PE([(lambda e, c=c: e.matmul(pB[:, half * 512:(half + 1) * 512], lhsT=yaT[:, c, j * 128:(j + 1) * 128],
                                                 rhs=w_out[:, 4 + c, half * 512:(half + 1) * 512], start=(c == 0), stop=(c == 3)))
                        for c in range(4)], r=[r_yaT[j], r_w_out], w=[r_pB[half]])
                V(lambda e: e.scalar_tensor_tensor(out=xt[j][:], in0=pA[:], scalar=rstdg[:, j:j + 1], in1=xt[j][:],
                                                   op0=ALU.mult, op1=ALU.add), r=r_pA + [r_rstdg, r_xt[j]], w=[r_xt[j]])
                V(lambda e: e.scalar_tensor_tensor(out=xt[j][:], in0=pB[:], scalar=rstdg[:, 4 + j:5 + j], in1=xt[j][:],
                                                   op0=ALU.mult, op1=ALU.add), r=r_pB + [r_rstdg, r_xt[j]], w=[r_xt[j]])

            def cross_attn():
                for hh in range(4):
                    k = hh % 2
                    for mt in range(2):
                        PE([(lambda e, dc=dc: e.matmul(pB[:, mt * 512:(mt + 1) * 512], lhsT=kTm[:, 2 * hh + dc, mt * 128:(mt + 1) * 128],
                                                       rhs=qxT[:, 2 * hh + dc, :], start=(dc == 0), stop=(dc == 1))) for dc in range(2)],
                           r=[r_kTm, r_qxT[2 * hh], r_qxT[2 * hh + 1]], w=[r_pB[mt]])
                        A(lambda e: e.activation(out=ex[k][:, mt, :], in_=pB[:, mt * 512:(mt + 1) * 512], func=AF.Exp, scale=0.0625),
                          r=[r_pB[mt]], w=[r_ex[k]])
                    PE([(lambda e, mt=mt: e.matmul(pD[:], lhsT=ones_bf[:], rhs=ex[k][:, mt, :], start=(mt == 0), stop=(mt == 1)))
                        for mt in range(2)], r=[r_ones, r_ex[k]], w=[r_pD])
                    V(lambda e: e.reciprocal(out=rdn[:], in_=pD[:]), r=[r_pD], w=[r_rdn])
                    for dc in range(2):
                        PE([(lambda e, mt=mt: e.matmul(pC[:, dc * 512:(dc + 1) * 512],
                                                       lhsT=vm[:, mt, (2 * hh + dc) * 128:(2 * hh + dc + 1) * 128],
                                                       rhs=ex[k][:, mt, :], start=(mt == 0), stop=(mt == 1))) for mt in range(2)],
                           r=[r_vm, r_ex[k]], w=[r_pC[dc]])
                        V(lambda e: e.tensor_tensor(out=hT[:, 2 * hh + dc, :], in0=pC[:, dc * 512:(dc + 1) * 512], in1=rdn[:], op=ALU.mult),
                          r=[r_pC[dc], r_rdn], w=r_hT)

            def wo_proj(j):
                pw, r_pw = (pA, r_pA) if j % 2 == 0 else (pB, r_pB)
                for half in range(2):
                    PE([(lambda e, kc=kc: e.matmul(pw[:, half * 512:(half + 1) * 512], lhsT=hT[:, kc, j * 128:(j + 1) * 128],
                                                   rhs=wo[:, kc, half * 512:(half + 1) * 512], start=(kc == 0), stop=(kc == 7)))
                        for kc in range(8)], r=r_hT + [r_wo], w=[r_pw[half]])
                V(lambda e: e.tensor_tensor(out=xt[j][:], in0=pw, in1=xt[j][:], op=ALU.add), r=r_pw + [r_xt[j]], w=[r_xt[j]])

            def route_group(g):
                R = [r_rt]
                h3r = [mix[:, 2 * j:2 * j + 2, :].rearrange("p a t -> p (a t)") for j in range(TPG)]
                r_h3r = [[r_mix[2 * j], r_mix[2 * j + 1]] for j in range(TPG)]
                for j in range(TPG):
                    i = g * TPG + j
                    A(lambda e: e.activation(out=h3r[j], in_=xt[j][:], func=AF.Copy, scale=rstd[:, j:j + 1]),
                      r=[r_xt[j], r_rstd], w=r_h3r[j])
                    tk.dma("sp", "x2st%d" % j, lambda e: e.dma_start(out=x2s_d[i * 128:(i + 1) * 128, :], in_=xt[j][:]), r=[r_xt[j]], w=[r_x2s[i]])
                    pq, r_pq = (pB, r_pB) if j % 2 == 0 else (pC, r_pC)
                    mm_transpose(pq, lambda b: h3r[j][:, b * 128:(b + 1) * 128], 8, r_h3r[j], r_pq)
                    V(lambda e: e.tensor_tensor(out=h3T[:], in0=pq.rearrange("p (k t) -> p k t", k=8),
                                                in1=ln3w.unsqueeze(2).broadcast_to([128, 8, 128]), op=ALU.mult), r=r_pq + [r_vecs], w=[r_h3T])
                    PE([(lambda e, kc=kc: e.matmul(pD[:, j * 64:j * 64 + 36], lhsT=h3T[:, kc, :], rhs=wr[:, kc, :], start=(kc == 0), stop=(kc == 7)))
                        for kc in range(8)], r=[r_h3T, r_wr], w=[r_pD])
                pDl = pD[:, 0:256].rearrange("p (j c) -> p j c", j=4)[:, :, 0:36]
                V(lambda e: e.tensor_tensor(out=lg4, in0=pDl, in1=rb_bc[:].unsqueeze(1).broadcast_to([128, 4, 36]), op=ALU.add),
                  r=[r_pD, r_rb], w=R)
                lgG = lg4[:, :, 0:4]
                lgE = lg4[:, :, 4:36].rearrange("p j (g e) -> p j g e", g=4)
                V(lambda e: e.tensor_reduce(out=gmax4, in_=lgG, axis=AX.X, op=ALU.max), r=R, w=R)
                V(lambda e: e.tensor_tensor(out=gsh4, in0=lgG, in1=gmax4.unsqueeze(2).broadcast_to([128, 4, 4]), op=ALU.subtract), r=R, w=R)
                A(lambda e: e.activation(out=gex4, in_=gsh4, func=AF.Exp), r=R, w=R)
                V(lambda e: e.tensor_reduce(out=gsum4, in_=gex4, axis=AX.X, op=ALU.add), r=R, w=R)
                V(lambda e: e.reciprocal(out=gp4, in_=gsum4), r=R, w=R)
                V(lambda e: e.tensor_tensor(out=goh4, in0=lgG, in1=gmax4.unsqueeze(2).broadcast_to([128, 4, 4]), op=ALU.is_ge), r=R, w=R)
                V(lambda e: e.tensor_tensor(out=prod4, in0=lgE, in1=goh4.unsqueeze(3).broadcast_to([128, 4, 4, 8]), op=ALU.mult), r=R, w=R)
                V(lambda e: e.tensor_reduce(out=el4, in_=prod4.rearrange("p j g e -> p j e g"), axis=AX.X, op=ALU.add), r=R, w=R)
                V(lambda e: e.tensor_reduce(out=m1_4, in_=el4, axis=AX.X, op=ALU.max), r=R, w=R)
                V(lambda e: e.tensor_tensor(out=oh1_4, in0=el4, in1=m1_4.unsqueeze(2).broadcast_to([128, 4, 8]), op=ALU.is_ge), r=R, w=R)
                V(lambda e: e.scalar_tensor_tensor(out=el2_4, in0=oh1_4, scalar=-1e30, in1=el4, op0=ALU.mult, op1=ALU.add), r=R, w=R)
                V(lambda e: e.tensor_reduce(out=m2_4, in_=el2_4, axis=AX.X, op=ALU.max), r=R, w=R)
                V(lambda e: e.tensor_tensor(out=oh2_4, in0=el2_4, in1=m2_4.unsqueeze(2).broadcast_to([128, 4, 8]), op=ALU.is_ge), r=R, w=R)
                V(lambda e: e.tensor_tensor(out=dd4, in0=m2_4, in1=m1_4, op=ALU.subtract), r=R, w=R)
                A(lambda e: e.activation(out=edd4, in_=dd4, func=AF.Exp), r=R, w=R)
                V(lambda e: e.tensor_scalar(out=w1_4, in0=edd4, scalar1=1.0, scalar2=None, op0=ALU.add), r=R, w=R)
                V(lambda e: e.reciprocal(out=w1_4, in_=w1_4), r=R, w=R)
                V(lambda e: e.tensor_tensor(out=w2_4, in0=edd4, in1=w1_4, op=ALU.mult), r=R, w=R)
                cwv = cw_all[:, 8 * g:8 * g + 8].rearrange("p (j k) -> p j k", k=2)
                r_cwg = [r_cw[g * TPG + j] for j in range(TPG)]
                V(lambda e: e.tensor_tensor(out=cwv[:, :, 0], in0=w1_4, in1=gp4, op=ALU.mult), r=R, w=r_cwg)
                V(lambda e: e.tensor_tensor(out=cwv[:, :, 1], in0=w2_4, in1=gp4, op=ALU.mult), r=R + r_cwg, w=r_cwg)
                gohb = goh4.unsqueeze(3).broadcast_to([128, 4, 4, 8])
                V(lambda e: e.tensor_tensor(out=A1_4.rearrange("p j (g e) -> p j g e", g=4), in0=gohb,
                                            in1=oh1_4.unsqueeze(2).broadcast_to([128, 4, 4, 8]), op=ALU.mult), r=R, w=R)
                V(lambda e: e.tensor_tensor(out=A2_4.rearrange("p j (g e) -> p j g e", g=4), in0=gohb,
                                            in1=oh2_4.unsqueeze(2).broadcast_to([128, 4, 4, 8]), op=ALU.mult), r=R, w=R)
                V(lambda e: e.tensor_tensor(out=Ab4[:], in0=A1_4, in1=A2_4, op=ALU.add), r=R, w=[r_Ab])
                for j in range(TPG):
                    fns = [lambda e: e.matmul(pD[:, 256 + j * 32:256 + (j + 1) * 32], lhsT=tri_bf[:], rhs=Ab4[:, j, :], start=True, stop=(j == 0))]
                    for jp in range(j):
                        fns.append(lambda e, jp=jp: e.matmul(pD[:, 256 + j * 32:256 + (j + 1) * 32], lhsT=ones_bf[:], rhs=Ab4[:, jp, :],
                                                             start=False, stop=(jp == j - 1)))
                    PE(fns, r=[r_tri, r_ones, r_Ab], w=[r_pD])
                PE([(lambda e, j=j: e.matmul(pD[:, 384:416], lhsT=ones_bf[:], rhs=Ab4[:, j, :], start=(j == 0), stop=(j == TPG - 1)))
                    for j in range(TPG)], r=[r_ones, r_Ab], w=[r_pD])
                V(lambda e: e.tensor_tensor(out=posf4, in0=pD[:, 256:384].rearrange("p (j e) -> p j e", j=4),
                                            in1=cnt[:].unsqueeze(1).broadcast_to([128, 4, NE]), op=ALU.add), r=[r_pD, r_cnt], w=R)
                V(lambda e: e.tensor_tensor(out=cnt[:], in0=pD[:, 384:416], in1=cnt[:], op=ALU.add), r=[r_pD, r_cnt], w=[r_cnt])
                eoffb = eoff[:].unsqueeze(1).broadcast_to([128, 4, NE])
                for kk, Af in enumerate((A1_4, A2_4)):
                    V(lambda e: e.tensor_tensor(out=prodE, in0=Af, in1=posf4, op=ALU.mult), r=R, w=R)
                    V(lambda e: e.tensor_reduce(out=pk4[:, :, kk], in_=prodE, axis=AX.X, op=ALU.add), r=R, w=R)
                    V(lambda e: e.tensor_tensor(out=prodE, in0=Af, in1=eoffb, op=ALU.mult), r=R + [r_eoff], w=R)
                    V(lambda e: e.tensor_reduce(out=ek4[:, :, kk], in_=prodE, axis=AX.X, op=ALU.add), r=R, w=R)
                V(lambda e: e.tensor_scalar(out=ok4, in0=pk4, scalar1=float(CAP), scalar2=None, op0=ALU.is_lt), r=R, w=R)
                V(lambda e: e.tensor_tensor(out=sd4, in0=pk4, in1=ek4, op=ALU.add), r=R, w=R)
                V(lambda e: e.scalar_tensor_tensor(out=sd4, in0=sd4, scalar=-float(TRASH), in1=ok4, op0=ALU.add, op1=ALU.mult), r=R, w=R)
                V(lambda e: e.tensor_scalar(out=sd4, in0=sd4, scalar1=float(TRASH), scalar2=None, op0=ALU.add), r=R, w=R)
                r_dg = [r_dest[g * TPG + j] for j in range(TPG)]
                V(lambda e: e.tensor_copy(out=dest_all[:, 8 * g:8 * g + 8], in_=sd4.rearrange("p j k -> p (j k)")), r=R, w=r_dg)
                def do_scatter():
                    for j in range(TPG):
                        i = g * TPG + j
                        for kk in range(2):
                            tk.dma("pool", "sc%d" % j, lambda e: e.indirect_dma_start(
                                out=xs_d, out_offset=bass.IndirectOffsetOnAxis(ap=dest_all[:, 2 * i + kk:2 * i + kk + 1], axis=0),
                                in_=h3r[j], in_offset=None), r=r_h3r[j] + [r_dest[i], r_xsz], w=[Res()])
                return do_scatter

            pending = None
            for g in range(NG):
                par = g % 2
                load_x(g)
                for j in range(TPG):
                    norm_stats(j)
                rstd_op(ssq[:], 4, D, stmp[:, 0:4], rstd[:], [r_ssq], r_stmp, r_rstd)
                if pending is not None:
                    pending()
                for j in range(TPG):
                    norm_to_hT(j, ln1w)
                rnn_all = []
                for c_ in range(4):
                    rnn_all.extend(rnn_steps(g, c_))

                def pump(n):
                    for _ in range(n):
                        if rnn_all:
                            rnn_all.pop(0)()

                GP(lambda e: e.memset(mix[:, 0:8, :].rearrange("p a t -> p (a t)"), 0.0), w=r_mix[0:8])
                for c in range(4):
                    proj_fm(w_in, r_w_in, 1024 + c * 128, pA[:, (c % 2) * 512:(c % 2 + 1) * 512], r_pA[c % 2])
                    for hh_ in range(2):
                        A(lambda e: e.activation(out=qT[hh_ * 64:(hh_ + 1) * 64, 2 * c + hh_, :],
                                                 in_=pA[hh_ * 64:(hh_ + 1) * 64, (c % 2) * 512:(c % 2 + 1) * 512], func=AF.Copy, scale=0.125),
                          r=[r_pA[c % 2]], w=[r_qT[2 * c + hh_]])
                    pump(3)
                for c in range(4):
                    proj_fm(w_in, r_w_in, 1536 + c * 128, pA[:, (c % 2) * 512:(c % 2 + 1) * 512], r_pA[c % 2])
                    A(lambda e: e.activation(out=kring[:, c, par * 512:(par + 1) * 512], in_=pA[:, (c % 2) * 512:(c % 2 + 1) * 512],
                                             func=AF.Copy), r=[r_pA[c % 2]], w=[r_kring[par][c]])
                    pump(3)
                for j in range(TPG):
                    slot = (g * TPG + j) % 8
                    PE([(lambda e, kc=kc: e.matmul(pC[:, (j % 2) * 512:(j % 2 + 1) * 512], lhsT=hT[:, kc, j * 128:(j + 1) * 128],
                                                   rhs=w_in[:, kc, 2048:2560], start=(kc == 0), stop=(kc == 7))) for kc in range(8)],
                       r=r_w_in + [r_hT[j]], w=[r_pC[j % 2]])
                    V(lambda e: e.tensor_copy(out=vring[:, slot, :, 0:64],
                                              in_=pC[:, (j % 2) * 512:(j % 2 + 1) * 512].rearrange("p (h d) -> p h d", h=8)),
                      r=[r_pC[j % 2]], w=[r_vring[slot]])
                    pump(3)
                for j in range(TPG):
                    att_tile(g, j, rnn_all)
                V(lambda e: e.tensor_copy(out=stmp[:, 0:4], in_=ssqr[:]), r=[r_ssqr], w=[r_stmp])
                V(lambda e: e.tensor_copy(out=stmp[:, 4:8], in_=ssqa[:]), r=[r_ssqa], w=[r_stmp])
                V(lambda e: e.tensor_scalar(out=stmp[:], in0=stmp[:], scalar1=1.0 / 512, scalar2=EPS, op0=ALU.mult, op1=ALU.add),
                  r=[r_stmp], w=[r_stmp])
                GP(lambda e: e.tensor_tensor(out=rstdg[:], in0=stmp[:], in1=neg05[:], op=ALU.pow), r=[r_stmp, r_neg], w=[r_rstdg])
                for j in range(TPG):
                    out_proj(j)
                    norm_stats(j)
                rstd_op(ssq[:], 4, D, stmp[:, 0:4], rstd[:], [r_ssq], r_stmp, r_rstd)
                for j in range(TPG):
                    norm_to_hT(j, ln2w)
                for fo in range(8):
                    proj_fm(wq, [r_wq], fo * 128, pA[:, (fo % 2) * 512:(fo % 2 + 1) * 512], r_pA[fo % 2])
                    A(lambda e: e.activation(out=qxT[:, fo, :], in_=pA[:, (fo % 2) * 512:(fo % 2 + 1) * 512], func=AF.Copy),
                      r=[r_pA[fo % 2]], w=[r_qxT[fo]])
                cross_attn()
                for j in range(TPG):
                    wo_proj(j)
                    norm_stats(j)
                rstd_op(ssq[:], 4, D, stmp[:, 0:4], rstd[:], [r_ssq], r_stmp, r_rstd)
                pending = route_group(g)
            pending()
            tk.barrier()

        with ExitStack() as sbk:
            NB = 3
            wg = [sb("wg%d" % k, [128, 8, 512], BF16, sbk) for k in range(NB)]
            wu = [sb("wu%d" % k, [128, 8, 512], BF16, sbk) for k in range(NB)]
            wd = [sb("wd%d" % k, [128, 4, D], BF16, sbk) for k in range(NB)]
            r_wg = [Res() for _ in range(NB)]; r_wu = [Res() for _ in range(NB)]; r_wd = [Res() for _ in range(NB)]
            xsl = [sb("xsl%d" % k, [128, CAP // 128, D], BF16, sbk) for k in range(NB)]
            r_xsl = [Res() for _ in range(NB)]
            xsT = sb("xsT", [128, 8, CAP], BF16, sbk); r_xsT = Res()
            sg = [sb("sg%d" % k, [128, CAP], F32, sbk) for k in range(2)]
            r_sg = [Res(), Res()]
            aT = sb("aT", [128, 4, CAP], BF16, sbk)
            r_aT = [Res() for _ in range(4)]
            yb = [sb("yb%d" % k, [128, D], F32, sbk) for k in range(2)]
            r_yb = [Res(), Res()]

            def load_expert(e_):
                k = e_ % NB
                tk.dma("pool", "ewg%d" % k, lambda e: e.dma_start(out=wg[k][:], in_=eg_d[e_].rearrange("(k p) n -> p k n", p=128)), w=[r_wg[k]])
                tk.dma("pool", "ewu%d" % k, lambda e: e.dma_start(out=wu[k][:], in_=eu_d[e_].rearrange("(k p) n -> p k n", p=128)), w=[r_wu[k]])
                tk.dma("pool", "ewd%d" % k, lambda e: e.dma_start(out=wd[k][:], in_=ed_d[e_].rearrange("(k p) n -> p k n", p=128)), w=[r_wd[k]])
                tk.dma("sp", "xsl%d" % k, lambda e: e.dma_start(out=xsl[k][:], in_=xs_d[e_ * CAP:(e_ + 1) * CAP, :].rearrange("(t p) d -> p t d", p=128)),
                       w=[r_xsl[k]])

            xsT2 = [xsT, sb("xsT_b", [128, 8, CAP], BF16, sbk)]
            r_xsT2 = [r_xsT, Res()]

            def tr_steps(e_):
                k = e_ % NB
                xo, r_xo = xsT2[e_ % 2], r_xsT2[e_ % 2]
                st_ = []
                n = 0
                for t in range(CAP // 128):
                    for hf in range(2):
                        pq, r_pq = (pD, r_pD) if n % 2 == 0 else (pE, r_pE)
                        n += 1
                        st_.append(lambda t=t, hf=hf, pq=pq, r_pq=r_pq: mm_transpose(
                            pq, lambda b: xsl[k][:, t, (hf * 4 + b) * 128:(hf * 4 + b + 1) * 128], 4, [r_xsl[k]], [r_pq]))
                        st_.append(lambda t=t, hf=hf, pq=pq, r_pq=r_pq: V(lambda e: e.tensor_tensor(
                            out=xo[:, hf * 4:(hf + 1) * 4, t * 128:(t + 1) * 128], in0=pq.rearrange("p (k t) -> p k t", k=4),
                            in1=ln3w[:, hf * 4:(hf + 1) * 4].unsqueeze(2).broadcast_to([128, 4, 128]), op=ALU.mult),
                            r=[r_pq, r_vecs], w=[r_xo]))
                return st_

            def main_steps(e_):
                k = e_ % NB
                xo, r_xo = xsT2[e_ % 2], r_xsT2[e_ % 2]
                st_ = []
                for fc in range(4):
                    b2 = fc % 2
                    st_.append(lambda fc=fc, b2=b2: PE([(lambda e, kc=kc: e.matmul(pA[:, b2 * 512:b2 * 512 + CAP], lhsT=wg[k][:, kc, fc * 128:(fc + 1) * 128],
                                                                                 rhs=xo[:, kc, :], start=(kc == 0), stop=(kc == 7))) for kc in range(8)],
                                                       r=[r_wg[k], r_xo], w=[r_pA[b2]]))
                    st_.append(lambda fc=fc, b2=b2: PE([(lambda e, kc=kc: e.matmul(pB[:, b2 * 512:b2 * 512 + CAP], lhsT=wu[k][:, kc, fc * 128:(fc + 1) * 128],
                                                                                 rhs=xo[:, kc, :], start=(kc == 0), stop=(kc == 7))) for kc in range(8)],
                                                       r=[r_wu[k], r_xo], w=[r_pB[b2]]))
                    st_.append(lambda b2=b2: A(lambda e: e.activation(out=sg[b2][:], in_=pA[:, b2 * 512:b2 * 512 + CAP], func=AF.Silu),
                                               r=[r_pA[b2]], w=[r_sg[b2]]))
                    st_.append(lambda fc=fc, b2=b2: V(lambda e: e.tensor_tensor(out=aT[:, fc, :], in0=pB[:, b2 * 512:b2 * 512 + CAP], in1=sg[b2][:], op=ALU.mult),
                                                      r=[r_pB[b2], r_sg[b2]], w=[r_aT[fc]]))
                for t in range(CAP // 128):
                    yk = (e_ * (CAP // 128) + t) % 2
                    for half in range(2):
                        st_.append(lambda t=t, half=half: PE([(lambda e, fc=fc: e.matmul(pC[:, half * 512:(half + 1) * 512], lhsT=aT[:, fc, t * 128:(t + 1) * 128],
                                                                                         rhs=wd[k][:, fc, half * 512:(half + 1) * 512], start=(fc == 0), stop=(fc == 3)))
                                                              for fc in range(4)], r=r_aT + [r_wd[k]], w=[r_pC[half]]))
                    st_.append(lambda yk=yk: A(lambda e: e.activation(out=yb[yk][:, 0:512], in_=pC[:, 0:512], func=AF.Copy), r=[r_pC[0], r_yb[yk]], w=[r_yb[yk]]))
                    st_.append(lambda yk=yk: V(lambda e: e.tensor_copy(out=yb[yk][:, 512:1024], in_=pC[:, 512:1024]), r=[r_pC[1], r_yb[yk]], w=[r_yb[yk]]))
                    row0 = e_ * CAP + t * 128
                    st_.append(lambda yk=yk, row0=row0: tk.dma("sp", "ys%d" % yk, lambda e: e.dma_start(out=ys_d[row0:row0 + 128, :], in_=yb[yk][:]),
                                                               r=[r_yb[yk]], w=[Res()]))
                return st_

            load_expert(0)
            load_expert(1)
            for f_ in tr_steps(0):
                f_()
            for e_ in range(NE):
                if e_ + 2 < NE:
                    load_expert(e_ + 2)
                ms_ = main_steps(e_)
                ts_ = tr_steps(e_ + 1) if e_ + 1 < NE else []
                while ms_ or ts_:
                    for _ in range(2):
                        if ms_:
                            ms_.pop(0)()
                    if ts_:
                        ts_.pop(0)()
            tk.barrier()

        with ExitStack() as sc:
            NBC = 4
            x2t = [sb("x2t%d" % k, [128, D], F32, sc) for k in range(NBC)]
            y1t = [sb("y1t%d" % k, [128, D], F32, sc) for k in range(NBC)]
            y2t = [sb("y2t%d" % k, [128, D], F32, sc) for k in range(NBC)]
            r_x2t = [Res() for _ in range(NBC)]; r_y1t = [Res() for _ in range(NBC)]; r_y2t = [Res() for _ in range(NBC)]
            junkc = [sb("junkc%d" % k, [128, D], BF16, sc) for k in range(2)]; r_junkc = [Res(), Res()]
            fin_bc = sb("fin_bc", [128, D], F32, sc); r_fin = Res()
            tk.dma("sp", "fin", lambda e: e.dma_start(out=fin_bc[:], in_=fin_d.partition_broadcast(128)), w=[r_fin])
            fss = sb("fss", [128, NBC], F32, sc); r_fss = [Res() for _ in range(NBC)]
            ftmp = sb("ftmp", [128, NBC], F32, sc); r_ftmp = [Res() for _ in range(NBC)]
            frs = sb("frs", [128, NBC], F32, sc); r_frs = [Res() for _ in range(NBC)]

            def load_c(i):
                k = i % NBC
                tk.dma("sp", "cx%d" % k, lambda e: e.dma_start(out=x2t[k][:], in_=x2s_d[i * 128:(i + 1) * 128, :]), r=[r_x2s[i]], w=[r_x2t[k]])
                tk.dma("pool", "cy1%d" % k, lambda e: e.indirect_dma_start(
                    out=y1t[k][:], out_offset=None, in_=ys_d, in_offset=bass.IndirectOffsetOnAxis(ap=dest_all[:, 2 * i:2 * i + 1], axis=0)),
                    r=[r_dest[i]], w=[r_y1t[k]])
                tk.dma("pool", "cy2%d" % k, lambda e: e.indirect_dma_start(
                    out=y2t[k][:], out_offset=None, in_=ys_d, in_offset=bass.IndirectOffsetOnAxis(ap=dest_all[:, 2 * i + 1:2 * i + 2], axis=0)),
                    r=[r_dest[i]], w=[r_y2t[k]])

            def c_steps(i):
                k = i % NBC
                jk = i % 2
                st_ = []
                st_.append(lambda: V(lambda e: e.scalar_tensor_tensor(out=x2t[k][:], in0=y1t[k][:], scalar=cw_all[:, 2 * i:2 * i + 1], in1=x2t[k][:],
                                                                      op0=ALU.mult, op1=ALU.add), r=[r_y1t[k], r_cw[i], r_x2t[k]], w=[r_x2t[k]]))
                st_.append(lambda: V(lambda e: e.scalar_tensor_tensor(out=x2t[k][:], in0=y2t[k][:], scalar=cw_all[:, 2 * i + 1:2 * i + 2], in1=x2t[k][:],
                                                                      op0=ALU.mult, op1=ALU.add), r=[r_y2t[k], r_cw[i], r_x2t[k]], w=[r_x2t[k]]))
                st_.append(lambda: A(lambda e: e.activation(out=junkc[jk][:], in_=x2t[k][:], func=AF.Square, accum_out=fss[:, k:k + 1]),
                                     r=[r_x2t[k]], w=[r_junkc[jk], r_fss[k]]))
                st_.append(lambda: V(lambda e: e.tensor_scalar(out=ftmp[:, k:k + 1], in0=fss[:, k:k + 1], scalar1=1.0 / D, scalar2=EPS,
                                                               op0=ALU.mult, op1=ALU.add), r=[r_fss[k]], w=[r_ftmp[k]]))
                st_.append(lambda: GP(lambda e: e.tensor_tensor(out=frs[:, k:k + 1], in0=ftmp[:, k:k + 1], in1=neg05[:, 0:1], op=ALU.pow),
                                      r=[r_ftmp[k], r_neg], w=[r_frs[k]]))
                st_.append(lambda: A(lambda e: e.activation(out=y2t[k][:], in_=x2t[k][:], func=AF.Copy, scale=frs[:, k:k + 1]),
                                     r=[r_x2t[k], r_frs[k]], w=[r_y2t[k]]))
                st_.append(lambda: V(lambda e: e.tensor_tensor(out=y1t[k][:], in0=y2t[k][:], in1=fin_bc[:], op=ALU.mult),
                                     r=[r_y2t[k], r_fin], w=[r_y1t[k]]))
                st_.append(lambda: tk.dma("sp", "out%d" % k, lambda e: e.dma_start(out=out_d[i * 128:(i + 1) * 128, :], in_=y1t[k][:]), r=[r_y1t[k]]))
                return st_

            load_c(0)
            load_c(1)
            for i in range(0, NT, 2):
                for t_ in (i + 2, i + 3):
                    if t_ < NT:
                        load_c(t_)
                a_ = c_steps(i)
                b_ = c_steps(i + 1)
                while a_ or b_:
                    if a_:
                        a_.pop(0)()
                    if b_:
                        b_.pop(0)()
            for k in range(NBC):
                ds = tk.dsem("out%d" % k)
                nc.sync.wait_ge(ds.sem, ds.cnt)
    return nc


def _prep_shared(inp):
    f = np.float32
    vecs = np.zeros((128, NV), f)

    def pc(v, n):
        return np.ascontiguousarray(np.asarray(v, f).reshape(n, 128).T)

    vecs[:, 0:8] = pc(inp["ln1_w"][0], 8)
    vecs[:, 8:16] = pc(inp["ln2_w"][0], 8)
    vecs[:, 16:20] = pc(inp["gn_rnn_w"][0], 4)
    vecs[:, 20:24] = pc(inp["gn_att_w"][0], 4)
    vecs[:, 24:28] = pc(inp["conv_b"][0], 4)
    vecs[:, 28:32] = pc(inp["rnn_ba"][0], 4)
    vecs[:, 32:36] = pc(inp["rnn_bx"][0], 4)
    vecs[:, 36:40] = pc(inp["rnn_lambda"][0], 4)
    vecs[:, 56:64] = pc(inp["ln3_w"][0], 8)
    cwt = np.asarray(inp["conv_w"][0], f)
    for c in range(4):
        for j in range(4):
            vecs[:, 40 + c * 4 + j] = cwt[j, c * 128:(c + 1) * 128]
    bd = np.zeros((128, 2, 4, 128), f)
    for gi, name in enumerate(("rnn_wa", "rnn_wx")):
        w = np.asarray(inp[name][0], f)
        for c in range(4):
            for b in range(2):
                bd[b * 64:(b + 1) * 64, gi, c, b * 64:(b + 1) * 64] = w[2 * c + b]
    rb = np.asarray(inp["rel_bias"][0], f)
    kk = np.arange(128)[:, None]
    qq = np.arange(640)[None, :]
    idx = np.clip(qq - kk, -128, 128) + 128
    valid = np.where(kk < 64, (qq < 576), (qq >= 64))
    bm = np.empty((128, 8, 640), f)
    for h in range(8):
        bm[:, h, :] = np.where(valid, rb[h][idx], f(-30000.0))
    wr = np.concatenate([np.asarray(inp["router_group_w"][0], f),
                         np.asarray(inp["router_expert_w"][0], f).transpose(1, 0, 2).reshape(D, 32)], axis=1)
    rbias = np.concatenate([np.asarray(inp["router_group_b"][0], f), np.asarray(inp["router_expert_b"][0], f).reshape(32)])
    shared = {
        "vecs": vecs, "bm": bm, "bd": bd, "wr": np.ascontiguousarray(wr), "rb": np.ascontiguousarray(rbias),
        "w_in": np.ascontiguousarray(inp["w_in"][0], f), "w_out": np.ascontiguousarray(inp["w_out"][0], f),
        "wq": np.ascontiguousarray(inp["xq_w"][0], f), "wk": np.ascontiguousarray(inp["xk_w"][0], f),
        "wv": np.ascontiguousarray(inp["xv_w"][0], f), "wo": np.ascontiguousarray(inp["xo_w"][0], f),
        "memw": np.ascontiguousarray(inp["mem_norm_w"], f),
        "fin": np.ascontiguousarray(inp["final_norm_w"], f),
        "eg": np.ascontiguousarray(inp["expert_gate_w"][0], f), "eu": np.ascontiguousarray(inp["expert_up_w"][0], f),
        "ed": np.ascontiguousarray(inp["expert_down_w"][0], f),
    }
    return shared


def kernel(**inputs):
    inp = {k: np.asarray(v) for k, v in inputs.items()}
    shared = _prep_shared(inp)
    nc = build_nc()
    in_maps = []
    for b in range(8):
        m = dict(shared)
        m["x"] = np.ascontiguousarray(inp["x"][b], np.float32)
        m["mem"] = np.ascontiguousarray(inp["mem"][b], np.float32)
        in_maps.append(m)
    res = run_bass_kernel_spmd(nc, in_maps, core_ids=list(range(8)))
    return np.stack([np.asarray(r["out"], np.float32) for r in res.results], axis=0)
```

```python
import numpy as np
from contextlib import ExitStack

import concourse.bass as bass
import concourse.mybir as mybir
from concourse.bass_utils import run_bass_kernel_spmd

F32 = mybir.dt.float32
BF16 = mybir.dt.bfloat16
I32 = mybir.dt.int32
AF = mybir.ActivationFunctionType
ALU = mybir.AluOpType
AX = mybir.AxisListType

S = 4096
D = 1024
NT = 32
GT = 512
NG = 8
TPG = 4
NE = 32
CAP = 512
NSLOT = NE * CAP
NROWS = NSLOT + 1024
TRASH = NSLOT
EPS = 1e-6
NV = 64
GELU_K = 0.7978845608028654


class Res:
    __slots__ = ("w", "r")

    def __init__(self):
        self.w = {}
        self.r = {}


class DSem:
    def __init__(self, sem):
        self.sem = sem
        self.cnt = 0


class TK:
    def __init__(self, nc, stack):
        self.nc = nc
        self.stack = stack
        self.eng = {"pe": nc.tensor, "dve": nc.vector, "act": nc.scalar, "pool": nc.gpsimd, "sp": nc.sync}
        self.esem = {}
        self.ecnt = {}
        self.seen = {}
        for k in self.eng:
            self.esem[k] = stack.enter_context(nc.semaphore("es_" + k))
            self.ecnt[k] = 0
            self.seen[k] = {}
        self.dsems = []
        self.dsd = {}

    def dsem(self, name):
        if name not in self.dsd:
            d = DSem(self.stack.enter_context(self.nc.semaphore("m_" + name)))
            self.dsems.append(d)
            self.dsd[name] = d
        return self.dsd[name]

    @staticmethod
    def _merge(d, tokd):
        for k, (s, v) in tokd.items():
            if v is None or k not in d or d[k][1] < v:
                d[k] = (s, v)

    @staticmethod
    def _flat(rs):
        out = []
        for r in rs:
            if isinstance(r, (list, tuple)):
                out.extend(TK._flat(r))
            else:
                out.append(r)
        return out

    def _deps(self, reads, writes):
        d = {}
        for r in self._flat(reads):
            self._merge(d, r.w)
        for w in self._flat(writes):
            self._merge(d, w.w)
            self._merge(d, w.r)
        return d

    def _wait(self, e, deps):
        E = self.eng[e]
        seen = self.seen[e]
        for k, (s, v) in deps.items():
            if e == "pe" and k == "pe":
                continue
            if v is None:
                sem, val = s.sem, s.cnt
            else:
                sem, val = s, v
            if seen.get(k, 0) < val:
                E.wait_ge(sem, val)
                seen[k] = val

    @staticmethod
    def _update(reads, writes, key, tok):
        for r in TK._flat(reads):
            r.r[key] = tok
        for w in TK._flat(writes):
            w.w = {key: tok}
            w.r = {}

    def op(self, e, fn, r=(), w=()):
        self._wait(e, self._deps(r, w))
        ins = fn(self.eng[e])
        self.ecnt[e] += 1
        ins.then_inc(self.esem[e], 1)
        self._update(r, w, e, (self.esem[e], self.ecnt[e]))

    def ops(self, e, fns, r=(), w=()):
        self._wait(e, self._deps(r, w))
        ins = None
        for fn in fns:
            ins = fn(self.eng[e])
        self.ecnt[e] += 1
        ins.then_inc(self.esem[e], 1)
        self._update(r, w, e, (self.esem[e], self.ecnt[e]))

    def dma(self, q, name, fn, r=(), w=()):
        ds = self.dsem(name)
        self._wait(q, self._deps(r, w))
        ins = fn(self.eng[q])
        ds.cnt += 16
        ins.then_inc(ds.sem, 16)
        self._update(r, w, "d_" + name, (ds, None))

    def barrier(self):
        d = {}
        for k in self.eng:
            if self.ecnt[k] > 0:
                d[k] = (self.esem[k], self.ecnt[k])
        for name, ds in self.dsd.items():
            if ds.cnt > 0:
                d["d_" + name] = (ds.sem, ds.cnt)
        for e in self.eng:
            E = self.eng[e]
            seen = self.seen[e]
            for k, (s, v) in d.items():
                if k == e:
                    continue
                if seen.get(k, 0) < v:
                    E.wait_ge(s, v)
                    seen[k] = v


def build_nc():
    nc = bass.Bass("TRN2", target_bir_lowering=False)

    def din(name, shape, dt=F32):
        return nc.dram_tensor(name, shape, dt, kind="ExternalInput").ap()

    x_d = din("x", [S, D])
    mem_d = din("mem", [256, D])
    vecs_d = din("vecs", [128, NV])
    bm_d = din("bm", [128, 8, 640])
    bd_d = din("bd", [128, 2, 4, 128])
    wr_d = din("wr", [D, 36])
    rb_d = din("rb", [36])
    w_in_d = din("w_in", [D, 2560])
    w_out_d = din("w_out", [D, D])
    wq_d = din("wq", [D, D])
    wk_d = din("wk", [D, D])
    wv_d = din("wv", [D, D])
    wo_d = din("wo", [D, D])
    memw_d = din("memw", [D])
    fin_d = din("fin", [D])
    eg_d = din("eg", [NE, D, 512])
    eu_d = din("eu", [NE, D, 512])
    ed_d = din("ed", [NE, 512, D])
    out_d = nc.dram_tensor("out", [S, D], F32, kind="ExternalOutput").ap()
    x2s_d = nc.dram_tensor("x2s", [S, D], F32, kind="Internal").ap()
    xs_d = nc.dram_tensor("xs", [NROWS, D], BF16, kind="Internal").ap()
    ys_d = nc.dram_tensor("ys", [NSLOT + 1, D], F32, kind="Internal").ap()

    with ExitStack() as st:
        tk = TK(nc, st)

        def sb(name, shape, dt, stack=st):
            return stack.enter_context(nc.sbuf_tensor("s_" + name, shape, dt))

        def psum(name, shape, dt):
            return st.enter_context(nc.psum_tensor(name, shape, dt))

        def V(fn, r=(), w=()):
            tk.op("dve", fn, r, w)

        def A(fn, r=(), w=()):
            tk.op("act", fn, r, w)

        def GP(fn, r=(), w=()):
            tk.op("pool", fn, r, w)

        def PE(fns, r=(), w=()):
            tk.ops("pe", fns, r, w)

        pall = psum("pall", [128, 8 * 512], F32)
        pA = pall[:, 0:1024]
        pB = pall[:, 1024:2048]
        pC = pall[:, 2048:3072]
        pD = pall[:, 3072:3584]
        pE = pall[:, 3584:4096]
        r_pE = Res()
        r_pA = [Res(), Res()]
        r_pB = [Res(), Res()]
        r_pC = [Res(), Res()]
        r_pD = Res()
        SX = [pA[:, 0:640], pB[:, 0:640]]
        r_SX = [r_pA, r_pB]

        identf = sb("identf", [128, 128], F32)
        ident = sb("ident", [128, 128], BF16)
        ones_bf = sb("ones_bf", [128, 128], BF16)
        trif = sb("trif", [128, 128], F32)
        tri_bf = sb("tri_bf", [128, 128], BF16)
        vecs = sb("vecs", [128, NV], F32)
        hb = sb("hb", [128, 8], F32)
        hc = sb("hc", [128, 8], F32)
        spt = sb("spt", [128, 4], F32)
        rb_bc = sb("rb_bc", [128, 36], F32)
        dest_all = sb("dest_all", [128, 2 * NT], I32)
        cw_all = sb("cw_all", [128, 2 * NT], F32)
        cnt = sb("cnt", [128, NE], F32)
        eoff_i = sb("eoff_i", [128, NE], I32)
        eoff = sb("eoff", [128, NE], F32)
        neg05 = sb("neg05", [128, 8], F32)
        qtr = sb("qtr", [128, 1], F32)
        hstate = sb("hstate", [128, 4], F32)
        halo = sb("halo", [128, 4, 3], F32)
        zt = sb("zt", [128, 8], F32)
        r_ident = Res(); r_identf = Res(); r_ones = Res(); r_trif = Res(); r_tri = Res()
        r_vecs = Res(); r_hb = Res(); r_hc = Res(); r_spt = Res()
        r_rb = Res()
        r_dest = [Res() for _ in range(NT)]
        r_cw = [Res() for _ in range(NT)]
        r_cnt = Res(); r_eoffi = Res(); r_eoff = Res(); r_neg = Res()
        r_hstate = [Res() for _ in range(4)]
        r_halo = [Res() for _ in range(4)]
        r_zrow = Res()
        r_x2s = [Res() for _ in range(NT)]
        r_xs = Res()
        r_ys = Res()

        ln1w = vecs[:, 0:8]
        ln2w = vecs[:, 8:16]
        gnrw = vecs[:, 16:20]
        gnaw = vecs[:, 20:24]
        convb = vecs[:, 24:28]
        lam = vecs[:, 36:40]
        ln3w = vecs[:, 56:64]

        tk.dma("sp", "vecs", lambda e: e.dma_start(out=vecs[:], in_=vecs_d), w=[r_vecs])
        tk.dma("sp", "rb", lambda e: e.dma_start(out=rb_bc[:], in_=rb_d.partition_broadcast(128)), w=[r_rb])
        GP(lambda e: e.memset(identf[:], 0.0), w=[r_identf])
        GP(lambda e: e.affine_select(out=identf[:], in_=identf[:], pattern=[[-1, 128]], compare_op=ALU.not_equal,
                                     fill=1.0, base=0, channel_multiplier=1), r=[r_identf], w=[r_identf])
        V(lambda e: e.tensor_copy(out=ident[:], in_=identf[:]), r=[r_identf], w=[r_ident])
        GP(lambda e: e.memset(trif[:], 1.0), w=[r_trif])
        GP(lambda e: e.affine_select(out=trif[:], in_=trif[:], pattern=[[1, 128]], compare_op=ALU.is_gt,
                                     fill=0.0, base=0, channel_multiplier=-1), r=[r_trif], w=[r_trif])
        V(lambda e: e.tensor_copy(out=tri_bf[:], in_=trif[:]), r=[r_trif], w=[r_tri])
        V(lambda e: e.memset(ones_bf[:], 1.0), w=[r_ones])
        GP(lambda e: e.iota(eoff_i[:], pattern=[[CAP, NE]], base=0, channel_multiplier=0), w=[r_eoffi])
        V(lambda e: e.tensor_copy(out=eoff[:], in_=eoff_i[:]), r=[r_eoffi], w=[r_eoff])
        V(lambda e: e.memset(neg05[:], -0.5), w=[r_neg])
        V(lambda e: e.memset(qtr[:], 0.25), w=[r_neg])
        V(lambda e: e.memset(cnt[:], 0.0), w=[r_cnt])
        V(lambda e: e.memset(hstate[:], 0.0), w=r_hstate)
        V(lambda e: e.memset(halo[:], 0.0), w=r_halo)
        V(lambda e: e.memset(zt[:], 0.0), w=[r_zrow])
        tk.dma("sp", "zt", lambda e: e.dma_start(out=ys_d[TRASH].rearrange("(p f) -> p f", p=128), in_=zt[:]), r=[r_zrow], w=[Res()])
        V(lambda e: e.tensor_scalar(out=hb[:], in0=vecs[:, 28:36], scalar1=0.5, scalar2=None, op0=ALU.mult), r=[r_vecs], w=[r_hb])
        A(lambda e: e.activation(out=spt[:], in_=lam, func=AF.Exp, scale=-1.0), r=[r_vecs], w=[r_spt])
        A(lambda e: e.activation(out=spt[:], in_=spt[:], func=AF.Ln, bias=1.0), r=[r_spt], w=[r_spt])
        V(lambda e: e.tensor_scalar(out=hc[:, 0:4], in0=spt[:], scalar1=-4.0, scalar2=None, op0=ALU.mult), r=[r_spt], w=[r_hc])
        V(lambda e: e.tensor_scalar(out=hc[:, 4:8], in0=spt[:], scalar1=-8.0, scalar2=None, op0=ALU.mult), r=[r_spt], w=[r_hc])

        def rstd_op(src_ap, n, dim, tmp_ap, out_ap, r_src, r_tmp, r_out):
            V(lambda e: e.tensor_scalar(out=tmp_ap, in0=src_ap, scalar1=1.0 / dim, scalar2=EPS, op0=ALU.mult, op1=ALU.add),
              r=r_src, w=[r_tmp])
            GP(lambda e: e.tensor_tensor(out=out_ap, in0=tmp_ap, in1=neg05[:, 0:n], op=ALU.pow), r=[r_tmp, r_neg], w=[r_out])

        with ExitStack() as sa:
            w_in = sb("w_in", [128, 8, 2560], BF16, sa)
            w_out = sb("w_out", [128, 8, D], BF16, sa)
            wq = sb("wq", [128, 8, D], BF16, sa)
            wo = sb("wo", [128, 8, D], BF16, sa)
            bd = sb("bd", [128, 2, 4, 128], BF16, sa)
            wr = sb("wr", [128, 8, 36], BF16, sa)
            bm = sb("bm", [128, 8, 640], BF16, sa)
            kTm = sb("kTm", [128, 8, 256], BF16, sa)
            vm = sb("vm", [128, 2, D], BF16, sa)
            kring = sb("kring", [128, 4, 1024], BF16, sa)
            vring = sb("vring", [128, 8, 8, 65], BF16, sa)
            r_w_in = [Res(), Res()]; r_w_out = Res(); r_wq = Res(); r_wo = Res(); r_bd = Res(); r_wr = Res(); r_bm = Res()
            r_kTm = Res(); r_vm = Res()
            r_kring = [[Res() for _ in range(4)] for _ in range(2)]
            r_vring = [Res() for _ in range(8)]

            def wview(dram):
                return dram.rearrange("(k p) n -> p k n", p=128)

            def mm_transpose(dst, src_fn, nblk, r, w):
                PE([(lambda e, b=b: e.matmul(dst[:, b * 128:(b + 1) * 128], lhsT=src_fn(b), rhs=ident[:], start=True, stop=True))
                    for b in range(nblk)], r=list(r) + [r_ident], w=w)


            zt2 = sb("zt2", [128, D], BF16, sa); r_zt2 = Res(); r_xsz = Res()
            V(lambda e: e.memset(zt2[:], 0.0), w=[r_zt2])

            tk.dma("pool", "w_in0", lambda e: e.dma_start(out=w_in[:, :, 0:1280], in_=wview(w_in_d)[:, :, 0:1280]), w=[r_w_in[0]])
            tk.dma("pool", "w_in1", lambda e: e.dma_start(out=w_in[:, :, 1280:2560], in_=wview(w_in_d)[:, :, 1280:2560]), w=[r_w_in[1]])

            with ExitStack() as s0:
                wk = sb("wk", [128, 8, D], BF16, s0)
                wv = sb("wv", [128, 8, D], BF16, s0)
                memt = sb("memt", [128, 2, D], F32, s0)
                memn = sb("memn", [128, 2, D], BF16, s0)
                memT = sb("memT", [128, 8, 256], BF16, s0)
                memw_bc = sb("memw_bc", [128, D], F32, s0)
                junk0 = sb("junk0", [128, D], F32, s0)
                mss = sb("mss", [128, 2], F32, s0)
                mtmp = sb("mtmp", [128, 2], F32, s0)
                mrs = sb("mrs", [128, 2], F32, s0)
                r_wk = Res(); r_wv = Res(); r_memt = Res(); r_memn = Res(); r_memT = Res(); r_memw = Res()
                r_junk0 = Res(); r_mss = Res(); r_mtmp = Res(); r_mrs = Res()
                tk.dma("sp", "memt", lambda e: e.dma_start(out=memt[:], in_=mem_d.rearrange("(t p) d -> p t d", p=128)), w=[r_memt])
                tk.dma("sp", "memw", lambda e: e.dma_start(out=memw_bc[:], in_=memw_d.partition_broadcast(128)), w=[r_memw])
                tk.dma("pool", "wk", lambda e: e.dma_start(out=wk[:], in_=wview(wk_d)), w=[r_wk])
                tk.dma("pool", "wv", lambda e: e.dma_start(out=wv[:], in_=wview(wv_d)), w=[r_wv])
                for t in range(2):
                    A(lambda e: e.activation(out=junk0[:], in_=memt[:, t, :], func=AF.Square, accum_out=mss[:, t:t + 1]),
                      r=[r_memt], w=[r_junk0, r_mss])
                rstd_op(mss[:], 2, D, mtmp[:], mrs[:], [r_mss], r_mtmp, r_mrs)
                for t in range(2):
                    V(lambda e: e.scalar_tensor_tensor(out=memn[:, t, :], in0=memt[:, t, :], scalar=mrs[:, t:t + 1], in1=memw_bc[:],
                                                       op0=ALU.mult, op1=ALU.mult), r=[r_memt, r_mrs, r_memw], w=[r_memn])
                    mm_transpose(pC, lambda b: memn[:, t, b * 128:(b + 1) * 128], 8, [r_memn], r_pC)
                    V(lambda e: e.tensor_copy(out=memT[:, :, t * 128:(t + 1) * 128], in_=pC.rearrange("p (k t) -> p k t", k=8)),
                      r=r_pC, w=[r_memT])
                for fo in range(8):
                    bank = fo % 2
                    PE([(lambda e, kc=kc: e.matmul(pA[:, bank * 512:bank * 512 + 256], lhsT=wk[:, kc, fo * 128:(fo + 1) * 128],
                                                   rhs=memT[:, kc, :], start=(kc == 0), stop=(kc == 7))) for kc in range(8)],
                       r=[r_wk, r_memT], w=[r_pA[bank]])
                    A(lambda e: e.activation(out=kTm[:, fo, :], in_=pA[:, bank * 512:bank * 512 + 256], func=AF.Copy),
                      r=[r_pA[bank]], w=[r_kTm])
                for mt in range(2):
                    for half in range(2):
                        PE([(lambda e, kc=kc: e.matmul(pB[:, half * 512:(half + 1) * 512], lhsT=memT[:, kc, mt * 128:(mt + 1) * 128],
                                                       rhs=wv[:, kc, half * 512:(half + 1) * 512], start=(kc == 0), stop=(kc == 7)))
                            for kc in range(8)], r=[r_wv, r_memT], w=[r_pB[half]])
                        A(lambda e: e.activation(out=vm[:, mt, half * 512:(half + 1) * 512], in_=pB[:, half * 512:(half + 1) * 512],
                                                 func=AF.Copy), r=[r_pB[half]], w=[r_vm])
                tk.barrier()

            tk.dma("pool", "w_out", lambda e: e.dma_start(out=w_out[:], in_=wview(w_out_d)), w=[r_w_out])
            tk.dma("pool", "wq", lambda e: e.dma_start(out=wq[:], in_=wview(wq_d)), w=[r_wq])
            tk.dma("pool", "wo", lambda e: e.dma_start(out=wo[:], in_=wview(wo_d)), w=[r_wo])
            tk.dma("pool", "bd", lambda e: e.dma_start(out=bd[:], in_=bd_d), w=[r_bd])
            tk.dma("pool", "wr", lambda e: e.dma_start(out=wr[:], in_=wview(wr_d)), w=[r_wr])
            tk.dma("pool", "bm", lambda e: e.dma_start(out=bm[:], in_=bm_d), w=[r_bm])
            V(lambda e: e.memset(vring[:].rearrange("p a b c -> p (a b c)"), 1.0), w=r_vring)
            xs_z = xs_d.rearrange("(p r) d -> p r d", p=128)
            RZ = NROWS // 128
            ZC = 34
            for zi in range(RZ // ZC):
                tk.dma("sp", "xsz", lambda e: e.dma_start(out=xs_z[:, zi * ZC:(zi + 1) * ZC, :],
                                                          in_=zt2[:].unsqueeze(1).broadcast_to([128, ZC, D])), r=[r_zt2], w=[r_xsz])

            xt = [sb("xt%d" % j, [128, D], F32, sa) for j in range(TPG)]
            r_xt = [Res() for _ in range(TPG)]
            xn = [sb("xn0", [128, D], BF16, sa)] * 2
            r_xn = [Res()] * 2
            junk = xn[0]; r_junk = r_xn[0]
            hT = sb("hT", [128, 8, GT], BF16, sa)
            r_hT = [Res() for _ in range(TPG)]
            mix = sb("mix", [128, 12, GT], BF16, sa)
            r_mix = [Res() for _ in range(12)]
            qT = mix[:, 0:8, :]
            r_qT = r_mix[0:8]
            yrT = mix[:, 8:12, :]
            r_yrT = r_mix[8:12]
            yaT = sb("yaT", [128, 4, GT], BF16, sa)
            r_yaT = [Res() for _ in range(TPG)]
            qxT = mix[:, 0:8, :]
            r_qxT = r_mix[0:8]
            ssq = sb("ssq", [128, 4], F32, sa); r_ssq = Res()
            stmp = sb("stmp", [128, 8], F32, sa); r_stmp = Res()
            rstd = sb("rstd", [128, 4], F32, sa); r_rstd = Res()
            ssqr = sb("ssqr", [128, 4], F32, sa); r_ssqr = Res()
            ssqa = sb("ssqa", [128, 4], F32, sa); r_ssqa = Res()
            rstdg = sb("rstdg", [128, 8], F32, sa); r_rstdg = Res()
            xrp = sb("xrp", [128, GT + 3], F32, sa); r_xrp = Res()
            xc = sb("xc", [128, GT], F32, sa); r_xc = Res()
            xcb = sb("xcb", [128, GT], BF16, sa); r_xcb = Res()
            tr_ = sb("tr_", [128, GT], F32, sa); r_tr = Res()
            ti_ = sb("ti_", [128, GT], F32, sa); r_ti = Res()
            av = sb("av", [128, GT], F32, sa); r_av = Res()
            vv = sb("vv", [128, GT], F32, sa); r_vv = Res()
            hs = sb("hs", [128, GT], F32, sa); r_hs = Res()
            xgs = sb("xgs", [128, GT], F32, sa); r_xgs = Res()
            g1 = sb("g1", [128, GT], F32, sa); r_g1 = Res()
            g2 = sb("g2", [128, GT], F32, sa); r_g2 = Res()
            a2 = hs; r_a2 = r_hs
            gl = av; r_gl = r_av
            yraw = vv; r_yraw = r_vv
            ysq = sb("ysq", [128, GT], BF16, sa); r_ysq = Res()
            Eb = [sb("Eb%d" % k, [128, 640], BF16, sa) for k in range(2)]
            r_Eb = [Res(), Res()]
            rden8 = sb("rden8", [128, 8], F32, sa); r_rden8 = Res()
            ex = [sb("ex0", [128, 2, GT], BF16, sa)] * 2
            r_ex = [Res()] * 2
            rdn = tr_; r_rdn = r_tr
            h3 = [sb("h3_%d" % k, [128, D], BF16, sa) for k in range(2)]
            r_h3 = [Res(), Res()]
            h3T = sb("h3T", [128, 8, 128], BF16, sa); r_h3T = Res()
            yat = h3[0][:].bitcast(F32); r_yat = r_h3[0]
            yab = Eb[0][:, 0:512]; r_yab = r_Eb[0]
            rt = sb("rt", [128, 772], F32, sa)
            r_rt = Res()
            Ab4 = xcb[:, 0:4 * NE].rearrange("p (j e) -> p j e", j=4); r_Ab = r_xcb

            def rtv(a, b, **kw):
                v = rt[:, a:b]
                return v.rearrange(kw.pop("pat"), **kw) if kw else v

            lg4 = rtv(0, 144, pat="p (j c) -> p j c", j=4)
            gmax4 = rt[:, 144:148]
            gsum4 = rt[:, 148:152]
            gp4 = rt[:, 152:156]
            m1_4 = rt[:, 156:160]
            m2_4 = rt[:, 160:164]
            dd4 = rt[:, 164:168]
            edd4 = rt[:, 168:172]
            w1_4 = rt[:, 172:176]
            w2_4 = rt[:, 176:180]
            gsh4 = rtv(180, 196, pat="p (j c) -> p j c", j=4)
            gex4 = rtv(196, 212, pat="p (j c) -> p j c", j=4)
            goh4 = rtv(212, 228, pat="p (j c) -> p j c", j=4)
            el4 = rtv(228, 260, pat="p (j c) -> p j c", j=4)
            oh1_4 = rtv(260, 292, pat="p (j c) -> p j c", j=4)
            el2_4 = rtv(292, 324, pat="p (j c) -> p j c", j=4)
            oh2_4 = rtv(324, 356, pat="p (j c) -> p j c", j=4)
            pk4 = rtv(356, 364, pat="p (j k) -> p j k", j=4)
            ek4 = rtv(364, 372, pat="p (j k) -> p j k", j=4)
            ok4 = rtv(372, 380, pat="p (j k) -> p j k", j=4)
            sd4 = rtv(380, 388, pat="p (j k) -> p j k", j=4)
            prod4 = rtv(388, 516, pat="p (j g e) -> p j g e", j=4, g=4)
            A1_4 = rtv(516, 644, pat="p (j c) -> p j c", j=4)
            A2_4 = rtv(644, 772, pat="p (j c) -> p j c", j=4)
            posf4 = rtv(0, 128, pat="p (j c) -> p j c", j=4)
            prodE = rtv(388, 516, pat="p (j c) -> p j c", j=4)

            def load_x(g):
                for j in range(TPG):
                    i = g * TPG + j
                    tk.dma("sp", "x%d" % j, lambda e: e.dma_start(out=xt[j][:], in_=x_d[i * 128:(i + 1) * 128, :]), w=[r_xt[j]])

            def norm_stats(j):
                A(lambda e: e.activation(out=junk[:], in_=xt[j][:], func=AF.Square, accum_out=ssq[:, j:j + 1]),
                  r=[r_xt[j]], w=[r_junk, r_ssq])

            def norm_to_hT(j, lnw):
                k = j % 2
                A(lambda e: e.activation(out=xn[k][:], in_=xt[j][:], func=AF.Copy, scale=rstd[:, j:j + 1]),
                  r=[r_xt[j], r_rstd], w=[r_xn[k]])
                pq, r_pq = (pB, r_pB) if j % 2 == 0 else (pC, r_pC)
                mm_transpose(pq, lambda b: xn[k][:, b * 128:(b + 1) * 128], 8, [r_xn[k]], r_pq)
                V(lambda e: e.tensor_tensor(out=hT[:, :, j * 128:(j + 1) * 128], in0=pq.rearrange("p (k t) -> p k t", k=8),
                                            in1=lnw.unsqueeze(2).broadcast_to([128, 8, 128]), op=ALU.mult),
                  r=r_pq + [r_vecs], w=[r_hT[j]])

            def proj_fm(wt, r_w, col0, bank_ap, r_bank):
                PE([(lambda e, kc=kc: e.matmul(bank_ap, lhsT=wt[:, kc, col0:col0 + 128], rhs=hT[:, kc, :],
                                               start=(kc == 0), stop=(kc == 7))) for kc in range(8)],
                   r=list(r_w) + r_hT, w=[r_bank])

            def rnn_steps(g, c):
                C0 = pD
                rC0 = r_pD
                cw0 = 40 + c * 4
                M_ = []
                G_ = []
                T_ = []
                G_.append(lambda: proj_fm(w_in, r_w_in, 512 + c * 128, C0, rC0))
                G_.append(lambda: V(lambda e: e.tensor_copy(out=xgs[:], in_=C0), r=[rC0], w=[r_xgs]))
                G_.append(lambda: A(lambda e: e.activation(out=g1[:], in_=xgs[:], func=AF.Square), r=[r_xgs], w=[r_g1]))
                G_.append(lambda: GP(lambda e: e.tensor_scalar(out=g1[:], in0=g1[:], scalar1=0.044715, scalar2=1.0, op0=ALU.mult, op1=ALU.add),
                                     r=[r_g1], w=[r_g1]))
                G_.append(lambda: GP(lambda e: e.tensor_tensor(out=g1[:], in0=xgs[:], in1=g1[:], op=ALU.mult), r=[r_xgs, r_g1], w=[r_g1]))
                G_.append(lambda: A(lambda e: e.activation(out=g2[:], in_=g1[:], func=AF.Tanh, scale=GELU_K), r=[r_g1], w=[r_g2]))
                G_.append(lambda: V(lambda e: e.scalar_tensor_tensor(out=g2[:], in0=g2[:], scalar=1.0, in1=xgs[:], op0=ALU.add, op1=ALU.mult),
                                    r=[r_g2, r_xgs], w=[r_g2]))
                M_.append(lambda: proj_fm(w_in, r_w_in, c * 128, C0, rC0))
                M_.append(lambda: V(lambda e: e.tensor_copy(out=xrp[:, 3:GT + 3], in_=C0), r=[rC0], w=[r_xrp]))
                M_.append(lambda: V(lambda e: e.tensor_copy(out=xrp[:, 0:3], in_=halo[:, c, :]), r=[r_halo[c]], w=[r_xrp]))
                M_.append(lambda: V(lambda e: e.tensor_scalar(out=xc[:], in0=xrp[:, 3:GT + 3], scalar1=vecs[:, cw0 + 3:cw0 + 4],
                                                               scalar2=convb[:, c:c + 1], op0=ALU.mult, op1=ALU.add),
                                    r=[r_xrp, r_vecs], w=[r_xc]))
                for jj in range(3):
                    M_.append(lambda jj=jj: V(lambda e: e.scalar_tensor_tensor(out=xc[:], in0=xrp[:, jj:jj + GT],
                                                                               scalar=vecs[:, cw0 + jj:cw0 + jj + 1], in1=xc[:],
                                                                               op0=ALU.mult, op1=ALU.add),
                                              r=[r_xrp, r_vecs, r_xc], w=[r_xc]))
                M_.append(lambda: V(lambda e: e.tensor_copy(out=halo[:, c, :], in_=xrp[:, GT:GT + 3]), r=[r_xrp], w=[r_halo[c]]))
                M_.append(lambda: GP(lambda e: e.tensor_copy(out=xcb[:], in_=xc[:]), r=[r_xc], w=[r_xcb]))
                M_.append(lambda: PE([lambda e: e.matmul(C0, lhsT=bd[:, 0, c, :], rhs=xcb[:], start=True, stop=True)],
                                     r=[r_bd, r_xcb], w=[rC0]))
                M_.append(lambda: A(lambda e: e.activation(out=tr_[:], in_=C0, func=AF.Tanh, scale=0.5, bias=hb[:, c:c + 1]),
                                    r=[rC0, r_hb], w=[r_tr]))
                M_.append(lambda: PE([lambda e: e.matmul(C0, lhsT=bd[:, 1, c, :], rhs=xcb[:], start=True, stop=True)],
                                     r=[r_bd, r_xcb], w=[rC0]))
                M_.append(lambda: A(lambda e: e.activation(out=ti_[:], in_=C0, func=AF.Tanh, scale=0.5, bias=hb[:, 4 + c:5 + c]),
                                    r=[rC0, r_hb], w=[r_ti]))
                M_.append(lambda: A(lambda e: e.activation(out=av[:], in_=tr_[:], func=AF.Exp, scale=hc[:, c:c + 1], bias=hc[:, c:c + 1]),
                                    r=[r_tr, r_hc], w=[r_av]))
                M_.append(lambda: A(lambda e: e.activation(out=a2[:], in_=tr_[:], func=AF.Exp, scale=hc[:, 4 + c:5 + c],
                                                           bias=hc[:, 4 + c:5 + c]), r=[r_tr, r_hc], w=[r_a2]))
                M_.append(lambda: V(lambda e: e.scalar_tensor_tensor(out=vv[:], in0=ti_[:], scalar=1.0, in1=xc[:], op0=ALU.add, op1=ALU.mult),
                                    r=[r_ti, r_xc], w=[r_vv]))
                M_.append(lambda: A(lambda e: e.activation(out=a2[:], in_=a2[:], func=AF.Sqrt, scale=-0.25, bias=qtr[:, 0:1]),
                                    r=[r_a2, r_neg], w=[r_a2]))
                M_.append(lambda: V(lambda e: e.tensor_tensor(out=vv[:], in0=vv[:], in1=a2[:], op=ALU.mult), r=[r_vv, r_a2], w=[r_vv]))
                M_.append(lambda: V(lambda e: e.tensor_tensor_scan(out=hs[:], data0=av[:], data1=vv[:], initial=hstate[:, c:c + 1],
                                                                    op0=ALU.mult, op1=ALU.add), r=[r_av, r_vv, r_hstate[c]], w=[r_hs]))
                M_.append(lambda: V(lambda e: e.tensor_copy(out=hstate[:, c:c + 1], in_=hs[:, GT - 1:GT]), r=[r_hs], w=[r_hstate[c]]))
                T_.append(lambda: V(lambda e: e.scalar_tensor_tensor(out=yraw[:], in0=g2[:], scalar=0.5, in1=hs[:], op0=ALU.mult, op1=ALU.mult),
                                    r=[r_g2, r_hs], w=[r_yraw]))
                T_.append(lambda: V(lambda e: e.tensor_scalar(out=yrT[:, c, :], in0=yraw[:], scalar1=gnrw[:, c:c + 1], scalar2=None, op0=ALU.mult),
                                    r=[r_yraw, r_vecs], w=[r_yrT[c]]))
                T_.append(lambda: GP(lambda e: e.tensor_tensor(out=ysq[:], in0=yraw[:], in1=yraw[:], op=ALU.mult), r=[r_yraw], w=[r_ysq]))
                T_.append(lambda: PE([(lambda e, j=j: e.matmul(C0[:, j:j + 1], lhsT=ysq[:, j * 128:(j + 1) * 128], rhs=ones_bf[:, 0:1],
                                                               start=True, stop=True)) for j in range(TPG)], r=[r_ysq, r_ones], w=[rC0]))
                if c == 0:
                    T_.append(lambda: V(lambda e: e.tensor_copy(out=ssqr[:], in_=C0[:, 0:4]), r=[rC0], w=[r_ssqr]))
                else:
                    T_.append(lambda: V(lambda e: e.tensor_tensor(out=ssqr[:], in0=C0[:, 0:4], in1=ssqr[:], op=ALU.add),
                                        r=[rC0, r_ssqr], w=[r_ssqr]))
                out_ = []
                gi = 0
                for mi, m_ in enumerate(M_):
                    out_.append(m_)
                    if mi % 3 == 1 and gi < len(G_):
                        out_.append(G_[gi]); gi += 1
                out_.extend(G_[gi:])
                out_.extend(T_)
                return out_

            def att_tile(g, j, steps):
                i = g * TPG + j
                ms = [m for m in range(5) if i - m >= 0]
                nb = len(ms)
                pBv = pC.rearrange("p (b x) -> p b x", b=2)[:, :, 0:260].rearrange("p b (h d) -> p b h d", d=65)
                kstep = 3

                def emit_S(h):
                    ch = h // 2
                    bi = h % 2
                    fns = []
                    for m in ms:
                        slot = (i - m) % 8
                        fns.append(lambda e, m=m: e.matmul(SX[bi][:, m * 128:(m + 1) * 128], lhsT=ident[:],
                                                           rhs=bm[:, h, m * 128:(m + 1) * 128], start=True, stop=False))
                        fns.append(lambda e, m=m, slot=slot: e.matmul(
                            SX[bi][:, m * 128:(m + 1) * 128], lhsT=kring[:, ch, slot * 128:(slot + 1) * 128],
                            rhs=qT[:, h, j * 128:(j + 1) * 128], start=False, stop=True))
                    PE(fns, r=[r_kring[0][ch], r_kring[1][ch], r_qT[h], r_bm, r_ident], w=[r_SX[bi]])

                emit_S(0)
                for h in range(8):
                    bi = h % 2
                    if h + 1 < 8:
                        emit_S(h + 1)
                    A(lambda e: e.activation(out=Eb[bi][:, 0:nb * 128], in_=SX[bi][:, 0:nb * 128], func=AF.Exp),
                      r=[r_SX[bi]], w=[r_Eb[bi]])
                    fns = []
                    for idx, m in enumerate(ms):
                        slot = (i - m) % 8
                        fns.append(lambda e, m=m, slot=slot, idx=idx: e.matmul(
                            pC[:, (h // 4) * 512 + (h % 4) * 65:(h // 4) * 512 + (h % 4) * 65 + 65],
                            lhsT=Eb[bi][:, m * 128:(m + 1) * 128], rhs=vring[:, slot, h, :], start=(idx == 0), stop=(idx == nb - 1)))
                    PE(fns, r=[r_Eb[bi]] + r_vring, w=[r_pC[h // 4]])
                    for _ in range(kstep):
                        if steps:
                            steps.pop(0)()
                if j == TPG - 1:
                    while steps:
                        steps.pop(0)()
                V(lambda e: e.reciprocal(out=rden8[:].rearrange("p (b h) -> p b h", b=2), in_=pBv[:, :, :, 64]),
                  r=r_pC, w=[r_rden8])
                V(lambda e: e.tensor_tensor(out=yat[:].rearrange("p (b h d) -> p b h d", b=2, h=4), in0=pBv[:, :, :, 0:64],
                                            in1=rden8[:].rearrange("p (b h) -> p b h", b=2).unsqueeze(3).broadcast_to([128, 2, 4, 64]),
                                            op=ALU.mult), r=r_pC + [r_rden8], w=[r_yat])
                A(lambda e: e.activation(out=junk[:, 0:512], in_=yat[:], func=AF.Square, accum_out=ssqa[:, j:j + 1]),
                  r=[r_yat], w=[r_junk, r_ssqa])
                A(lambda e: e.activation(out=yab[:], in_=yat[:], func=AF.Copy), r=[r_yat], w=[r_yab])
                mm_transpose(pE, lambda b: yab[:, b * 128:(b + 1) * 128], 4, [r_yab], [r_pE])
                V(lambda e: e.tensor_tensor(out=yaT[:, :, j * 128:(j + 1) * 128], in0=pE.rearrange("p (k t) -> p k t", k=4),
                                            in1=gnaw.unsqueeze(2).broadcast_to([128, 4, 128]), op=ALU.mult),
                  r=[r_pE, r_vecs], w=[r_yaT[j]])

            def out_proj(j):
                for half in range(2):
                    PE([(lambda e, c=c: e.matmul(pA[:, half * 512:(half + 1) * 512], lhsT=yrT[:, c, j * 128:(j + 1) * 128],
                                                 rhs=w_out[:, c, half * 512:(half + 1) * 512], start=(c == 0), stop=(c == 3)))
                        for c in range(4)], r=r_yrT + [r_w_out], w=[r_pA[half]])
                for half in range(2):
                    PE([(lambda e, c=c: e.matmul(pB[:, half * 512:(half + 1) * 512], lhsT=yaT[:, c, j * 128:(j + 1) * 128],
                                                 rhs=w_out[:, 4 + c, half * 512:(half + 1) * 512], start=(c == 0), stop=(c == 3)))
                        for c in range(4)], r=[r_yaT[j], r_w_out], w=[r_pB[half]])
                V(lambda e: e.scalar_tensor_tensor(out=xt[j][:], in0=pA[:], scalar=rstdg[:, j:j + 1], in1=xt[j][:],
                                                   op0=ALU.mult, op1=ALU.add), r=r_pA + [r_rstdg, r_xt[j]], w=[r_xt[j]])
                V(lambda e: e.scalar_tensor_tensor(out=xt[j][:], in0=pB[:], scalar=rstdg[:, 4 + j:5 + j], in1=xt[j][:],
                                                   op0=ALU.mult, op1=ALU.add), r=r_pB + [r_rstdg, r_xt[j]], w=[r_xt[j]])

            def cross_attn():
                for hh in range(4):
                    k = hh % 2
                    for mt in range(2):
                        PE([(lambda e, dc=dc: e.matmul(pB[:, mt * 512:(mt + 1) * 512], lhsT=kTm[:, 2 * hh + dc, mt * 128:(mt + 1) * 128],
                                                       rhs=qxT[:, 2 * hh + dc, :], start=(dc == 0), stop=(dc == 1))) for dc in range(2)],
                           r=[r_kTm, r_qxT[2 * hh], r_qxT[2 * hh + 1]], w=[r_pB[mt]])
                        A(lambda e: e.activation(out=ex[k][:, mt, :], in_=pB[:, mt * 512:(mt + 1) * 512], func=AF.Exp, scale=0.0625),
                          r=[r_pB[mt]], w=[r_ex[k]])
                    PE([(lambda e, mt=mt: e.matmul(pD[:], lhsT=ones_bf[:], rhs=ex[k][:, mt, :], start=(mt == 0), stop=(mt == 1)))
                        for mt in range(2)], r=[r_ones, r_ex[k]], w=[r_pD])
                    V(lambda e: e.reciprocal(out=rdn[:], in_=pD[:]), r=[r_pD], w=[r_rdn])
                    for dc in range(2):
                        PE([(lambda e, mt=mt: e.matmul(pC[:, dc * 512:(dc + 1) * 512],
                                                       lhsT=vm[:, mt, (2 * hh + dc) * 128:(2 * hh + dc + 1) * 128],
                                                       rhs=ex[k][:, mt, :], start=(mt == 0), stop=(mt == 1))) for mt in range(2)],
                           r=[r_vm, r_ex[k]], w=[r_pC[dc]])
                        V(lambda e: e.tensor_tensor(out=hT[:, 2 * hh + dc, :], in0=pC[:, dc * 512:(dc + 1) * 512], in1=rdn[:], op=ALU.mult),
                          r=[r_pC[dc], r_rdn], w=r_hT)

            def wo_proj(j):
                pw, r_pw = (pA, r_pA) if j % 2 == 0 else (pB, r_pB)
                for half in range(2):
                    PE([(lambda e, kc=kc: e.matmul(pw[:, half * 512:(half + 1) * 512], lhsT=hT[:, kc, j * 128:(j + 1) * 128],
                                                   rhs=wo[:, kc, half * 512:(half + 1) * 512], start=(kc == 0), stop=(kc == 7)))
                        for kc in range(8)], r=r_hT + [r_wo], w=[r_pw[half]])
                V(lambda e: e.tensor_tensor(out=xt[j][:], in0=pw, in1=xt[j][:], op=ALU.add), r=r_pw + [r_xt[j]], w=[r_xt[j]])

            def route_group(g):
                R = [r_rt]
                h3r = [mix[:, 2 * j:2 * j + 2, :].rearrange("p a t -> p (a t)") for j in range(TPG)]
                r_h3r = [[r_mix[2 * j], r_mix[2 * j + 1]] for j in range(TPG)]
                for j in range(TPG):
                    i = g * TPG + j
                    A(lambda e: e.activation(out=h3r[j], in_=xt[j][:], func=AF.Copy, scale=rstd[:, j:j + 1]),
                      r=[r_xt[j], r_rstd], w=r_h3r[j])
                    tk.dma("sp", "x2st%d" % j, lambda e: e.dma_start(out=x2s_d[i * 128:(i + 1) * 128, :], in_=xt[j][:]), r=[r_xt[j]], w=[r_x2s[i]])
                    pq, r_pq = (pB, r_pB) if j % 2 == 0 else (pC, r_pC)
                    mm_transpose(pq, lambda b: h3r[j][:, b * 128:(b + 1) * 128], 8, r_h3r[j], r_pq)
                    V(lambda e: e.tensor_tensor(out=h3T[:], in0=pq.rearrange("p (k t) -> p k t", k=8),
                                                in1=ln3w.unsqueeze(2).broadcast_to([128, 8, 128]), op=ALU.mult), r=r_pq + [r_vecs], w=[r_h3T])
                    PE([(lambda e, kc=kc: e.matmul(pD[:, j * 64:j * 64 + 36], lhsT=h3T[:, kc, :], rhs=wr[:, kc, :], start=(kc == 0), stop=(kc == 7)))
                        for kc in range(8)], r=[r_h3T, r_wr], w=[r_pD])
                pDl = pD[:, 0:256].rearrange("p (j c) -> p j c", j=4)[:, :, 0:36]
                V(lambda e: e.tensor_tensor(out=lg4, in0=pDl, in1=rb_bc[:].unsqueeze(1).broadcast_to([128, 4, 36]), op=ALU.add),
                  r=[r_pD, r_rb], w=R)
                lgG = lg4[:, :, 0:4]
                lgE = lg4[:, :, 4:36].rearrange("p j (g e) -> p j g e", g=4)
                V(lambda e: e.tensor_reduce(out=gmax4, in_=lgG, axis=AX.X, op=ALU.max), r=R, w=R)
                V(lambda e: e.tensor_tensor(out=gsh4, in0=lgG, in1=gmax4.unsqueeze(2).broadcast_to([128, 4, 4]), op=ALU.subtract), r=R, w=R)
                A(lambda e: e.activation(out=gex4, in_=gsh4, func=AF.Exp), r=R, w=R)
                V(lambda e: e.tensor_reduce(out=gsum4, in_=gex4, axis=AX.X, op=ALU.add), r=R, w=R)
                V(lambda e: e.reciprocal(out=gp4, in_=gsum4), r=R, w=R)
                V(lambda e: e.tensor_tensor(out=goh4, in0=lgG, in1=gmax4.unsqueeze(2).broadcast_to([128, 4, 4]), op=ALU.is_ge), r=R, w=R)
                V(lambda e: e.tensor_tensor(out=prod4, in0=lgE, in1=goh4.unsqueeze(3).broadcast_to([128, 4, 4, 8]), op=ALU.mult), r=R, w=R)
                V(lambda e: e.tensor_reduce(out=el4, in_=prod4.rearrange("p j g e -> p j e g"), axis=AX.X, op=ALU.add), r=R, w=R)
                V(lambda e: e.tensor_reduce(out=m1_4, in_=el4, axis=AX.X, op=ALU.max), r=R, w=R)
                V(lambda e: e.tensor_tensor(out=oh1_4, in0=el4, in1=m1_4.unsqueeze(2).broadcast_to([128, 4, 8]), op=ALU.is_ge), r=R, w=R)
                V(lambda e: e.scalar_tensor_tensor(out=el2_4, in0=oh1_4, scalar=-1e30, in1=el4, op0=ALU.mult, op1=ALU.add), r=R, w=R)
                V(lambda e: e.tensor_reduce(out=m2_4, in_=el2_4, axis=AX.X, op=ALU.max), r=R, w=R)
                V(lambda e: e.tensor_tensor(out=oh2_4, in0=el2_4, in1=m2_4.unsqueeze(2).broadcast_to([128, 4, 8]), op=ALU.is_ge), r=R, w=R)
                V(lambda e: e.tensor_tensor(out=dd4, in0=m2_4, in1=m1_4, op=ALU.subtract), r=R, w=R)
                A(lambda e: e.activation(out=edd4, in_=dd4, func=AF.Exp), r=R, w=R)
                V(lambda e: e.tensor_scalar(out=w1_4, in0=edd4, scalar1=1.0, scalar2=None, op0=ALU.add), r=R, w=R)
                V(lambda e: e.reciprocal(out=w1_4, in_=w1_4), r=R, w=R)
                V(lambda e: e.tensor_tensor(out=w2_4, in0=edd4, in1=w1_4, op=ALU.mult), r=R, w=R)
                cwv = cw_all[:, 8 * g:8 * g + 8].rearrange("p (j k) -> p j k", k=2)
                r_cwg = [r_cw[g * TPG + j] for j in range(TPG)]
                V(lambda e: e.tensor_tensor(out=cwv[:, :, 0], in0=w1_4, in1=gp4, op=ALU.mult), r=R, w=r_cwg)
                V(lambda e: e.tensor_tensor(out=cwv[:, :, 1], in0=w2_4, in1=gp4, op=ALU.mult), r=R + r_cwg, w=r_cwg)
                gohb = goh4.unsqueeze(3).broadcast_to([128, 4, 4, 8])
                V(lambda e: e.tensor_tensor(out=A1_4.rearrange("p j (g e) -> p j g e", g=4), in0=gohb,
                                            in1=oh1_4.unsqueeze(2).broadcast_to([128, 4, 4, 8]), op=ALU.mult), r=R, w=R)
                V(lambda e: e.tensor_tensor(out=A2_4.rearrange("p j (g e) -> p j g e", g=4), in0=gohb,
                                            in1=oh2_4.unsqueeze(2).broadcast_to([128, 4, 4, 8]), op=ALU.mult), r=R, w=R)
                V(lambda e: e.tensor_tensor(out=Ab4[:], in0=A1_4, in1=A2_4, op=ALU.add), r=R, w=[r_Ab])
                for j in range(TPG):
                    fns = [lambda e: e.matmul(pD[:, 256 + j * 32:256 + (j + 1) * 32], lhsT=tri_bf[:], rhs=Ab4[:, j, :], start=True, stop=(j == 0))]
                    for jp in range(j):
                        fns.append(lambda e, jp=jp: e.matmul(pD[:, 256 + j * 32:256 + (j + 1) * 32], lhsT=ones_bf[:], rhs=Ab4[:, jp, :],
                                                             start=False, stop=(jp == j - 1)))
                    PE(fns, r=[r_tri, r_ones, r_Ab], w=[r_pD])
                PE([(lambda e, j=j: e.matmul(pD[:, 384:416], lhsT=ones_bf[:], rhs=Ab4[:, j, :], start=(j == 0), stop=(j == TPG - 1)))
                    for j in range(TPG)], r=[r_ones, r_Ab], w=[r_pD])
                V(lambda e: e.tensor_tensor(out=posf4, in0=pD[:, 256:384].rearrange("p (j e) -> p j e", j=4),
                                            in1=cnt[:].unsqueeze(1).broadcast_to([128, 4, NE]), op=ALU.add), r=[r_pD, r_cnt], w=R)
                V(lambda e: e.tensor_tensor(out=cnt[:], in0=pD[:, 384:416], in1=cnt[:], op=ALU.add), r=[r_pD, r_cnt], w=[r_cnt])
                eoffb = eoff[:].unsqueeze(1).broadcast_to([128, 4, NE])
                for kk, Af in enumerate((A1_4, A2_4)):
                    V(lambda e: e.tensor_tensor(out=prodE, in0=Af, in1=posf4, op=ALU.mult), r=R, w=R)
                    V(lambda e: e.tensor_reduce(out=pk4[:, :, kk], in_=prodE, axis=AX.X, op=ALU.add), r=R, w=R)
                    V(lambda e: e.tensor_tensor(out=prodE, in0=Af, in1=eoffb, op=ALU.mult), r=R + [r_eoff], w=R)
                    V(lambda e: e.tensor_reduce(out=ek4[:, :, kk], in_=prodE, axis=AX.X, op=ALU.add), r=R, w=R)
                V(lambda e: e.tensor_scalar(out=ok4, in0=pk4, scalar1=float(CAP), scalar2=None, op0=ALU.is_lt), r=R, w=R)
                V(lambda e: e.tensor_tensor(out=sd4, in0=pk4, in1=ek4, op=ALU.add), r=R, w=R)
                V(lambda e: e.scalar_tensor_tensor(out=sd4, in0=sd4, scalar=-float(TRASH), in1=ok4, op0=ALU.add, op1=ALU.mult), r=R, w=R)
                V(lambda e: e.tensor_scalar(out=sd4, in0=sd4, scalar1=float(TRASH), scalar2=None, op0=ALU.add), r=R, w=R)
                r_dg = [r_dest[g * TPG + j] for j in range(TPG)]
                V(lambda e: e.tensor_copy(out=dest_all[:, 8 * g:8 * g + 8], in_=sd4.rearrange("p j k -> p (j k)")), r=R, w=r_dg)
                def do_scatter():
                    for j in range(TPG):
                        i = g * TPG + j
                        for kk in range(2):
                            tk.dma("pool", "sc%d" % j, lambda e: e.indirect_dma_start(
                                out=xs_d, out_offset=bass.IndirectOffsetOnAxis(ap=dest_all[:, 2 * i + kk:2 * i + kk + 1], axis=0),
                                in_=h3r[j], in_offset=None), r=r_h3r[j] + [r_dest[i], r_xsz], w=[Res()])
                return do_scatter

            pending = None
            for g in range(NG):
                par = g % 2
                load_x(g)
                for j in range(TPG):
                    norm_stats(j)
                rstd_op(ssq[:], 4, D, stmp[:, 0:4], rstd[:], [r_ssq], r_stmp, r_rstd)
                if pending is not None:
                    pending()
                for j in range(TPG):
                    norm_to_hT(j, ln1w)
                rnn_all = []
                for c_ in range(4):
                    rnn_all.extend(rnn_steps(g, c_))

                def pump(n):
                    for _ in range(n):
                        if rnn_all:
                            rnn_all.pop(0)()

                GP(lambda e: e.memset(mix[:, 0:8, :].rearrange("p a t -> p (a t)"), 0.0), w=r_mix[0:8])
                for c in range(4):
                    proj_fm(w_in, r_w_in, 1024 + c * 128, pA[:, (c % 2) * 512:(c % 2 + 1) * 512], r_pA[c % 2])
                    for hh_ in range(2):
                        A(lambda e: e.activation(out=qT[hh_ * 64:(hh_ + 1) * 64, 2 * c + hh_, :],
                                                 in_=pA[hh_ * 64:(hh_ + 1) * 64, (c % 2) * 512:(c % 2 + 1) * 512], func=AF.Copy, scale=0.125),
                          r=[r_pA[c % 2]], w=[r_qT[2 * c + hh_]])
                    pump(3)
                for c in range(4):
                    proj_fm(w_in, r_w_in, 1536 + c * 128, pA[:, (c % 2) * 512:(c % 2 + 1) * 512], r_pA[c % 2])
                    A(lambda e: e.activation(out=kring[:, c, par * 512:(par + 1) * 512], in_=pA[:, (c % 2) * 512:(c % 2 + 1) * 512],
                                             func=AF.Copy), r=[r_pA[c % 2]], w=[r_kring[par][c]])
                    pump(3)
                for j in range(TPG):
                    slot = (g * TPG + j) % 8
                    PE([(lambda e, kc=kc: e.matmul(pC[:, (j % 2) * 512:(j % 2 + 1) * 512], lhsT=hT[:, kc, j * 128:(j + 1) * 128],
                                                   rhs=w_in[:, kc, 2048:2560], start=(kc == 0), stop=(kc == 7))) for kc in range(8)],
                       r=r_w_in + [r_hT[j]], w=[r_pC[j % 2]])
                    V(lambda e: e.tensor_copy(out=vring[:, slot, :, 0:64],
                                              in_=pC[:, (j % 2) * 512:(j % 2 + 1) * 512].rearrange("p (h d) -> p h d", h=8)),
                      r=[r_pC[j % 2]], w=[r_vring[slot]])
                    pump(3)
                for j in range(TPG):
                    att_tile(g, j, rnn_all)
                V(lambda e: e.tensor_copy(out=stmp[:, 0:4], in_=ssqr[:]), r=[r_ssqr], w=[r_stmp])
                V(lambda e: e.tensor_copy(out=stmp[:, 4:8], in_=ssqa[:]), r=[r_ssqa], w=[r_stmp])
                V(lambda e: e.tensor_scalar(out=stmp[:], in0=stmp[:], scalar1=1.0 / 512, scalar2=EPS, op0=ALU.mult, op1=ALU.add),
                  r=[r_stmp], w=[r_stmp])
                GP(lambda e: e.tensor_tensor(out=rstdg[:], in0=stmp[:], in1=neg05[:], op=ALU.pow), r=[r_stmp, r_neg], w=[r_rstdg])
                for j in range(TPG):
                    out_proj(j)
                    norm_stats(j)
                rstd_op(ssq[:], 4, D, stmp[:, 0:4], rstd[:], [r_ssq], r_stmp, r_rstd)
                for j in range(TPG):
                    norm_to_hT(j, ln2w)
                for fo in range(8):
                    proj_fm(wq, [r_wq], fo * 128, pA[:, (fo % 2) * 512:(fo % 2 + 1) * 512], r_pA[fo % 2])
                    A(lambda e: e.activation(out=qxT[:, fo, :], in_=pA[:, (fo % 2) * 512:(fo % 2 + 1) * 512], func=AF.Copy),
                      r=[r_pA[fo % 2]], w=[r_qxT[fo]])
                cross_attn()
                for j in range(TPG):
                    wo_proj(j)
                    norm_stats(j)
                rstd_op(ssq[:], 4, D, stmp[:, 0:4], rstd[:], [r_ssq], r_stmp, r_rstd)
                pending = route_group(g)
            pending()
            tk.barrier()

        with ExitStack() as sbk:
            NB = 3
            wg = [sb("wg%d" % k, [128, 8, 512], BF16, sbk) for k in range(NB)]
            wu = [sb("wu%d" % k, [128, 8, 512], BF16, sbk) for k in range(NB)]
            wd = [sb("wd%d" % k, [128, 4, D], BF16, sbk) for k in range(NB)]
            r_wg = [Res() for _ in range(NB)]; r_wu = [Res() for _ in range(NB)]; r_wd = [Res() for _ in range(NB)]
            xsl = [sb("xsl%d" % k, [128, CAP // 128, D], BF16, sbk) for k in range(NB)]
            r_xsl = [Res() for _ in range(NB)]
            xsT = sb("xsT", [128, 8, CAP], BF16, sbk); r_xsT = Res()
            sg = [sb("sg%d" % k, [128, CAP], F32, sbk) for k in range(2)]
            r_sg = [Res(), Res()]
            aT = sb("aT", [128, 4, CAP], BF16, sbk)
            r_aT = [Res() for _ in range(4)]
            yb = [sb("yb%d" % k, [128, D], F32, sbk) for k in range(2)]
            r_yb = [Res(), Res()]

            def load_expert(e_):
                k = e_ % NB
                tk.dma("pool", "ewg%d" % k, lambda e: e.dma_start(out=wg[k][:], in_=eg_d[e_].rearrange("(k p) n -> p k n", p=128)), w=[r_wg[k]])
                tk.dma("pool", "ewu%d" % k, lambda e: e.dma_start(out=wu[k][:], in_=eu_d[e_].rearrange("(k p) n -> p k n", p=128)), w=[r_wu[k]])
                tk.dma("pool", "ewd%d" % k, lambda e: e.dma_start(out=wd[k][:], in_=ed_d[e_].rearrange("(k p) n -> p k n", p=128)), w=[r_wd[k]])
                tk.dma("sp", "xsl%d" % k, lambda e: e.dma_start(out=xsl[k][:], in_=xs_d[e_ * CAP:(e_ + 1) * CAP, :].rearrange("(t p) d -> p t d", p=128)),
                       w=[r_xsl[k]])

            xsT2 = [xsT, sb("xsT_b", [128, 8, CAP], BF16, sbk)]
            r_xsT2 = [r_xsT, Res()]

            def tr_steps(e_):
                k = e_ % NB
                xo, r_xo = xsT2[e_ % 2], r_xsT2[e_ % 2]
                st_ = []
                n = 0
                for t in range(CAP // 128):
                    for hf in range(2):
                        pq, r_pq = (pD, r_pD) if n % 2 == 0 else (pE, r_pE)
                        n += 1
                        st_.append(lambda t=t, hf=hf, pq=pq, r_pq=r_pq: mm_transpose(
                            pq, lambda b: xsl[k][:, t, (hf * 4 + b) * 128:(hf * 4 + b + 1) * 128], 4, [r_xsl[k]], [r_pq]))
                        st_.append(lambda t=t, hf=hf, pq=pq, r_pq=r_pq: V(lambda e: e.tensor_tensor(
                            out=xo[:, hf * 4:(hf + 1) * 4, t * 128:(t + 1) * 128], in0=pq.rearrange("p (k t) -> p k t", k=4),
                            in1=ln3w[:, hf * 4:(hf + 1) * 4].unsqueeze(2).broadcast_to([128, 4, 128]), op=ALU.mult),
                            r=[r_pq, r_vecs], w=[r_xo]))
                return st_

            def main_steps(e_):
                k = e_ % NB
                xo, r_xo = xsT2[e_ % 2], r_xsT2[e_ % 2]
                st_ = []
                for fc in range(4):
                    b2 = fc % 2
                    st_.append(lambda fc=fc, b2=b2: PE([(lambda e, kc=kc: e.matmul(pA[:, b2 * 512:b2 * 512 + CAP], lhsT=wg[k][:, kc, fc * 128:(fc + 1) * 128],
                                                                                 rhs=xo[:, kc, :], start=(kc == 0), stop=(kc == 7))) for kc in range(8)],
                                                       r=[r_wg[k], r_xo], w=[r_pA[b2]]))
                    st_.append(lambda fc=fc, b2=b2: PE([(lambda e, kc=kc: e.matmul(pB[:, b2 * 512:b2 * 512 + CAP], lhsT=wu[k][:, kc, fc * 128:(fc + 1) * 128],
                                                                                 rhs=xo[:, kc, :], start=(kc == 0), stop=(kc == 7))) for kc in range(8)],
                                                       r=[r_wu[k], r_xo], w=[r_pB[b2]]))
                    st_.append(lambda b2=b2: A(lambda e: e.activation(out=sg[b2][:], in_=pA[:, b2 * 512:b2 * 512 + CAP], func=AF.Silu),
                                               r=[r_pA[b2]], w=[r_sg[b2]]))
                    st_.append(lambda fc=fc, b2=b2: V(lambda e: e.tensor_tensor(out=aT[:, fc, :], in0=pB[:, b2 * 512:b2 * 512 + CAP], in1=sg[b2][:], op=ALU.mult),
                                                      r=[r_pB[b2], r_sg[b2]], w=[r_aT[fc]]))
                for t in range(CAP // 128):
                    yk = (e_ * (CAP // 128) + t) % 2
                    for half in range(2):
                        st_.append(lambda t=t, half=half: PE([(lambda e, fc=fc: e.matmul(pC[:, half * 512:(half + 1) * 512], lhsT=aT[:, fc, t * 128:(t + 1) * 128],
                                                                                         rhs=wd[k][:, fc, half * 512:(half + 1) * 512], start=(fc == 0), stop=(fc == 3)))
                                                              for fc in range(4)], r=r_aT + [r_wd[k]], w=[r_pC[half]]))
                    st_.append(lambda yk=yk: A(lambda e: e.activation(out=yb[yk][:, 0:512], in_=pC[:, 0:512], func=AF.Copy), r=[r_pC[0], r_yb[yk]], w=[r_yb[yk]]))
                    st_.append(lambda yk=yk: V(lambda e: e.tensor_copy(out=yb[yk][:, 512:1024], in_=pC[:, 512:1024]), r=[r_pC[1], r_yb[yk]], w=[r_yb[yk]]))
                    row0 = e_ * CAP + t * 128
                    st_.append(lambda yk=yk, row0=row0: tk.dma("sp", "ys%d" % yk, lambda e: e.dma_start(out=ys_d[row0:row0 + 128, :], in_=yb[yk][:]),
                                                               r=[r_yb[yk]], w=[Res()]))
                return st_

            load_expert(0)
            load_expert(1)
            for f_ in tr_steps(0):
                f_()
            for e_ in range(NE):
                if e_ + 2 < NE:
                    load_expert(e_ + 2)
                ms_ = main_steps(e_)
                ts_ = tr_steps(e_ + 1) if e_ + 1 < NE else []
                while ms_ or ts_:
                    for _ in range(2):
                        if ms_:
                            ms_.pop(0)()
                    if ts_:
                        ts_.pop(0)()
            tk.barrier()

        with ExitStack() as sc:
            NBC = 4
            x2t = [sb("x2t%d" % k, [128, D], F32, sc) for k in range(NBC)]
            y1t = [sb("y1t%d" % k, [128, D], F32, sc) for k in range(NBC)]
            y2t = [sb("y2t%d" % k, [128, D], F32, sc) for k in range(NBC)]
            r_x2t = [Res() for _ in range(NBC)]; r_y1t = [Res() for _ in range(NBC)]; r_y2t = [Res() for _ in range(NBC)]
            junkc = [sb("junkc%d" % k, [128, D], BF16, sc) for k in range(2)]; r_junkc = [Res(), Res()]
            fin_bc = sb("fin_bc", [128, D], F32, sc); r_fin = Res()
            tk.dma("sp", "fin", lambda e: e.dma_start(out=fin_bc[:], in_=fin_d.partition_broadcast(128)), w=[r_fin])
            fss = sb("fss", [128, NBC], F32, sc); r_fss = [Res() for _ in range(NBC)]
            ftmp = sb("ftmp", [128, NBC], F32, sc); r_ftmp = [Res() for _ in range(NBC)]
            frs = sb("frs", [128, NBC], F32, sc); r_frs = [Res() for _ in range(NBC)]

            def load_c(i):
                k = i % NBC
                tk.dma("sp", "cx%d" % k, lambda e: e.dma_start(out=x2t[k][:], in_=x2s_d[i * 128:(i + 1) * 128, :]), r=[r_x2s[i]], w=[r_x2t[k]])
                tk.dma("pool", "cy1%d" % k, lambda e: e.indirect_dma_start(
                    out=y1t[k][:], out_offset=None, in_=ys_d, in_offset=bass.IndirectOffsetOnAxis(ap=dest_all[:, 2 * i:2 * i + 1], axis=0)),
                    r=[r_dest[i]], w=[r_y1t[k]])
                tk.dma("pool", "cy2%d" % k, lambda e: e.indirect_dma_start(
                    out=y2t[k][:], out_offset=None, in_=ys_d, in_offset=bass.IndirectOffsetOnAxis(ap=dest_all[:, 2 * i + 1:2 * i + 2], axis=0)),
                    r=[r_dest[i]], w=[r_y2t[k]])

            def c_steps(i):
                k = i % NBC
                jk = i % 2
                st_ = []
                st_.append(lambda: V(lambda e: e.scalar_tensor_tensor(out=x2t[k][:], in0=y1t[k][:], scalar=cw_all[:, 2 * i:2 * i + 1], in1=x2t[k][:],
                                                                      op0=ALU.mult, op1=ALU.add), r=[r_y1t[k], r_cw[i], r_x2t[k]], w=[r_x2t[k]]))
                st_.append(lambda: V(lambda e: e.scalar_tensor_tensor(out=x2t[k][:], in0=y2t[k][:], scalar=cw_all[:, 2 * i + 1:2 * i + 2], in1=x2t[k][:],
                                                                      op0=ALU.mult, op1=ALU.add), r=[r_y2t[k], r_cw[i], r_x2t[k]], w=[r_x2t[k]]))
                st_.append(lambda: A(lambda e: e.activation(out=junkc[jk][:], in_=x2t[k][:], func=AF.Square, accum_out=fss[:, k:k + 1]),
                                     r=[r_x2t[k]], w=[r_junkc[jk], r_fss[k]]))
                st_.append(lambda: V(lambda e: e.tensor_scalar(out=ftmp[:, k:k + 1], in0=fss[:, k:k + 1], scalar1=1.0 / D, scalar2=EPS,
                                                               op0=ALU.mult, op1=ALU.add), r=[r_fss[k]], w=[r_ftmp[k]]))
                st_.append(lambda: GP(lambda e: e.tensor_tensor(out=frs[:, k:k + 1], in0=ftmp[:, k:k + 1], in1=neg05[:, 0:1], op=ALU.pow),
                                      r=[r_ftmp[k], r_neg], w=[r_frs[k]]))
                st_.append(lambda: A(lambda e: e.activation(out=y2t[k][:], in_=x2t[k][:], func=AF.Copy, scale=frs[:, k:k + 1]),
                                     r=[r_x2t[k], r_frs[k]], w=[r_y2t[k]]))
                st_.append(lambda: V(lambda e: e.tensor_tensor(out=y1t[k][:], in0=y2t[k][:], in1=fin_bc[:], op=ALU.mult),
                                     r=[r_y2t[k], r_fin], w=[r_y1t[k]]))
                st_.append(lambda: tk.dma("sp", "out%d" % k, lambda e: e.dma_start(out=out_d[i * 128:(i + 1) * 128, :], in_=y1t[k][:]), r=[r_y1t[k]]))
                return st_

            load_c(0)
            load_c(1)
            for i in range(0, NT, 2):
                for t_ in (i + 2, i + 3):
                    if t_ < NT:
                        load_c(t_)
                a_ = c_steps(i)
                b_ = c_steps(i + 1)
                while a_ or b_:
                    if a_:
                        a_.pop(0)()
                    if b_:
                        b_.pop(0)()
            for k in range(NBC):
                ds = tk.dsem("out%d" % k)
                nc.sync.wait_ge(ds.sem, ds.cnt)
    return nc


def _prep_shared(inp):
    f = np.float32
    vecs = np.zeros((128, NV), f)

    def pc(v, n):
        return np.ascontiguousarray(np.asarray(v, f).reshape(n, 128).T)

    vecs[:, 0:8] = pc(inp["ln1_w"][0], 8)
    vecs[:, 8:16] = pc(inp["ln2_w"][0], 8)
    vecs[:, 16:20] = pc(inp["gn_rnn_w"][0], 4)
    vecs[:, 20:24] = pc(inp["gn_att_w"][0], 4)
    vecs[:, 24:28] = pc(inp["conv_b"][0], 4)
    vecs[:, 28:32] = pc(inp["rnn_ba"][0], 4)
    vecs[:, 32:36] = pc(inp["rnn_bx"][0], 4)
    vecs[:, 36:40] = pc(inp["rnn_lambda"][0], 4)
    vecs[:, 56:64] = pc(inp["ln3_w"][0], 8)
    cwt = np.asarray(inp["conv_w"][0], f)
    for c in range(4):
        for j in range(4):
            vecs[:, 40 + c * 4 + j] = cwt[j, c * 128:(c + 1) * 128]
    bd = np.zeros((128, 2, 4, 128), f)
    for gi, name in enumerate(("rnn_wa", "rnn_wx")):
        w = np.asarray(inp[name][0], f)
        for c in range(4):
            for b in range(2):
                bd[b * 64:(b + 1) * 64, gi, c, b * 64:(b + 1) * 64] = w[2 * c + b]
    rb = np.asarray(inp["rel_bias"][0], f)
    kk = np.arange(128)[:, None]
    qq = np.arange(640)[None, :]
    idx = np.clip(qq - kk, -128, 128) + 128
    valid = np.where(kk < 64, (qq < 576), (qq >= 64))
    bm = np.empty((128, 8, 640), f)
    for h in range(8):
        bm[:, h, :] = np.where(valid, rb[h][idx], f(-30000.0))
    wr = np.concatenate([np.asarray(inp["router_group_w"][0], f),
                         np.asarray(inp["router_expert_w"][0], f).transpose(1, 0, 2).reshape(D, 32)], axis=1)
    rbias = np.concatenate([np.asarray(inp["router_group_b"][0], f), np.asarray(inp["router_expert_b"][0], f).reshape(32)])
    shared = {
        "vecs": vecs, "bm": bm, "bd": bd, "wr": np.ascontiguousarray(wr), "rb": np.ascontiguousarray(rbias),
        "w_in": np.ascontiguousarray(inp["w_in"][0], f), "w_out": np.ascontiguousarray(inp["w_out"][0], f),
        "wq": np.ascontiguousarray(inp["xq_w"][0], f), "wk": np.ascontiguousarray(inp["xk_w"][0], f),
        "wv": np.ascontiguousarray(inp["xv_w"][0], f), "wo": np.ascontiguousarray(inp["xo_w"][0], f),
        "memw": np.ascontiguousarray(inp["mem_norm_w"], f),
        "fin": np.ascontiguousarray(inp["final_norm_w"], f),
        "eg": np.ascontiguousarray(inp["expert_gate_w"][0], f), "eu": np.ascontiguousarray(inp["expert_up_w"][0], f),
        "ed": np.ascontiguousarray(inp["expert_down_w"][0], f),
    }
    return shared


def kernel(**inputs):
    inp = {k: np.asarray(v) for k, v in inputs.items()}
    shared = _prep_shared(inp)
    nc = build_nc()
    in_maps = []
    for b in range(8):
        m = dict(shared)
        m["x"] = np.ascontiguousarray(inp["x"][b], np.float32)
        m["mem"] = np.ascontiguousarray(inp["mem"][b], np.float32)
        in_maps.append(m)
    res = run_bass_kernel_spmd(nc, in_maps, core_ids=list(range(8)))
    return np.stack([np.asarray(r["out"], np.float32) for r in res.results], axis=0)
```

```python
import numpy as np
from contextlib import ExitStack

import concourse.bass as bass
import concourse.mybir as mybir
from concourse.bass_utils import run_bass_kernel_spmd

F32 = mybir.dt.float32
BF16 = mybir.dt.bfloat16
I32 = mybir.dt.int32
AF = mybir.ActivationFunctionType
ALU = mybir.AluOpType
AX = mybir.AxisListType

S = 4096
D = 1024
NT = 32
GT = 512
NG = 8
TPG = 4
NE = 32
CAP = 512
NSLOT = NE * CAP
NROWS = NSLOT + 1024
TRASH = NSLOT
EPS = 1e-6
NV = 64
GELU_K = 0.7978845608028654


class Res:
    __slots__ = ("w", "r")

    def __init__(self):
        self.w = {}
        self.r = {}


class DSem:
    def __init__(self, sem):
        self.sem = sem
        self.cnt = 0


class TK:
    def __init__(self, nc, stack):
        self.nc = nc
        self.stack = stack
        self.eng = {"pe": nc.tensor, "dve": nc.vector, "act": nc.scalar, "pool": nc.gpsimd, "sp": nc.sync}
        self.esem = {}
        self.ecnt = {}
        self.seen = {}
        for k in self.eng:
            self.esem[k] = stack.enter_context(nc.semaphore("es_" + k))
            self.ecnt[k] = 0
            self.seen[k] = {}
        self.dsems = []
        self.dsd = {}

    def dsem(self, name):
        if name not in self.dsd:
            d = DSem(self.stack.enter_context(self.nc.semaphore("m_" + name)))
            self.dsems.append(d)
            self.dsd[name] = d
        return self.dsd[name]

    @staticmethod
    def _merge(d, tokd):
        for k, (s, v) in tokd.items():
            if v is None or k not in d or d[k][1] < v:
                d[k] = (s, v)

    @staticmethod
    def _flat(rs):
        out = []
        for r in rs:
            if isinstance(r, (list, tuple)):
                out.extend(TK._flat(r))
            else:
                out.append(r)
        return out

    def _deps(self, reads, writes):
        d = {}
        for r in self._flat(reads):
            self._merge(d, r.w)
        for w in self._flat(writes):
            self._merge(d, w.w)
            self._merge(d, w.r)
        return d

    def _wait(self, e, deps):
        E = self.eng[e]
        seen = self.seen[e]
        for k, (s, v) in deps.items():
            if e == "pe" and k == "pe":
                continue
            if v is None:
                sem, val = s.sem, s.cnt
            else:
                sem, val = s, v
            if seen.get(k, 0) < val:
                E.wait_ge(sem, val)
                seen[k] = val

    @staticmethod
    def _update(reads, writes, key, tok):
        for r in TK._flat(reads):
            r.r[key] = tok
        for w in TK._flat(writes):
            w.w = {key: tok}
            w.r = {}

    def op(self, e, fn, r=(), w=()):
        self._wait(e, self._deps(r, w))
        ins = fn(self.eng[e])
        self.ecnt[e] += 1
        ins.then_inc(self.esem[e], 1)
        self._update(r, w, e, (self.esem[e], self.ecnt[e]))

    def ops(self, e, fns, r=(), w=()):
        self._wait(e, self._deps(r, w))
        ins = None
        for fn in fns:
            ins = fn(self.eng[e])
        self.ecnt[e] += 1
        ins.then_inc(self.esem[e], 1)
        self._update(r, w, e, (self.esem[e], self.ecnt[e]))

    def dma(self, q, name, fn, r=(), w=()):
        ds = self.dsem(name)
        self._wait(q, self._deps(r, w))
        ins = fn(self.eng[q])
        ds.cnt += 16
        ins.then_inc(ds.sem, 16)
        self._update(r, w, "d_" + name, (ds, None))

    def barrier(self):
        d = {}
        for k in self.eng:
            if self.ecnt[k] > 0:
                d[k] = (self.esem[k], self.ecnt[k])
        for name, ds in self.dsd.items():
            if ds.cnt > 0:
                d["d_" + name] = (ds.sem, ds.cnt)
        for e in self.eng:
            E = self.eng[e]
            seen = self.seen[e]
            for k, (s, v) in d.items():
                if k == e:
                    continue
                if seen.get(k, 0) < v:
                    E.wait_ge(s, v)
                    seen[k] = v


def build_nc():
    nc = bass.Bass("TRN2", target_bir_lowering=False)

    def din(name, shape, dt=F32):
        return nc.dram_tensor(name, shape, dt, kind="ExternalInput").ap()

    x_d = din("x", [S, D])
    mem_d = din("mem", [256, D])
    vecs_d = din("vecs", [128, NV])
    bm_d = din("bm", [128, 8, 640])
    bd_d = din("bd", [128, 2, 4, 128])
    wr_d = din("wr", [D, 36])
    rb_d = din("rb", [36])
    w_in_d = din("w_in", [D, 2560])
    w_out_d = din("w_out", [D, D])
    wq_d = din("wq", [D, D])
    wk_d = din("wk", [D, D])
    wv_d = din("wv", [D, D])
    wo_d = din("wo", [D, D])
    memw_d = din("memw", [D])
    fin_d = din("fin", [D])
    eg_d = din("eg", [NE, D, 512])
    eu_d = din("eu", [NE, D, 512])
    ed_d = din("ed", [NE, 512, D])
    out_d = nc.dram_tensor("out", [S, D], F32, kind="ExternalOutput").ap()
    x2s_d = nc.dram_tensor("x2s", [S, D], F32, kind="Internal").ap()
    xs_d = nc.dram_tensor("xs", [NROWS, D], BF16, kind="Internal").ap()
    ys_d = nc.dram_tensor("ys", [NSLOT + 1, D], F32, kind="Internal").ap()

    with ExitStack() as st:
        tk = TK(nc, st)

        def sb(name, shape, dt, stack=st):
            return stack.enter_context(nc.sbuf_tensor("s_" + name, shape, dt))

        def psum(name, shape, dt):
            return st.enter_context(nc.psum_tensor(name, shape, dt))

        def V(fn, r=(), w=()):
            tk.op("dve", fn, r, w)

        def A(fn, r=(), w=()):
            tk.op("act", fn, r, w)

        def GP(fn, r=(), w=()):
            tk.op("pool", fn, r, w)

        def PE(fns, r=(), w=()):
            tk.ops("pe", fns, r, w)

        pall = psum("pall", [128, 8 * 512], F32)
        pA = pall[:, 0:1024]
        pB = pall[:, 1024:2048]
        pC = pall[:, 2048:3072]
        pD = pall[:, 3072:3584]
        pE = pall[:, 3584:4096]
        r_pE = Res()
        r_pA = [Res(), Res()]
        r_pB = [Res(), Res()]
        r_pC = [Res(), Res()]
        r_pD = Res()
        SX = [pA[:, 0:640], pB[:, 0:640]]
        r_SX = [r_pA, r_pB]

        identf = sb("identf", [128, 128], F32)
        ident = sb("ident", [128, 128], BF16)
        ones_bf = sb("ones_bf", [128, 128], BF16)
        trif = sb("trif", [128, 128], F32)
        tri_bf = sb("tri_bf", [128, 128], BF16)
        vecs = sb("vecs", [128, NV], F32)
        hb = sb("hb", [128, 8], F32)
        hc = sb("hc", [128, 8], F32)
        spt = sb("spt", [128, 4], F32)
        rb_bc = sb("rb_bc", [128, 36], F32)
        dest_all = sb("dest_all", [128, 2 * NT], I32)
        cw_all = sb("cw_all", [128, 2 * NT], F32)
        cnt = sb("cnt", [128, NE], F32)
        eoff_i = sb("eoff_i", [128, NE], I32)
        eoff = sb("eoff", [128, NE], F32)
        neg05 = sb("neg05", [128, 8], F32)
        qtr = sb("qtr", [128, 1], F32)
        hstate = sb("hstate", [128, 4], F32)
        halo = sb("halo", [128, 4, 3], F32)
        zt = sb("zt", [128, 8], F32)
        r_ident = Res(); r_identf = Res(); r_ones = Res(); r_trif = Res(); r_tri = Res()
        r_vecs = Res(); r_hb = Res(); r_hc = Res(); r_spt = Res()
        r_rb = Res()
        r_dest = [Res() for _ in range(NT)]
        r_cw = [Res() for _ in range(NT)]
        r_cnt = Res(); r_eoffi = Res(); r_eoff = Res(); r_neg = Res()
        r_hstate = [Res() for _ in range(4)]
        r_halo = [Res() for _ in range(4)]
        r_zrow = Res()
        r_x2s = [Res() for _ in range(NT)]
        r_xs = Res()
        r_ys = Res()

        ln1w = vecs[:, 0:8]
        ln2w = vecs[:, 8:16]
        gnrw = vecs[:, 16:20]
        gnaw = vecs[:, 20:24]
        convb = vecs[:, 24:28]
        lam = vecs[:, 36:40]
        ln3w = vecs[:, 56:64]

        tk.dma("sp", "vecs", lambda e: e.dma_start(out=vecs[:], in_=vecs_d), w=[r_vecs])
        tk.dma("sp", "rb", lambda e: e.dma_start(out=rb_bc[:], in_=rb_d.partition_broadcast(128)), w=[r_rb])
        GP(lambda e: e.memset(identf[:], 0.0), w=[r_identf])
        GP(lambda e: e.affine_select(out=identf[:], in_=identf[:], pattern=[[-1, 128]], compare_op=ALU.not_equal,
                                     fill=1.0, base=0, channel_multiplier=1), r=[r_identf], w=[r_identf])
        V(lambda e: e.tensor_copy(out=ident[:], in_=identf[:]), r=[r_identf], w=[r_ident])
        GP(lambda e: e.memset(trif[:], 1.0), w=[r_trif])
        GP(lambda e: e.affine_select(out=trif[:], in_=trif[:], pattern=[[1, 128]], compare_op=ALU.is_gt,
                                     fill=0.0, base=0, channel_multiplier=-1), r=[r_trif], w=[r_trif])
        V(lambda e: e.tensor_copy(out=tri_bf[:], in_=trif[:]), r=[r_trif], w=[r_tri])
        V(lambda e: e.memset(ones_bf[:], 1.0), w=[r_ones])
        GP(lambda e: e.iota(eoff_i[:], pattern=[[CAP, NE]], base=0, channel_multiplier=0), w=[r_eoffi])
        V(lambda e: e.tensor_copy(out=eoff[:], in_=eoff_i[:]), r=[r_eoffi], w=[r_eoff])
        V(lambda e: e.memset(neg05[:], -0.5), w=[r_neg])
        V(lambda e: e.memset(qtr[:], 0.25), w=[r_neg])
        V(lambda e: e.memset(cnt[:], 0.0), w=[r_cnt])
        V(lambda e: e.memset(hstate[:], 0.0), w=r_hstate)
        V(lambda e: e.memset(halo[:], 0.0), w=r_halo)
        V(lambda e: e.memset(zt[:], 0.0), w=[r_zrow])
        tk.dma("sp", "zt", lambda e: e.dma_start(out=ys_d[TRASH].rearrange("(p f) -> p f", p=128), in_=zt[:]), r=[r_zrow], w=[Res()])
        V(lambda e: e.tensor_scalar(out=hb[:], in0=vecs[:, 28:36], scalar1=0.5, scalar2=None, op0=ALU.mult), r=[r_vecs], w=[r_hb])
        A(lambda e: e.activation(out=spt[:], in_=lam, func=AF.Exp, scale=-1.0), r=[r_vecs], w=[r_spt])
        A(lambda e: e.activation(out=spt[:], in_=spt[:], func=AF.Ln, bias=1.0), r=[r_spt], w=[r_spt])
        V(lambda e: e.tensor_scalar(out=hc[:, 0:4], in0=spt[:], scalar1=-4.0, scalar2=None, op0=ALU.mult), r=[r_spt], w=[r_hc])
        V(lambda e: e.tensor_scalar(out=hc[:, 4:8], in0=spt[:], scalar1=-8.0, scalar2=None, op0=ALU.mult), r=[r_spt], w=[r_hc])

        def rstd_op(src_ap, n, dim, tmp_ap, out_ap, r_src, r_tmp, r_out):
            V(lambda e: e.tensor_scalar(out=tmp_ap, in0=src_ap, scalar1=1.0 / dim, scalar2=EPS, op0=ALU.mult, op1=ALU.add),
              r=r_src, w=[r_tmp])
            GP(lambda e: e.tensor_tensor(out=out_ap, in0=tmp_ap, in1=neg05[:, 0:n], op=ALU.pow), r=[r_tmp, r_neg], w=[r_out])

        with ExitStack() as sa:
            w_in = sb("w_in", [128, 8, 2560], BF16, sa)
            w_out = sb("w_out", [128, 8, D], BF16, sa)
            wq = sb("wq", [128, 8, D], BF16, sa)
            wo = sb("wo", [128, 8, D], BF16, sa)
            bd = sb("bd", [128, 2, 4, 128], BF16, sa)
            wr = sb("wr", [128, 8, 36], BF16, sa)
            bm = sb("bm", [128, 8, 640], BF16, sa)
            kTm = sb("kTm", [128, 8, 256], BF16, sa)
            vm = sb("vm", [128, 2, D], BF16, sa)
            kring = sb("kring", [128, 4, 1024], BF16, sa)
            vring = sb("vring", [128, 8, 8, 65], BF16, sa)
            r_w_in = [Res(), Res()]; r_w_out = Res(); r_wq = Res(); r_wo = Res(); r_bd = Res(); r_wr = Res(); r_bm = Res()
            r_kTm = Res(); r_vm = Res()
            r_kring = [[Res() for _ in range(4)] for _ in range(2)]
            r_vring = [Res() for _ in range(8)]

            def wview(dram):
                return dram.rearrange("(k p) n -> p k n", p=128)

            def mm_transpose(dst, src_fn, nblk, r, w):
                PE([(lambda e, b=b: e.matmul(dst[:, b * 128:(b + 1) * 128], lhsT=src_fn(b), rhs=ident[:], start=True, stop=True))
                    for b in range(nblk)], r=list(r) + [r_ident], w=w)


            zt2 = sb("zt2", [128, D], BF16, sa); r_zt2 = Res(); r_xsz = Res()
            V(lambda e: e.memset(zt2[:], 0.0), w=[r_zt2])

            tk.dma("pool", "w_in0", lambda e: e.dma_start(out=w_in[:, :, 0:1280], in_=wview(w_in_d)[:, :, 0:1280]), w=[r_w_in[0]])
            tk.dma("pool", "w_in1", lambda e: e.dma_start(out=w_in[:, :, 1280:2560], in_=wview(w_in_d)[:, :, 1280:2560]), w=[r_w_in[1]])

            with ExitStack() as s0:
                wk = sb("wk", [128, 8, D], BF16, s0)
                wv = sb("wv", [128, 8, D], BF16, s0)
                memt = sb("memt", [128, 2, D], F32, s0)
                memn = sb("memn", [128, 2, D], BF16, s0)
                memT = sb("memT", [128, 8, 256], BF16, s0)
                memw_bc = sb("memw_bc", [128, D], F32, s0)
                junk0 = sb("junk0", [128, D], F32, s0)
                mss = sb("mss", [128, 2], F32, s0)
                mtmp = sb("mtmp", [128, 2], F32, s0)
                mrs = sb("mrs", [128, 2], F32, s0)
                r_wk = Res(); r_wv = Res(); r_memt = Res(); r_memn = Res(); r_memT = Res(); r_memw = Res()
                r_junk0 = Res(); r_mss = Res(); r_mtmp = Res(); r_mrs = Res()
                tk.dma("sp", "memt", lambda e: e.dma_start(out=memt[:], in_=mem_d.rearrange("(t p) d -> p t d", p=128)), w=[r_memt])
                tk.dma("sp", "memw", lambda e: e.dma_start(out=memw_bc[:], in_=memw_d.partition_broadcast(128)), w=[r_memw])
                tk.dma("pool", "wk", lambda e: e.dma_start(out=wk[:], in_=wview(wk_d)), w=[r_wk])
                tk.dma("pool", "wv", lambda e: e.dma_start(out=wv[:], in_=wview(wv_d)), w=[r_wv])
                for t in range(2):
                    A(lambda e: e.activation(out=junk0[:], in_=memt[:, t, :], func=AF.Square, accum_out=mss[:, t:t + 1]),
                      r=[r_memt], w=[r_junk0, r_mss])
                rstd_op(mss[:], 2, D, mtmp[:], mrs[:], [r_mss], r_mtmp, r_mrs)
                for t in range(2):
                    V(lambda e: e.scalar_tensor_tensor(out=memn[:, t, :], in0=memt[:, t, :], scalar=mrs[:, t:t + 1], in1=memw_bc[:],
                                                       op0=ALU.mult, op1=ALU.mult), r=[r_memt, r_mrs, r_memw], w=[r_memn])
                    mm_transpose(pC, lambda b: memn[:, t, b * 128:(b + 1) * 128], 8, [r_memn], r_pC)
                    V(lambda e: e.tensor_copy(out=memT[:, :, t * 128:(t + 1) * 128], in_=pC.rearrange("p (k t) -> p k t", k=8)),
                      r=r_pC, w=[r_memT])
                for fo in range(8):
                    bank = fo % 2
                    PE([(lambda e, kc=kc: e.matmul(pA[:, bank * 512:bank * 512 + 256], lhsT=wk[:, kc, fo * 128:(fo + 1) * 128],
                                                   rhs=memT[:, kc, :], start=(kc == 0), stop=(kc == 7))) for kc in range(8)],
                       r=[r_wk, r_memT], w=[r_pA[bank]])
                    A(lambda e: e.activation(out=kTm[:, fo, :], in_=pA[:, bank * 512:bank * 512 + 256], func=AF.Copy),
                      r=[r_pA[bank]], w=[r_kTm])
                for mt in range(2):
                    for half in range(2):
                        PE([(lambda e, kc=kc: e.matmul(pB[:, half * 512:(half + 1) * 512], lhsT=memT[:, kc, mt * 128:(mt + 1) * 128],
                                                       rhs=wv[:, kc, half * 512:(half + 1) * 512], start=(kc == 0), stop=(kc == 7)))
                            for kc in range(8)], r=[r_wv, r_memT], w=[r_pB[half]])
                        A(lambda e: e.activation(out=vm[:, mt, half * 512:(half + 1) * 512], in_=pB[:, half * 512:(half + 1) * 512],
                                                 func=AF.Copy), r=[r_pB[half]], w=[r_vm])
                tk.barrier()

            tk.dma("pool", "bd", lambda e: e.dma_start(out=bd[:], in_=bd_d), w=[r_bd])
            tk.dma("pool", "bm", lambda e: e.dma_start(out=bm[:], in_=bm_d), w=[r_bm])
            tk.dma("pool", "wr", lambda e: e.dma_start(out=wr[:], in_=wview(wr_d)), w=[r_wr])
            tk.dma("pool", "w_out", lambda e: e.dma_start(out=w_out[:], in_=wview(w_out_d)), w=[r_w_out])
            tk.dma("pool", "wq", lambda e: e.dma_start(out=wq[:], in_=wview(wq_d)), w=[r_wq])
            tk.dma("pool", "wo", lambda e: e.dma_start(out=wo[:], in_=wview(wo_d)), w=[r_wo])
            V(lambda e: e.memset(vring[:].rearrange("p a b c -> p (a b c)"), 1.0), w=r_vring)
            xs_z = xs_d.rearrange("(p r) d -> p r d", p=128)
            RZ = NROWS // 128
            ZC = 34
            for zi in range(RZ // ZC):
                tk.dma("sp", "xsz", lambda e: e.dma_start(out=xs_z[:, zi * ZC:(zi + 1) * ZC, :],
                                                          in_=zt2[:].unsqueeze(1).broadcast_to([128, ZC, D])), r=[r_zt2, r_w_out, r_wq, r_wo, r_bm, r_bd, r_wr], w=[r_xsz])

            xt = [sb("xt%d" % j, [128, D], F32, sa) for j in range(TPG)]
            r_xt = [Res() for _ in range(TPG)]
            xn = [sb("xn0", [128, D], BF16, sa)] * 2
            r_xn = [Res()] * 2
            junk = xn[0]; r_junk = r_xn[0]
            hT = sb("hT", [128, 8, GT], BF16, sa)
            r_hT = [Res() for _ in range(TPG)]
            mix = sb("mix", [128, 12, GT], BF16, sa)
            r_mix = [Res() for _ in range(12)]
            qT = mix[:, 0:8, :]
            r_qT = r_mix[0:8]
            yrT = mix[:, 8:12, :]
            r_yrT = r_mix[8:12]
            yaT = sb("yaT", [128, 4, GT], BF16, sa)
            r_yaT = [Res() for _ in range(TPG)]
            qxT = mix[:, 0:8, :]
            r_qxT = r_mix[0:8]
            ssq = sb("ssq", [128, 4], F32, sa); r_ssq = Res()
            stmp = sb("stmp", [128, 8], F32, sa); r_stmp = Res()
            rstd = sb("rstd", [128, 4], F32, sa); r_rstd = Res()
            ssqr = sb("ssqr", [128, 4], F32, sa); r_ssqr = Res()
            ssqa = sb("ssqa", [128, 4], F32, sa); r_ssqa = Res()
            rstdg = sb("rstdg", [128, 8], F32, sa); r_rstdg = Res()
            xrp = sb("xrp", [128, GT + 3], F32, sa); r_xrp = Res()
            xc = sb("xc", [128, GT], F32, sa); r_xc = Res()
            xcb = sb("xcb", [128, GT], BF16, sa); r_xcb = Res()
            tr_ = sb("tr_", [128, GT], F32, sa); r_tr = Res()
            ti_ = sb("ti_", [128, GT], F32, sa); r_ti = Res()
            av = sb("av", [128, GT], F32, sa); r_av = Res()
            vv = sb("vv", [128, GT], F32, sa); r_vv = Res()
            hs = sb("hs", [128, GT], F32, sa); r_hs = Res()
            xgs = sb("xgs", [128, GT], F32, sa); r_xgs = Res()
            g1 = sb("g1", [128, GT], F32, sa); r_g1 = Res()
            g2 = sb("g2", [128, GT], F32, sa); r_g2 = Res()
            a2 = hs; r_a2 = r_hs
            gl = av; r_gl = r_av
            yraw = vv; r_yraw = r_vv
            ysq = sb("ysq", [128, GT], BF16, sa); r_ysq = Res()
            Eb = [sb("Eb%d" % k, [128, 640], BF16, sa) for k in range(2)]
            r_Eb = [Res(), Res()]
            rden8 = sb("rden8", [128, 8], F32, sa); r_rden8 = Res()
            ex = [sb("ex0", [128, 2, GT], BF16, sa)] * 2
            r_ex = [Res()] * 2
            rdn = tr_; r_rdn = r_tr
            h3 = [sb("h3_%d" % k, [128, D], BF16, sa) for k in range(2)]
            r_h3 = [Res(), Res()]
            h3T = sb("h3T", [128, 8, 128], BF16, sa); r_h3T = Res()
            yat = h3[0][:].bitcast(F32); r_yat = r_h3[0]
            yab = Eb[0][:, 0:512]; r_yab = r_Eb[0]
            rt = sb("rt", [128, 772], F32, sa)
            r_rt = Res()
            Ab4 = xcb[:, 0:4 * NE].rearrange("p (j e) -> p j e", j=4); r_Ab = r_xcb

            def rtv(a, b, **kw):
                v = rt[:, a:b]
                return v.rearrange(kw.pop("pat"), **kw) if kw else v

            lg4 = rtv(0, 144, pat="p (j c) -> p j c", j=4)
            gmax4 = rt[:, 144:148]
            gsum4 = rt[:, 148:152]
            gp4 = rt[:, 152:156]
            m1_4 = rt[:, 156:160]
            m2_4 = rt[:, 160:164]
            dd4 = rt[:, 164:168]
            edd4 = rt[:, 168:172]
            w1_4 = rt[:, 172:176]
            w2_4 = rt[:, 176:180]
            gsh4 = rtv(180, 196, pat="p (j c) -> p j c", j=4)
            gex4 = rtv(196, 212, pat="p (j c) -> p j c", j=4)
            goh4 = rtv(212, 228, pat="p (j c) -> p j c", j=4)
            el4 = rtv(228, 260, pat="p (j c) -> p j c", j=4)
            oh1_4 = rtv(260, 292, pat="p (j c) -> p j c", j=4)
            el2_4 = rtv(292, 324, pat="p (j c) -> p j c", j=4)
            oh2_4 = rtv(324, 356, pat="p (j c) -> p j c", j=4)
            pk4 = rtv(356, 364, pat="p (j k) -> p j k", j=4)
            ek4 = rtv(364, 372, pat="p (j k) -> p j k", j=4)
            ok4 = rtv(372, 380, pat="p (j k) -> p j k", j=4)
            sd4 = rtv(380, 388, pat="p (j k) -> p j k", j=4)
            prod4 = rtv(388, 516, pat="p (j g e) -> p j g e", j=4, g=4)
            A1_4 = rtv(516, 644, pat="p (j c) -> p j c", j=4)
            A2_4 = rtv(644, 772, pat="p (j c) -> p j c", j=4)
            posf4 = rtv(0, 128, pat="p (j c) -> p j c", j=4)
            prodE = rtv(388, 516, pat="p (j c) -> p j c", j=4)

            def load_x(g):
                for j in range(TPG):
                    i = g * TPG + j
                    tk.dma("sp", "x%d" % j, lambda e: e.dma_start(out=xt[j][:], in_=x_d[i * 128:(i + 1) * 128, :]), w=[r_xt[j]])

            def norm_stats(j):
                A(lambda e: e.activation(out=junk[:], in_=xt[j][:], func=AF.Square, accum_out=ssq[:, j:j + 1]),
                  r=[r_xt[j]], w=[r_junk, r_ssq])

            def norm_to_hT(j, lnw):
                k = j % 2
                A(lambda e: e.activation(out=xn[k][:], in_=xt[j][:], func=AF.Copy, scale=rstd[:, j:j + 1]),
                  r=[r_xt[j], r_rstd], w=[r_xn[k]])
                pq, r_pq = (pB, r_pB) if j % 2 == 0 else (pC, r_pC)
                mm_transpose(pq, lambda b: xn[k][:, b * 128:(b + 1) * 128], 8, [r_xn[k]], r_pq)
                V(lambda e: e.tensor_tensor(out=hT[:, :, j * 128:(j + 1) * 128], in0=pq.rearrange("p (k t) -> p k t", k=8),
                                            in1=lnw.unsqueeze(2).broadcast_to([128, 8, 128]), op=ALU.mult),
                  r=r_pq + [r_vecs], w=[r_hT[j]])

            def proj_fm(wt, r_w, col0, bank_ap, r_bank):
                PE([(lambda e, kc=kc: e.matmul(bank_ap, lhsT=wt[:, kc, col0:col0 + 128], rhs=hT[:, kc, :],
                                               start=(kc == 0), stop=(kc == 7))) for kc in range(8)],
                   r=list(r_w) + r_hT, w=[r_bank])

            def rnn_steps(g, c):
                C0 = pD
                rC0 = r_pD
                cw0 = 40 + c * 4
                M_ = []
                G_ = []
                T_ = []
                G_.append(lambda: proj_fm(w_in, r_w_in, 512 + c * 128, C0, rC0))
                G_.append(lambda: V(lambda e: e.tensor_copy(out=xgs[:], in_=C0), r=[rC0], w=[r_xgs]))
                G_.append(lambda: A(lambda e: e.activation(out=g1[:], in_=xgs[:], func=AF.Square), r=[r_xgs], w=[r_g1]))
                G_.append(lambda: GP(lambda e: e.tensor_scalar(out=g1[:], in0=g1[:], scalar1=0.044715, scalar2=1.0, op0=ALU.mult, op1=ALU.add),
                                     r=[r_g1], w=[r_g1]))
                G_.append(lambda: GP(lambda e: e.tensor_tensor(out=g1[:], in0=xgs[:], in1=g1[:], op=ALU.mult), r=[r_xgs, r_g1], w=[r_g1]))
                G_.append(lambda: A(lambda e: e.activation(out=g2[:], in_=g1[:], func=AF.Tanh, scale=GELU_K), r=[r_g1], w=[r_g2]))
                G_.append(lambda: V(lambda e: e.scalar_tensor_tensor(out=g2[:], in0=g2[:], scalar=1.0, in1=xgs[:], op0=ALU.add, op1=ALU.mult),
                                    r=[r_g2, r_xgs], w=[r_g2]))
                M_.append(lambda: proj_fm(w_in, r_w_in, c * 128, C0, rC0))
                M_.append(lambda: V(lambda e: e.tensor_copy(out=xrp[:, 3:GT + 3], in_=C0), r=[rC0], w=[r_xrp]))
                M_.append(lambda: V(lambda e: e.tensor_copy(out=xrp[:, 0:3], in_=halo[:, c, :]), r=[r_halo[c]], w=[r_xrp]))
                M_.append(lambda: V(lambda e: e.tensor_scalar(out=xc[:], in0=xrp[:, 3:GT + 3], scalar1=vecs[:, cw0 + 3:cw0 + 4],
                                                               scalar2=convb[:, c:c + 1], op0=ALU.mult, op1=ALU.add),
                                    r=[r_xrp, r_vecs], w=[r_xc]))
                for jj in range(3):
                    M_.append(lambda jj=jj: V(lambda e: e.scalar_tensor_tensor(out=xc[:], in0=xrp[:, jj:jj + GT],
                                                                               scalar=vecs[:, cw0 + jj:cw0 + jj + 1], in1=xc[:],
                                                                               op0=ALU.mult, op1=ALU.add),
                                              r=[r_xrp, r_vecs, r_xc], w=[r_xc]))
                M_.append(lambda: V(lambda e: e.tensor_copy(out=halo[:, c, :], in_=xrp[:, GT:GT + 3]), r=[r_xrp], w=[r_halo[c]]))
                M_.append(lambda: GP(lambda e: e.tensor_copy(out=xcb[:], in_=xc[:]), r=[r_xc], w=[r_xcb]))
                M_.append(lambda: PE([lambda e: e.matmul(C0, lhsT=bd[:, 0, c, :], rhs=xcb[:], start=True, stop=True)],
                                     r=[r_bd, r_xcb], w=[rC0]))
                M_.append(lambda: A(lambda e: e.activation(out=tr_[:], in_=C0, func=AF.Tanh, scale=0.5, bias=hb[:, c:c + 1]),
                                    r=[rC0, r_hb], w=[r_tr]))
                M_.append(lambda: PE([lambda e: e.matmul(C0, lhsT=bd[:, 1, c, :], rhs=xcb[:], start=True, stop=True)],
                                     r=[r_bd, r_xcb], w=[rC0]))
                M_.append(lambda: A(lambda e: e.activation(out=ti_[:], in_=C0, func=AF.Tanh, scale=0.5, bias=hb[:, 4 + c:5 + c]),
                                    r=[rC0, r_hb], w=[r_ti]))
                M_.append(lambda: A(lambda e: e.activation(out=av[:], in_=tr_[:], func=AF.Exp, scale=hc[:, c:c + 1], bias=hc[:, c:c + 1]),
                                    r=[r_tr, r_hc], w=[r_av]))
                M_.append(lambda: A(lambda e: e.activation(out=a2[:], in_=tr_[:], func=AF.Exp, scale=hc[:, 4 + c:5 + c],
                                                           bias=hc[:, 4 + c:5 + c]), r=[r_tr, r_hc], w=[r_a2]))
                M_.append(lambda: V(lambda e: e.scalar_tensor_tensor(out=vv[:], in0=ti_[:], scalar=1.0, in1=xc[:], op0=ALU.add, op1=ALU.mult),
                                    r=[r_ti, r_xc], w=[r_vv]))
                M_.append(lambda: A(lambda e: e.activation(out=a2[:], in_=a2[:], func=AF.Sqrt, scale=-0.25, bias=qtr[:, 0:1]),
                                    r=[r_a2, r_neg], w=[r_a2]))
                M_.append(lambda: V(lambda e: e.tensor_tensor(out=vv[:], in0=vv[:], in1=a2[:], op=ALU.mult), r=[r_vv, r_a2], w=[r_vv]))
                M_.append(lambda: V(lambda e: e.tensor_tensor_scan(out=hs[:], data0=av[:], data1=vv[:], initial=hstate[:, c:c + 1],
                                                                    op0=ALU.mult, op1=ALU.add), r=[r_av, r_vv, r_hstate[c]], w=[r_hs]))
                M_.append(lambda: V(lambda e: e.tensor_copy(out=hstate[:, c:c + 1], in_=hs[:, GT - 1:GT]), r=[r_hs], w=[r_hstate[c]]))
                T_.append(lambda: V(lambda e: e.scalar_tensor_tensor(out=yraw[:], in0=g2[:], scalar=0.5, in1=hs[:], op0=ALU.mult, op1=ALU.mult),
                                    r=[r_g2, r_hs], w=[r_yraw]))
                T_.append(lambda: V(lambda e: e.tensor_scalar(out=yrT[:, c, :], in0=yraw[:], scalar1=gnrw[:, c:c + 1], scalar2=None, op0=ALU.mult),
                                    r=[r_yraw, r_vecs], w=[r_yrT[c]]))
                T_.append(lambda: GP(lambda e: e.tensor_tensor(out=ysq[:], in0=yraw[:], in1=yraw[:], op=ALU.mult), r=[r_yraw], w=[r_ysq]))
                T_.append(lambda: PE([(lambda e, j=j: e.matmul(C0[:, j:j + 1], lhsT=ysq[:, j * 128:(j + 1) * 128], rhs=ones_bf[:, 0:1],
                                                               start=True, stop=True)) for j in range(TPG)], r=[r_ysq, r_ones], w=[rC0]))
                if c == 0:
                    T_.append(lambda: V(lambda e: e.tensor_copy(out=ssqr[:], in_=C0[:, 0:4]), r=[rC0], w=[r_ssqr]))
                else:
                    T_.append(lambda: V(lambda e: e.tensor_tensor(out=ssqr[:], in0=C0[:, 0:4], in1=ssqr[:], op=ALU.add),
                                        r=[rC0, r_ssqr], w=[r_ssqr]))
                out_ = []
                gi = 0
                for mi, m_ in enumerate(M_):
                    out_.append(m_)
                    if mi % 3 == 1 and gi < len(G_):
                        out_.append(G_[gi]); gi += 1
                out_.extend(G_[gi:])
                out_.extend(T_)
                return out_

            def att_tile(g, j, steps):
                i = g * TPG + j
                ms = [m for m in range(5) if i - m >= 0]
                nb = len(ms)
                pBv = pC.rearrange("p (b x) -> p b x", b=2)[:, :, 0:260].rearrange("p b (h d) -> p b h d", d=65)
                kstep = 3

                def emit_S(h):
                    ch = h // 2
                    bi = h % 2
                    fns = []
                    for m in ms:
                        slot = (i - m) % 8
                        fns.append(lambda e, m=m: e.matmul(SX[bi][:, m * 128:(m + 1) * 128], lhsT=ident[:],
                                                           rhs=bm[:, h, m * 128:(m + 1) * 128], start=True, stop=False))
                        fns.append(lambda e, m=m, slot=slot: e.matmul(
                            SX[bi][:, m * 128:(m + 1) * 128], lhsT=kring[:, ch, slot * 128:(slot + 1) * 128],
                            rhs=qT[:, h, j * 128:(j + 1) * 128], start=False, stop=True))
                    PE(fns, r=[r_kring[0][ch], r_kring[1][ch], r_qT[h], r_bm, r_ident], w=[r_SX[bi]])

                emit_S(0)
                for h in range(8):
                    bi = h % 2
                    if h + 1 < 8:
                        emit_S(h + 1)
                    A(lambda e: e.activation(out=Eb[bi][:, 0:nb * 128], in_=SX[bi][:, 0:nb * 128], func=AF.Exp),
                      r=[r_SX[bi]], w=[r_Eb[bi]])
                    fns = []
                    for idx, m in enumerate(ms):
                        slot = (i - m) % 8
                        fns.append(lambda e, m=m, slot=slot, idx=idx: e.matmul(
                            pC[:, (h // 4) * 512 + (h % 4) * 65:(h // 4) * 512 + (h % 4) * 65 + 65],
                            lhsT=Eb[bi][:, m * 128:(m + 1) * 128], rhs=vring[:, slot, h, :], start=(idx == 0), stop=(idx == nb - 1)))
                    PE(fns, r=[r_Eb[bi]] + r_vring, w=[r_pC[h // 4]])
                    for _ in range(kstep):
                        if steps:
                            steps.pop(0)()
                if j == TPG - 1:
                    while steps:
                        steps.pop(0)()
                V(lambda e: e.reciprocal(out=rden8[:].rearrange("p (b h) -> p b h", b=2), in_=pBv[:, :, :, 64]),
                  r=r_pC, w=[r_rden8])
                V(lambda e: e.tensor_tensor(out=yat[:].rearrange("p (b h d) -> p b h d", b=2, h=4), in0=pBv[:, :, :, 0:64],
                                            in1=rden8[:].rearrange("p (b h) -> p b h", b=2).unsqueeze(3).broadcast_to([128, 2, 4, 64]),
                                            op=ALU.mult), r=r_pC + [r_rden8], w=[r_yat])
                A(lambda e: e.activation(out=junk[:, 0:512], in_=yat[:], func=AF.Square, accum_out=ssqa[:, j:j + 1]),
                  r=[r_yat], w=[r_junk, r_ssqa])
                A(lambda e: e.activation(out=yab[:], in_=yat[:], func=AF.Copy), r=[r_yat], w=[r_yab])
                mm_transpose(pE, lambda b: yab[:, b * 128:(b + 1) * 128], 4, [r_yab], [r_pE])
                V(lambda e: e.tensor_tensor(out=yaT[:, :, j * 128:(j + 1) * 128], in0=pE.rearrange("p (k t) -> p k t", k=4),
                                            in1=gnaw.unsqueeze(2).broadcast_to([128, 4, 128]), op=ALU.mult),
                  r=[r_pE, r_vecs], w=[r_yaT[j]])

            def out_proj(j):
                for half in range(2):
                    PE([(lambda e, c=c: e.matmul(pA[:, half * 512:(half + 1) * 512], lhsT=yrT[:, c, j * 128:(j + 1) * 128],
                                                 rhs=w_out[:, c, half * 512:(half + 1) * 512], start=(c == 0), stop=(c == 3)))
                        for c in range(4)], r=r_yrT + [r_w_out], w=[r_pA[half]])
                for half in range(2):
                    PE([(lambda e, c=c: e.matmul(pB[:, half * 512:(half + 1) * 512], lhsT=yaT[:, c, j * 128:(j + 1) * 128],
                                                 rhs=w_out[:, 4 + c, half * 512:(half + 1) * 512], start=(c == 0), stop=(c == 3)))
                        for c in range(4)], r=[r_yaT[j], r_w_out], w=[r_pB[half]])
                V(lambda e: e.scalar_tensor_tensor(out=xt[j][:], in0=pA[:], scalar=rstdg[:, j:j + 1], in1=xt[j][:],
                                                   op0=ALU.mult, op1=ALU.add), r=r_pA + [r_rstdg, r_xt[j]], w=[r_xt[j]])
                V(lambda e: e.scalar_tensor_tensor(out=xt[j][:], in0=pB[:], scalar=rstdg[:, 4 + j:5 + j], in1=xt[j][:],
                                                   op0=ALU.mult, op1=ALU.add), r=r_pB + [r_rstdg, r_xt[j]], w=[r_xt[j]])

            def cross_attn():
                for hh in range(4):
                    k = hh % 2
                    for mt in range(2):
                        PE([(lambda e, dc=dc: e.matmul(pB[:, mt * 512:(mt + 1) * 512], lhsT=kTm[:, 2 * hh + dc, mt * 128:(mt + 1) * 128],
                                                       rhs=qxT[:, 2 * hh + dc, :], start=(dc == 0), stop=(dc == 1))) for dc in range(2)],
                           r=[r_kTm, r_qxT[2 * hh], r_qxT[2 * hh + 1]], w=[r_pB[mt]])
                        A(lambda e: e.activation(out=ex[k][:, mt, :], in_=pB[:, mt * 512:(mt + 1) * 512], func=AF.Exp, scale=0.0625),
                          r=[r_pB[mt]], w=[r_ex[k]])
                    PE([(lambda e, mt=mt: e.matmul(pD[:], lhsT=ones_bf[:], rhs=ex[k][:, mt, :], start=(mt == 0), stop=(mt == 1)))
                        for mt in range(2)], r=[r_ones, r_ex[k]], w=[r_pD])
                    V(lambda e: e.reciprocal(out=rdn[:], in_=pD[:]), r=[r_pD], w=[r_rdn])
                    for dc in range(2):
                        PE([(lambda e, mt=mt: e.matmul(pC[:, dc * 512:(dc + 1) * 512],
                                                       lhsT=vm[:, mt, (2 * hh + dc) * 128:(2 * hh + dc + 1) * 128],
                                                       rhs=ex[k][:, mt, :], start=(mt == 0), stop=(mt == 1))) for mt in range(2)],
                           r=[r_vm, r_ex[k]], w=[r_pC[dc]])
                        V(lambda e: e.tensor_tensor(out=hT[:, 2 * hh + dc, :], in0=pC[:, dc * 512:(dc + 1) * 512], in1=rdn[:], op=ALU.mult),
                          r=[r_pC[dc], r_rdn], w=r_hT)

            def wo_proj(j):
                pw, r_pw = (pA, r_pA) if j % 2 == 0 else (pB, r_pB)
                for half in range(2):
                    PE([(lambda e, kc=kc: e.matmul(pw[:, half * 512:(half + 1) * 512], lhsT=hT[:, kc, j * 128:(j + 1) * 128],
                                                   rhs=wo[:, kc, half * 512:(half + 1) * 512], start=(kc == 0), stop=(kc == 7)))
                        for kc in range(8)], r=r_hT + [r_wo], w=[r_pw[half]])
                V(lambda e: e.tensor_tensor(out=xt[j][:], in0=pw, in1=xt[j][:], op=ALU.add), r=r_pw + [r_xt[j]], w=[r_xt[j]])

            def route_group(g):
                R = [r_rt]
                h3r = [mix[:, 2 * j:2 * j + 2, :].rearrange("p a t -> p (a t)") for j in range(TPG)]
                r_h3r = [[r_mix[2 * j], r_mix[2 * j + 1]] for j in range(TPG)]
                for j in range(TPG):
                    i = g * TPG + j
                    A(lambda e: e.activation(out=h3r[j], in_=xt[j][:], func=AF.Copy, scale=rstd[:, j:j + 1]),
                      r=[r_xt[j], r_rstd], w=r_h3r[j])
                    tk.dma("sp", "x2st%d" % j, lambda e: e.dma_start(out=x2s_d[i * 128:(i + 1) * 128, :], in_=xt[j][:]), r=[r_xt[j]], w=[r_x2s[i]])
                    pq, r_pq = (pB, r_pB) if j % 2 == 0 else (pC, r_pC)
                    mm_transpose(pq, lambda b: h3r[j][:, b * 128:(b + 1) * 128], 8, r_h3r[j], r_pq)
                    V(lambda e: e.tensor_tensor(out=h3T[:], in0=pq.rearrange("p (k t) -> p k t", k=8),
                                                in1=ln3w.unsqueeze(2).broadcast_to([128, 8, 128]), op=ALU.mult), r=r_pq + [r_vecs], w=[r_h3T])
                    PE([(lambda e, kc=kc: e.matmul(pD[:, j * 64:j * 64 + 36], lhsT=h3T[:, kc, :], rhs=wr[:, kc, :], start=(kc == 0), stop=(kc == 7)))
                        for kc in range(8)], r=[r_h3T, r_wr], w=[r_pD])
                pDl = pD[:, 0:256].rearrange("p (j c) -> p j c", j=4)[:, :, 0:36]
                V(lambda e: e.tensor_tensor(out=lg4, in0=pDl, in1=rb_bc[:].unsqueeze(1).broadcast_to([128, 4, 36]), op=ALU.add),
                  r=[r_pD, r_rb], w=R)
                lgG = lg4[:, :, 0:4]
                lgE = lg4[:, :, 4:36].rearrange("p j (g e) -> p j g e", g=4)
                V(lambda e: e.tensor_reduce(out=gmax4, in_=lgG, axis=AX.X, op=ALU.max), r=R, w=R)
                V(lambda e: e.tensor_tensor(out=gsh4, in0=lgG, in1=gmax4.unsqueeze(2).broadcast_to([128, 4, 4]), op=ALU.subtract), r=R, w=R)
                A(lambda e: e.activation(out=gex4, in_=gsh4, func=AF.Exp), r=R, w=R)
                V(lambda e: e.tensor_reduce(out=gsum4, in_=gex4, axis=AX.X, op=ALU.add), r=R, w=R)
                V(lambda e: e.reciprocal(out=gp4, in_=gsum4), r=R, w=R)
                V(lambda e: e.tensor_tensor(out=goh4, in0=lgG, in1=gmax4.unsqueeze(2).broadcast_to([128, 4, 4]), op=ALU.is_ge), r=R, w=R)
                V(lambda e: e.tensor_tensor(out=prod4, in0=lgE, in1=goh4.unsqueeze(3).broadcast_to([128, 4, 4, 8]), op=ALU.mult), r=R, w=R)
                V(lambda e: e.tensor_reduce(out=el4, in_=prod4.rearrange("p j g e -> p j e g"), axis=AX.X, op=ALU.add), r=R, w=R)
                V(lambda e: e.tensor_reduce(out=m1_4, in_=el4, axis=AX.X, op=ALU.max), r=R, w=R)
                V(lambda e: e.tensor_tensor(out=oh1_4, in0=el4, in1=m1_4.unsqueeze(2).broadcast_to([128, 4, 8]), op=ALU.is_ge), r=R, w=R)
                V(lambda e: e.scalar_tensor_tensor(out=el2_4, in0=oh1_4, scalar=-1e30, in1=el4, op0=ALU.mult, op1=ALU.add), r=R, w=R)
                V(lambda e: e.tensor_reduce(out=m2_4, in_=el2_4, axis=AX.X, op=ALU.max), r=R, w=R)
                V(lambda e: e.tensor_tensor(out=oh2_4, in0=el2_4, in1=m2_4.unsqueeze(2).broadcast_to([128, 4, 8]), op=ALU.is_ge), r=R, w=R)
                V(lambda e: e.tensor_tensor(out=dd4, in0=m2_4, in1=m1_4, op=ALU.subtract), r=R, w=R)
                A(lambda e: e.activation(out=edd4, in_=dd4, func=AF.Exp), r=R, w=R)
                V(lambda e: e.tensor_scalar(out=w1_4, in0=edd4, scalar1=1.0, scalar2=None, op0=ALU.add), r=R, w=R)
                V(lambda e: e.reciprocal(out=w1_4, in_=w1_4), r=R, w=R)
                V(lambda e: e.tensor_tensor(out=w2_4, in0=edd4, in1=w1_4, op=ALU.mult), r=R, w=R)
                cwv = cw_all[:, 8 * g:8 * g + 8].rearrange("p (j k) -> p j k", k=2)
                r_cwg = [r_cw[g * TPG + j] for j in range(TPG)]
                V(lambda e: e.tensor_tensor(out=cwv[:, :, 0], in0=w1_4, in1=gp4, op=ALU.mult), r=R, w=r_cwg)
                V(lambda e: e.tensor_tensor(out=cwv[:, :, 1], in0=w2_4, in1=gp4, op=ALU.mult), r=R + r_cwg, w=r_cwg)
                gohb = goh4.unsqueeze(3).broadcast_to([128, 4, 4, 8])
                V(lambda e: e.tensor_tensor(out=A1_4.rearrange("p j (g e) -> p j g e", g=4), in0=gohb,
                                            in1=oh1_4.unsqueeze(2).broadcast_to([128, 4, 4, 8]), op=ALU.mult), r=R, w=R)
                V(lambda e: e.tensor_tensor(out=A2_4.rearrange("p j (g e) -> p j g e", g=4), in0=gohb,
                                            in1=oh2_4.unsqueeze(2).broadcast_to([128, 4, 4, 8]), op=ALU.mult), r=R, w=R)
                V(lambda e: e.tensor_tensor(out=Ab4[:], in0=A1_4, in1=A2_4, op=ALU.add), r=R, w=[r_Ab])
                for j in range(TPG):
                    fns = [lambda e: e.matmul(pD[:, 256 + j * 32:256 + (j + 1) * 32], lhsT=tri_bf[:], rhs=Ab4[:, j, :], start=True, stop=(j == 0))]
                    for jp in range(j):
                        fns.append(lambda e, jp=jp: e.matmul(pD[:, 256 + j * 32:256 + (j + 1) * 32], lhsT=ones_bf[:], rhs=Ab4[:, jp, :],
                                                             start=False, stop=(jp == j - 1)))
                    PE(fns, r=[r_tri, r_ones, r_Ab], w=[r_pD])
                PE([(lambda e, j=j: e.matmul(pD[:, 384:416], lhsT=ones_bf[:], rhs=Ab4[:, j, :], start=(j == 0), stop=(j == TPG - 1)))
                    for j in range(TPG)], r=[r_ones, r_Ab], w=[r_pD])
                V(lambda e: e.tensor_tensor(out=posf4, in0=pD[:, 256:384].rearrange("p (j e) -> p j e", j=4),
                                            in1=cnt[:].unsqueeze(1).broadcast_to([128, 4, NE]), op=ALU.add), r=[r_pD, r_cnt], w=R)
                V(lambda e: e.tensor_tensor(out=cnt[:], in0=pD[:, 384:416], in1=cnt[:], op=ALU.add), r=[r_pD, r_cnt], w=[r_cnt])
                eoffb = eoff[:].unsqueeze(1).broadcast_to([128, 4, NE])
                for kk, Af in enumerate((A1_4, A2_4)):
                    V(lambda e: e.tensor_tensor(out=prodE, in0=Af, in1=posf4, op=ALU.mult), r=R, w=R)
                    V(lambda e: e.tensor_reduce(out=pk4[:, :, kk], in_=prodE, axis=AX.X, op=ALU.add), r=R, w=R)
                    V(lambda e: e.tensor_tensor(out=prodE, in0=Af, in1=eoffb, op=ALU.mult), r=R + [r_eoff], w=R)
                    V(lambda e: e.tensor_reduce(out=ek4[:, :, kk], in_=prodE, axis=AX.X, op=ALU.add), r=R, w=R)
                V(lambda e: e.tensor_scalar(out=ok4, in0=pk4, scalar1=float(CAP), scalar2=None, op0=ALU.is_lt), r=R, w=R)
                V(lambda e: e.tensor_tensor(out=sd4, in0=pk4, in1=ek4, op=ALU.add), r=R, w=R)
                V(lambda e: e.scalar_tensor_tensor(out=sd4, in0=sd4, scalar=-float(TRASH), in1=ok4, op0=ALU.add, op1=ALU.mult), r=R, w=R)
                V(lambda e: e.tensor_scalar(out=sd4, in0=sd4, scalar1=float(TRASH), scalar2=None, op0=ALU.add), r=R, w=R)
                r_dg = [r_dest[g * TPG + j] for j in range(TPG)]
                V(lambda e: e.tensor_copy(out=dest_all[:, 8 * g:8 * g + 8], in_=sd4.rearrange("p j k -> p (j k)")), r=R, w=r_dg)
                def do_scatter():
                    for j in range(TPG):
                        i = g * TPG + j
                        for kk in range(2):
                            tk.dma("pool", "sc%d" % j, lambda e: e.indirect_dma_start(
                                out=xs_d, out_offset=bass.IndirectOffsetOnAxis(ap=dest_all[:, 2 * i + kk:2 * i + kk + 1], axis=0),
                                in_=h3r[j], in_offset=None), r=r_h3r[j] + [r_dest[i], r_xsz], w=[Res()])
                return do_scatter

            pending = None
            for g in range(NG):
                par = g % 2
                load_x(g)
                for j in range(TPG):
                    norm_stats(j)
                rstd_op(ssq[:], 4, D, stmp[:, 0:4], rstd[:], [r_ssq], r_stmp, r_rstd)
                if pending is not None:
                    pending()
                for j in range(TPG):
                    norm_to_hT(j, ln1w)
                rnn_all = []
                for c_ in range(4):
                    rnn_all.extend(rnn_steps(g, c_))

                def pump(n):
                    for _ in range(n):
                        if rnn_all:
                            rnn_all.pop(0)()

                GP(lambda e: e.memset(mix[:, 0:8, :].rearrange("p a t -> p (a t)"), 0.0), w=r_mix[0:8])
                for c in range(4):
                    proj_fm(w_in, r_w_in, 1024 + c * 128, pA[:, (c % 2) * 512:(c % 2 + 1) * 512], r_pA[c % 2])
                    for hh_ in range(2):
                        A(lambda e: e.activation(out=qT[hh_ * 64:(hh_ + 1) * 64, 2 * c + hh_, :],
                                                 in_=pA[hh_ * 64:(hh_ + 1) * 64, (c % 2) * 512:(c % 2 + 1) * 512], func=AF.Copy, scale=0.125),
                          r=[r_pA[c % 2]], w=[r_qT[2 * c + hh_]])
                    pump(3)
                for c in range(4):
                    proj_fm(w_in, r_w_in, 1536 + c * 128, pA[:, (c % 2) * 512:(c % 2 + 1) * 512], r_pA[c % 2])
                    A(lambda e: e.activation(out=kring[:, c, par * 512:(par + 1) * 512], in_=pA[:, (c % 2) * 512:(c % 2 + 1) * 512],
                                             func=AF.Copy), r=[r_pA[c % 2]], w=[r_kring[par][c]])
                    pump(3)
                for j in range(TPG):
                    slot = (g * TPG + j) % 8
                    PE([(lambda e, kc=kc: e.matmul(pC[:, (j % 2) * 512:(j % 2 + 1) * 512], lhsT=hT[:, kc, j * 128:(j + 1) * 128],
                                                   rhs=w_in[:, kc, 2048:2560], start=(kc == 0), stop=(kc == 7))) for kc in range(8)],
                       r=r_w_in + [r_hT[j]], w=[r_pC[j % 2]])
                    V(lambda e: e.tensor_copy(out=vring[:, slot, :, 0:64],
                                              in_=pC[:, (j % 2) * 512:(j % 2 + 1) * 512].rearrange("p (h d) -> p h d", h=8)),
                      r=[r_pC[j % 2]], w=[r_vring[slot]])
                    pump(3)
                for j in range(TPG):
                    att_tile(g, j, rnn_all)
                V(lambda e: e.tensor_copy(out=stmp[:, 0:4], in_=ssqr[:]), r=[r_ssqr], w=[r_stmp])
                V(lambda e: e.tensor_copy(out=stmp[:, 4:8], in_=ssqa[:]), r=[r_ssqa], w=[r_stmp])
                V(lambda e: e.tensor_scalar(out=stmp[:], in0=stmp[:], scalar1=1.0 / 512, scalar2=EPS, op0=ALU.mult, op1=ALU.add),
                  r=[r_stmp], w=[r_stmp])
                GP(lambda e: e.tensor_tensor(out=rstdg[:], in0=stmp[:], in1=neg05[:], op=ALU.pow), r=[r_stmp, r_neg], w=[r_rstdg])
                for j in range(TPG):
                    out_proj(j)
                    norm_stats(j)
                rstd_op(ssq[:], 4, D, stmp[:, 0:4], rstd[:], [r_ssq], r_stmp, r_rstd)
                for j in range(TPG):
                    norm_to_hT(j, ln2w)
                for fo in range(8):
                    proj_fm(wq, [r_wq], fo * 128, pA[:, (fo % 2) * 512:(fo % 2 + 1) * 512], r_pA[fo % 2])
                    A(lambda e: e.activation(out=qxT[:, fo, :], in_=pA[:, (fo % 2) * 512:(fo % 2 + 1) * 512], func=AF.Copy),
                      r=[r_pA[fo % 2]], w=[r_qxT[fo]])
                cross_attn()
                for j in range(TPG):
                    wo_proj(j)
                    norm_stats(j)
                rstd_op(ssq[:], 4, D, stmp[:, 0:4], rstd[:], [r_ssq], r_stmp, r_rstd)
                pending = route_group(g)
            pending()
            tk.barrier()

        with ExitStack() as sbk:
            NB = 3
            wg = [sb("wg%d" % k, [128, 8, 512], BF16, sbk) for k in range(NB)]
            wu = [sb("wu%d" % k, [128, 8, 512], BF16, sbk) for k in range(NB)]
            wd = [sb("wd%d" % k, [128, 4, D], BF16, sbk) for k in range(NB)]
            r_wg = [Res() for _ in range(NB)]; r_wu = [Res() for _ in range(NB)]; r_wd = [Res() for _ in range(NB)]
            xsl = [sb("xsl%d" % k, [128, CAP // 128, D], BF16, sbk) for k in range(NB)]
            r_xsl = [Res() for _ in range(NB)]
            xsT = sb("xsT", [128, 8, CAP], BF16, sbk); r_xsT = Res()
            sg = [sb("sg%d" % k, [128, CAP], F32, sbk) for k in range(2)]
            r_sg = [Res(), Res()]
            aT = sb("aT", [128, 4, CAP], BF16, sbk)
            r_aT = [Res() for _ in range(4)]
            yb = [sb("yb%d" % k, [128, D], F32, sbk) for k in range(2)]
            r_yb = [Res(), Res()]

            def load_expert(e_):
                k = e_ % NB
                tk.dma("pool", "ewg%d" % k, lambda e: e.dma_start(out=wg[k][:], in_=eg_d[e_].rearrange("(k p) n -> p k n", p=128)), w=[r_wg[k]])
                tk.dma("pool", "ewu%d" % k, lambda e: e.dma_start(out=wu[k][:], in_=eu_d[e_].rearrange("(k p) n -> p k n", p=128)), w=[r_wu[k]])
                tk.dma("pool", "ewd%d" % k, lambda e: e.dma_start(out=wd[k][:], in_=ed_d[e_].rearrange("(k p) n -> p k n", p=128)), w=[r_wd[k]])
                tk.dma("sp", "xsl%d" % k, lambda e: e.dma_start(out=xsl[k][:], in_=xs_d[e_ * CAP:(e_ + 1) * CAP, :].rearrange("(t p) d -> p t d", p=128)),
                       w=[r_xsl[k]])

            xsT2 = [xsT, sb("xsT_b", [128, 8, CAP], BF16, sbk)]
            r_xsT2 = [r_xsT, Res()]

            def tr_steps(e_):
                k = e_ % NB
                xo, r_xo = xsT2[e_ % 2], r_xsT2[e_ % 2]
                st_ = []
                n = 0
                for t in range(CAP // 128):
                    for hf in range(2):
                        pq, r_pq = (pD, r_pD) if n % 2 == 0 else (pE, r_pE)
                        n += 1
                        st_.append(lambda t=t, hf=hf, pq=pq, r_pq=r_pq: mm_transpose(
                            pq, lambda b: xsl[k][:, t, (hf * 4 + b) * 128:(hf * 4 + b + 1) * 128], 4, [r_xsl[k]], [r_pq]))
                        st_.append(lambda t=t, hf=hf, pq=pq, r_pq=r_pq: V(lambda e: e.tensor_tensor(
                            out=xo[:, hf * 4:(hf + 1) * 4, t * 128:(t + 1) * 128], in0=pq.rearrange("p (k t) -> p k t", k=4),
                            in1=ln3w[:, hf * 4:(hf + 1) * 4].unsqueeze(2).broadcast_to([128, 4, 128]), op=ALU.mult),
                            r=[r_pq, r_vecs], w=[r_xo]))
                return st_

            def main_steps(e_):
                k = e_ % NB
                xo, r_xo = xsT2[e_ % 2], r_xsT2[e_ % 2]
                st_ = []
                for fc in range(4):
                    b2 = fc % 2
                    st_.append(lambda fc=fc, b2=b2: PE([(lambda e, kc=kc: e.matmul(pA[:, b2 * 512:b2 * 512 + CAP], lhsT=wg[k][:, kc, fc * 128:(fc + 1) * 128],
                                                                                 rhs=xo[:, kc, :], start=(kc == 0), stop=(kc == 7))) for kc in range(8)],
                                                       r=[r_wg[k], r_xo], w=[r_pA[b2]]))
                    st_.append(lambda fc=fc, b2=b2: PE([(lambda e, kc=kc: e.matmul(pB[:, b2 * 512:b2 * 512 + CAP], lhsT=wu[k][:, kc, fc * 128:(fc + 1) * 128],
                                                                                 rhs=xo[:, kc, :], start=(kc == 0), stop=(kc == 7))) for kc in range(8)],
                                                       r=[r_wu[k], r_xo], w=[r_pB[b2]]))
                    st_.append(lambda b2=b2: A(lambda e: e.activation(out=sg[b2][:], in_=pA[:, b2 * 512:b2 * 512 + CAP], func=AF.Silu),
                                               r=[r_pA[b2]], w=[r_sg[b2]]))
                    st_.append(lambda fc=fc, b2=b2: V(lambda e: e.tensor_tensor(out=aT[:, fc, :], in0=pB[:, b2 * 512:b2 * 512 + CAP], in1=sg[b2][:], op=ALU.mult),
                                                      r=[r_pB[b2], r_sg[b2]], w=[r_aT[fc]]))
                for t in range(CAP // 128):
                    yk = (e_ * (CAP // 128) + t) % 2
                    for half in range(2):
                        st_.append(lambda t=t, half=half: PE([(lambda e, fc=fc: e.matmul(pC[:, half * 512:(half + 1) * 512], lhsT=aT[:, fc, t * 128:(t + 1) * 128],
                                                                                         rhs=wd[k][:, fc, half * 512:(half + 1) * 512], start=(fc == 0), stop=(fc == 3)))
                                                              for fc in range(4)], r=r_aT + [r_wd[k]], w=[r_pC[half]]))
                    st_.append(lambda yk=yk: A(lambda e: e.activation(out=yb[yk][:, 0:512], in_=pC[:, 0:512], func=AF.Copy), r=[r_pC[0], r_yb[yk]], w=[r_yb[yk]]))
                    st_.append(lambda yk=yk: V(lambda e: e.tensor_copy(out=yb[yk][:, 512:1024], in_=pC[:, 512:1024]), r=[r_pC[1], r_yb[yk]], w=[r_yb[yk]]))
                    row0 = e_ * CAP + t * 128
                    st_.append(lambda yk=yk, row0=row0: tk.dma("sp", "ys%d" % yk, lambda e: e.dma_start(out=ys_d[row0:row0 + 128, :], in_=yb[yk][:]),
                                                               r=[r_yb[yk]], w=[Res()]))
                return st_

            load_expert(0)
            load_expert(1)
            for f_ in tr_steps(0):
                f_()
            for e_ in range(NE):
                if e_ + 2 < NE:
                    load_expert(e_ + 2)
                ms_ = main_steps(e_)
                ts_ = tr_steps(e_ + 1) if e_ + 1 < NE else []
                while ms_ or ts_:
                    for _ in range(2):
                        if ms_:
                            ms_.pop(0)()
                    if ts_:
                        ts_.pop(0)()
            tk.barrier()

        with ExitStack() as sc:
            NBC = 4
            x2t = [sb("x2t%d" % k, [128, D], F32, sc) for k in range(NBC)]
            y1t = [sb("y1t%d" % k, [128, D], F32, sc) for k in range(NBC)]
            y2t = [sb("y2t%d" % k, [128, D], F32, sc) for k in range(NBC)]
            r_x2t = [Res() for _ in range(NBC)]; r_y1t = [Res() for _ in range(NBC)]; r_y2t = [Res() for _ in range(NBC)]
            junkc = [sb("junkc%d" % k, [128, D], BF16, sc) for k in range(2)]; r_junkc = [Res(), Res()]
            fin_bc = sb("fin_bc", [128, D], F32, sc); r_fin = Res()
            tk.dma("sp", "fin", lambda e: e.dma_start(out=fin_bc[:], in_=fin_d.partition_broadcast(128)), w=[r_fin])
            fss = sb("fss", [128, NBC], F32, sc); r_fss = [Res() for _ in range(NBC)]
            ftmp = sb("ftmp", [128, NBC], F32, sc); r_ftmp = [Res() for _ in range(NBC)]
            frs = sb("frs", [128, NBC], F32, sc); r_frs = [Res() for _ in range(NBC)]

            def load_c(i):
                k = i % NBC
                tk.dma("sp", "cx%d" % k, lambda e: e.dma_start(out=x2t[k][:], in_=x2s_d[i * 128:(i + 1) * 128, :]), r=[r_x2s[i]], w=[r_x2t[k]])
                tk.dma("pool", "cy1%d" % k, lambda e: e.indirect_dma_start(
                    out=y1t[k][:], out_offset=None, in_=ys_d, in_offset=bass.IndirectOffsetOnAxis(ap=dest_all[:, 2 * i:2 * i + 1], axis=0)),
                    r=[r_dest[i]], w=[r_y1t[k]])
                tk.dma("pool", "cy2%d" % k, lambda e: e.indirect_dma_start(
                    out=y2t[k][:], out_offset=None, in_=ys_d, in_offset=bass.IndirectOffsetOnAxis(ap=dest_all[:, 2 * i + 1:2 * i + 2], axis=0)),
                    r=[r_dest[i]], w=[r_y2t[k]])

            def c_steps(i):
                k = i % NBC
                jk = i % 2
                st_ = []
                st_.append(lambda: V(lambda e: e.scalar_tensor_tensor(out=x2t[k][:], in0=y1t[k][:], scalar=cw_all[:, 2 * i:2 * i + 1], in1=x2t[k][:],
                                                                      op0=ALU.mult, op1=ALU.add), r=[r_y1t[k], r_cw[i], r_x2t[k]], w=[r_x2t[k]]))
                st_.append(lambda: V(lambda e: e.scalar_tensor_tensor(out=x2t[k][:], in0=y2t[k][:], scalar=cw_all[:, 2 * i + 1:2 * i + 2], in1=x2t[k][:],
                                                                      op0=ALU.mult, op1=ALU.add), r=[r_y2t[k], r_cw[i], r_x2t[k]], w=[r_x2t[k]]))
                st_.append(lambda: A(lambda e: e.activation(out=junkc[jk][:], in_=x2t[k][:], func=AF.Square, accum_out=fss[:, k:k + 1]),
                                     r=[r_x2t[k]], w=[r_junkc[jk], r_fss[k]]))
                st_.append(lambda: V(lambda e: e.tensor_scalar(out=ftmp[:, k:k + 1], in0=fss[:, k:k + 1], scalar1=1.0 / D, scalar2=EPS,
                                                               op0=ALU.mult, op1=ALU.add), r=[r_fss[k]], w=[r_ftmp[k]]))
                st_.append(lambda: GP(lambda e: e.tensor_tensor(out=frs[:, k:k + 1], in0=ftmp[:, k:k + 1], in1=neg05[:, 0:1], op=ALU.pow),
                                      r=[r_ftmp[k], r_neg], w=[r_frs[k]]))
                st_.append(lambda: A(lambda e: e.activation(out=y2t[k][:], in_=x2t[k][:], func=AF.Copy, scale=frs[:, k:k + 1]),
                                     r=[r_x2t[k], r_frs[k]], w=[r_y2t[k]]))
                st_.append(lambda: V(lambda e: e.tensor_tensor(out=y1t[k][:], in0=y2t[k][:], in1=fin_bc[:], op=ALU.mult),
                                     r=[r_y2t[k], r_fin], w=[r_y1t[k]]))
                st_.append(lambda: tk.dma("sp", "out%d" % k, lambda e: e.dma_start(out=out_d[i * 128:(i + 1) * 128, :], in_=y1t[k][:]), r=[r_y1t[k]]))
                return st_

            load_c(0)
            load_c(1)
            for i in range(0, NT, 2):
                for t_ in (i + 2, i + 3):
                    if t_ < NT:
                        load_c(t_)
                a_ = c_steps(i)
                b_ = c_steps(i + 1)
                while a_ or b_:
                    if a_:
                        a_.pop(0)()
                    if b_:
                        b_.pop(0)()
            for k in range(NBC):
                ds = tk.dsem("out%d" % k)
                nc.sync.wait_ge(ds.sem, ds.cnt)
    return nc


def _prep_shared(inp):
    f = np.float32
    vecs = np.zeros((128, NV), f)

    def pc(v, n):
        return np.ascontiguousarray(np.asarray(v, f).reshape(n, 128).T)

    vecs[:, 0:8] = pc(inp["ln1_w"][0], 8)
    vecs[:, 8:16] = pc(inp["ln2_w"][0], 8)
    vecs[:, 16:20] = pc(inp["gn_rnn_w"][0], 4)
    vecs[:, 20:24] = pc(inp["gn_att_w"][0], 4)
    vecs[:, 24:28] = pc(inp["conv_b"][0], 4)
    vecs[:, 28:32] = pc(inp["rnn_ba"][0], 4)
    vecs[:, 32:36] = pc(inp["rnn_bx"][0], 4)
    vecs[:, 36:40] = pc(inp["rnn_lambda"][0], 4)
    vecs[:, 56:64] = pc(inp["ln3_w"][0], 8)
    cwt = np.asarray(inp["conv_w"][0], f)
    for c in range(4):
        for j in range(4):
            vecs[:, 40 + c * 4 + j] = cwt[j, c * 128:(c + 1) * 128]
    bd = np.zeros((128, 2, 4, 128), f)
    for gi, name in enumerate(("rnn_wa", "rnn_wx")):
        w = np.asarray(inp[name][0], f)
        for c in range(4):
            for b in range(2):
                bd[b * 64:(b + 1) * 64, gi, c, b * 64:(b + 1) * 64] = w[2 * c + b]
    rb = np.asarray(inp["rel_bias"][0], f)
    kk = np.arange(128)[:, None]
    qq = np.arange(640)[None, :]
    idx = np.clip(qq - kk, -128, 128) + 128
    valid = np.where(kk < 64, (qq < 576), (qq >= 64))
    bm = np.empty((128, 8, 640), f)
    for h in range(8):
        bm[:, h, :] = np.where(valid, rb[h][idx], f(-30000.0))
    wr = np.concatenate([np.asarray(inp["router_group_w"][0], f),
                         np.asarray(inp["router_expert_w"][0], f).transpose(1, 0, 2).reshape(D, 32)], axis=1)
    rbias = np.concatenate([np.asarray(inp["router_group_b"][0], f), np.asarray(inp["router_expert_b"][0], f).reshape(32)])
    shared = {
        "vecs": vecs, "bm": bm, "bd": bd, "wr": np.ascontiguousarray(wr), "rb": np.ascontiguousarray(rbias),
        "w_in": np.ascontiguousarray(inp["w_in"][0], f), "w_out": np.ascontiguousarray(inp["w_out"][0], f),
        "wq": np.ascontiguousarray(inp["xq_w"][0], f), "wk": np.ascontiguousarray(inp["xk_w"][0], f),
        "wv": np.ascontiguousarray(inp["xv_w"][0], f), "wo": np.ascontiguousarray(inp["xo_w"][0], f),
        "memw": np.ascontiguousarray(inp["mem_norm_w"], f),
        "fin": np.ascontiguousarray(inp["final_norm_w"], f),
        "eg": np.ascontiguousarray(inp["expert_gate_w"][0], f), "eu": np.ascontiguousarray(inp["expert_up_w"][0], f),
        "ed": np.ascontiguousarray(inp["expert_down_w"][0], f),
    }
    return shared


def kernel(**inputs):
    inp = {k: np.asarray(v) for k, v in inputs.items()}
    shared = _prep_shared(inp)
    nc = build_nc()
    in_maps = []
    for b in range(8):
        m = dict(shared)
        m["x"] = np.ascontiguousarray(inp["x"][b], np.float32)
        m["mem"] = np.ascontiguousarray(inp["mem"][b], np.float32)
        in_maps.append(m)
    res = run_bass_kernel_spmd(nc, in_maps, core_ids=list(range(8)))
    return np.stack([np.asarray(r["out"], np.float32) for r in res.results], axis=0)
```

```python
import numpy as np
from contextlib import ExitStack

import concourse.bass as bass
import concourse.mybir as mybir
from concourse.bass_utils import run_bass_kernel_spmd

F32 = mybir.dt.float32
BF16 = mybir.dt.bfloat16
I32 = mybir.dt.int32
AF = mybir.ActivationFunctionType
ALU = mybir.AluOpType
AX = mybir.AxisListType

S = 4096
D = 1024
NT = 32
GT = 512
NG = 8
TPG = 4
NE = 32
CAP = 512
NSLOT = NE * CAP
NROWS = NSLOT + 1024
TRASH = NSLOT
EPS = 1e-6
NV = 64
GELU_K = 0.7978845608028654


class Res:
    __slots__ = ("w", "r")

    def __init__(self):
        self.w = {}
        self.r = {}


class DSem:
    def __init__(self, sem):
        self.sem = sem
        self.cnt = 0


class TK:
    def __init__(self, nc, stack):
        self.nc = nc
        self.stack = stack
        self.eng = {"pe": nc.tensor, "dve": nc.vector, "act": nc.scalar, "pool": nc.gpsimd, "sp": nc.sync}
        self.esem = {}
        self.ecnt = {}
        self.seen = {}
        for k in self.eng:
            self.esem[k] = stack.enter_context(nc.semaphore("es_" + k))
            self.ecnt[k] = 0
            self.seen[k] = {}
        self.dsems = []
        self.dsd = {}

    def dsem(self, name):
        if name not in self.dsd:
            d = DSem(self.stack.enter_context(self.nc.semaphore("m_" + name)))
            self.dsems.append(d)
            self.dsd[name] = d
        return self.dsd[name]

    @staticmethod
    def _merge(d, tokd):
        for k, (s, v) in tokd.items():
            if v is None or k not in d or d[k][1] < v:
                d[k] = (s, v)

    @staticmethod
    def _flat(rs):
        out = []
        for r in rs:
            if isinstance(r, (list, tuple)):
                out.extend(TK._flat(r))
            else:
                out.append(r)
        return out

    def _deps(self, reads, writes):
        d = {}
        for r in self._flat(reads):
            self._merge(d, r.w)
        for w in self._flat(writes):
            self._merge(d, w.w)
            self._merge(d, w.r)
        return d

    def _wait(self, e, deps):
        E = self.eng[e]
        seen = self.seen[e]
        for k, (s, v) in deps.items():
            if e == "pe" and k == "pe":
                continue
            if v is None:
                sem, val = s.sem, s.cnt
            else:
                sem, val = s, v
            if seen.get(k, 0) < val:
                E.wait_ge(sem, val)
                seen[k] = val

    @staticmethod
    def _update(reads, writes, key, tok):
        for r in TK._flat(reads):
            r.r[key] = tok
        for w in TK._flat(writes):
            w.w = {key: tok}
            w.r = {}

    def op(self, e, fn, r=(), w=()):
        self._wait(e, self._deps(r, w))
        ins = fn(self.eng[e])
        self.ecnt[e] += 1
        ins.then_inc(self.esem[e], 1)
        self._update(r, w, e, (self.esem[e], self.ecnt[e]))

    def ops(self, e, fns, r=(), w=()):
        self._wait(e, self._deps(r, w))
        ins = None
        for fn in fns:
            ins = fn(self.eng[e])
        self.ecnt[e] += 1
        ins.then_inc(self.esem[e], 1)
        self._update(r, w, e, (self.esem[e], self.ecnt[e]))

    def dma(self, q, name, fn, r=(), w=()):
        ds = self.dsem(name)
        self._wait(q, self._deps(r, w))
        ins = fn(self.eng[q])
        ds.cnt += 16
        ins.then_inc(ds.sem, 16)
        self._update(r, w, "d_" + name, (ds, None))

    def barrier(self):
        d = {}
        for k in self.eng:
            if self.ecnt[k] > 0:
                d[k] = (self.esem[k], self.ecnt[k])
        for name, ds in self.dsd.items():
            if ds.cnt > 0:
                d["d_" + name] = (ds.sem, ds.cnt)
        for e in self.eng:
            E = self.eng[e]
            seen = self.seen[e]
            for k, (s, v) in d.items():
                if k == e:
                    continue
                if seen.get(k, 0) < v:
                    E.wait_ge(s, v)
                    seen[k] = v


def build_nc():
    nc = bass.Bass("TRN2", target_bir_lowering=False)

    def din(name, shape, dt=F32):
        return nc.dram_tensor(name, shape, dt, kind="ExternalInput").ap()

    x_d = din("x", [S, D])
    mem_d = din("mem", [256, D])
    vecs_d = din("vecs", [128, NV])
    bm_d = din("bm", [128, 8, 640])
    bd_d = din("bd", [128, 2, 4, 128])
    wr_d = din("wr", [D, 36])
    rb_d = din("rb", [36])
    w_in_d = din("w_in", [D, 2560])
    w_out_d = din("w_out", [D, D])
    wq_d = din("wq", [D, D])
    wk_d = din("wk", [D, D])
    wv_d = din("wv", [D, D])
    wo_d = din("wo", [D, D])
    memw_d = din("memw", [D])
    fin_d = din("fin", [D])
    eg_d = din("eg", [NE, D, 512])
    eu_d = din("eu", [NE, D, 512])
    ed_d = din("ed", [NE, 512, D])
    out_d = nc.dram_tensor("out", [S, D], F32, kind="ExternalOutput").ap()
    x2s_d = nc.dram_tensor("x2s", [S, D], F32, kind="Internal").ap()
    xs_d = nc.dram_tensor("xs", [NROWS, D], BF16, kind="Internal").ap()
    ys_d = nc.dram_tensor("ys", [NSLOT + 1, D], F32, kind="Internal").ap()

    with ExitStack() as st:
        tk = TK(nc, st)

        def sb(name, shape, dt, stack=st):
            return stack.enter_context(nc.sbuf_tensor("s_" + name, shape, dt))

        def psum(name, shape, dt):
            return st.enter_context(nc.psum_tensor(name, shape, dt))

        def V(fn, r=(), w=()):
            tk.op("dve", fn, r, w)

        def A(fn, r=(), w=()):
            tk.op("act", fn, r, w)

        def GP(fn, r=(), w=()):
            tk.op("pool", fn, r, w)

        def PE(fns, r=(), w=()):
            tk.ops("pe", fns, r, w)

        pall = psum("pall", [128, 8 * 512], F32)
        pA = pall[:, 0:1024]
        pB = pall[:, 1024:2048]
        pC = pall[:, 2048:3072]
        pD = pall[:, 3072:3584]
        pE = pall[:, 3584:4096]
        r_pE = Res()
        r_pA = [Res(), Res()]
        r_pB = [Res(), Res()]
        r_pC = [Res(), Res()]
        r_pD = Res()
        SX = [pA[:, 0:640], pB[:, 0:640]]
        r_SX = [r_pA, r_pB]

        identf = sb("identf", [128, 128], F32)
        ident = sb("ident", [128, 128], BF16)
        ones_bf = sb("ones_bf", [128, 128], BF16)
        trif = sb("trif", [128, 128], F32)
        tri_bf = sb("tri_bf", [128, 128], BF16)
        vecs = sb("vecs", [128, NV], F32)
        hb = sb("hb", [128, 8], F32)
        hc = sb("hc", [128, 8], F32)
        spt = sb("spt", [128, 4], F32)
        rb_bc = sb("rb_bc", [128, 36], F32)
        dest_all = sb("dest_all", [128, 2 * NT], I32)
        cw_all = sb("cw_all", [128, 2 * NT], F32)
        cnt = sb("cnt", [128, NE], F32)
        eoff_i = sb("eoff_i", [128, NE], I32)
        eoff = sb("eoff", [128, NE], F32)
        neg05 = sb("neg05", [128, 8], F32)
        qtr = sb("qtr", [128, 1], F32)
        hstate = sb("hstate", [128, 4], F32)
        halo = sb("halo", [128, 4, 3], F32)
        zt = sb("zt", [128, 8], F32)
        r_ident = Res(); r_identf = Res(); r_ones = Res(); r_trif = Res(); r_tri = Res()
        r_vecs = Res(); r_hb = Res(); r_hc = Res(); r_spt = Res()
        r_rb = Res()
        r_dest = [Res() for _ in range(NT)]
        r_cw = [Res() for _ in range(NT)]
        r_cnt = Res(); r_eoffi = Res(); r_eoff = Res(); r_neg = Res()
        r_hstate = [Res() for _ in range(4)]
        r_halo = [Res() for _ in range(4)]
        r_zrow = Res()
        r_x2s = [Res() for _ in range(NT)]
        r_xs = Res()
        r_ys = Res()

        ln1w = vecs[:, 0:8]
        ln2w = vecs[:, 8:16]
        gnrw = vecs[:, 16:20]
        gnaw = vecs[:, 20:24]
        convb = vecs[:, 24:28]
        lam = vecs[:, 36:40]
        ln3w = vecs[:, 56:64]

        tk.dma("sp", "vecs", lambda e: e.dma_start(out=vecs[:], in_=vecs_d), w=[r_vecs])
        tk.dma("sp", "rb", lambda e: e.dma_start(out=rb_bc[:], in_=rb_d.partition_broadcast(128)), w=[r_rb])
        GP(lambda e: e.memset(identf[:], 0.0), w=[r_identf])
        GP(lambda e: e.affine_select(out=identf[:], in_=identf[:], pattern=[[-1, 128]], compare_op=ALU.not_equal,
                                     fill=1.0, base=0, channel_multiplier=1), r=[r_identf], w=[r_identf])
        V(lambda e: e.tensor_copy(out=ident[:], in_=identf[:]), r=[r_identf], w=[r_ident])
        GP(lambda e: e.memset(trif[:], 1.0), w=[r_trif])
        GP(lambda e: e.affine_select(out=trif[:], in_=trif[:], pattern=[[1, 128]], compare_op=ALU.is_gt,
                                     fill=0.0, base=0, channel_multiplier=-1), r=[r_trif], w=[r_trif])
        V(lambda e: e.tensor_copy(out=tri_bf[:], in_=trif[:]), r=[r_trif], w=[r_tri])
        V(lambda e: e.memset(ones_bf[:], 1.0), w=[r_ones])
        GP(lambda e: e.iota(eoff_i[:], pattern=[[CAP, NE]], base=0, channel_multiplier=0), w=[r_eoffi])
        V(lambda e: e.tensor_copy(out=eoff[:], in_=eoff_i[:]), r=[r_eoffi], w=[r_eoff])
        V(lambda e: e.memset(neg05[:], -0.5), w=[r_neg])
        V(lambda e: e.memset(qtr[:], 0.25), w=[r_neg])
        V(lambda e: e.memset(cnt[:], 0.0), w=[r_cnt])
        V(lambda e: e.memset(hstate[:], 0.0), w=r_hstate)
        V(lambda e: e.memset(halo[:], 0.0), w=r_halo)
        V(lambda e: e.memset(zt[:], 0.0), w=[r_zrow])
        tk.dma("sp", "zt", lambda e: e.dma_start(out=ys_d[TRASH].rearrange("(p f) -> p f", p=128), in_=zt[:]), r=[r_zrow], w=[Res()])
        V(lambda e: e.tensor_scalar(out=hb[:], in0=vecs[:, 28:36], scalar1=0.5, scalar2=None, op0=ALU.mult), r=[r_vecs], w=[r_hb])
        A(lambda e: e.activation(out=spt[:], in_=lam, func=AF.Exp, scale=-1.0), r=[r_vecs], w=[r_spt])
        A(lambda e: e.activation(out=spt[:], in_=spt[:], func=AF.Ln, bias=1.0), r=[r_spt], w=[r_spt])
        V(lambda e: e.tensor_scalar(out=hc[:, 0:4], in0=spt[:], scalar1=-4.0, scalar2=None, op0=ALU.mult), r=[r_spt], w=[r_hc])
        V(lambda e: e.tensor_scalar(out=hc[:, 4:8], in0=spt[:], scalar1=-8.0, scalar2=None, op0=ALU.mult), r=[r_spt], w=[r_hc])

        def rstd_op(src_ap, n, dim, tmp_ap, out_ap, r_src, r_tmp, r_out):
            V(lambda e: e.tensor_scalar(out=tmp_ap, in0=src_ap, scalar1=1.0 / dim, scalar2=EPS, op0=ALU.mult, op1=ALU.add),
              r=r_src, w=[r_tmp])
            GP(lambda e: e.tensor_tensor(out=out_ap, in0=tmp_ap, in1=neg05[:, 0:n], op=ALU.pow), r=[r_tmp, r_neg], w=[r_out])

        with ExitStack() as sa:
            w_in = sb("w_in", [128, 8, 2560], BF16, sa)
            w_out = sb("w_out", [128, 8, D], BF16, sa)
            wq = sb("wq", [128, 8, D], BF16, sa)
            wo = sb("wo", [128, 8, D], BF16, sa)
            bd = sb("bd", [128, 2, 4, 128], BF16, sa)
            wr = sb("wr", [128, 8, 36], BF16, sa)
            bm = sb("bm", [128, 8, 640], BF16, sa)
            kTm = sb("kTm", [128, 8, 256], BF16, sa)
            vm = sb("vm", [128, 2, D], BF16, sa)
            kring = sb("kring", [128, 4, 1024], BF16, sa)
            vring = sb("vring", [128, 8, 8, 65], BF16, sa)
            r_w_in = [Res(), Res()]; r_w_out = Res(); r_wq = Res(); r_wo = Res(); r_bd = Res(); r_wr = Res(); r_bm = Res()
            r_kTm = Res(); r_vm = Res()
            r_kring = [[Res() for _ in range(4)] for _ in range(2)]
            r_vring = [Res() for _ in range(8)]

            def wview(dram):
                return dram.rearrange("(k p) n -> p k n", p=128)

            def mm_transpose(dst, src_fn, nblk, r, w):
                PE([(lambda e, b=b: e.matmul(dst[:, b * 128:(b + 1) * 128], lhsT=src_fn(b), rhs=ident[:], start=True, stop=True))
                    for b in range(nblk)], r=list(r) + [r_ident], w=w)


            zt2 = sb("zt2", [128, D], BF16, sa); r_zt2 = Res(); r_xsz = Res()
            V(lambda e: e.memset(zt2[:], 0.0), w=[r_zt2])

            tk.dma("pool", "w_in0", lambda e: e.dma_start(out=w_in[:, :, 0:1280], in_=wview(w_in_d)[:, :, 0:1280]), w=[r_w_in[0]])
            tk.dma("pool", "w_in1", lambda e: e.dma_start(out=w_in[:, :, 1280:2560], in_=wview(w_in_d)[:, :, 1280:2560]), w=[r_w_in[1]])

            with ExitStack() as s0:
                wk = sb("wk", [128, 8, D], BF16, s0)
                wv = sb("wv", [128, 8, D], BF16, s0)
                memt = sb("memt", [128, 2, D], F32, s0)
                memn = sb("memn", [128, 2, D], BF16, s0)
                memT = sb("memT", [128, 8, 256], BF16, s0)
                memw_bc = sb("memw_bc", [128, D], F32, s0)
                junk0 = sb("junk0", [128, D], F32, s0)
                mss = sb("mss", [128, 2], F32, s0)
                mtmp = sb("mtmp", [128, 2], F32, s0)
                mrs = sb("mrs", [128, 2], F32, s0)
                r_wk = Res(); r_wv = Res(); r_memt = Res(); r_memn = Res(); r_memT = Res(); r_memw = Res()
                r_junk0 = Res(); r_mss = Res(); r_mtmp = Res(); r_mrs = Res()
                tk.dma("sp", "memt", lambda e: e.dma_start(out=memt[:], in_=mem_d.rearrange("(t p) d -> p t d", p=128)), w=[r_memt])
                tk.dma("sp", "memw", lambda e: e.dma_start(out=memw_bc[:], in_=memw_d.partition_broadcast(128)), w=[r_memw])
                tk.dma("pool", "wk", lambda e: e.dma_start(out=wk[:], in_=wview(wk_d)), w=[r_wk])
                tk.dma("pool", "wv", lambda e: e.dma_start(out=wv[:], in_=wview(wv_d)), w=[r_wv])
                for t in range(2):
                    A(lambda e: e.activation(out=junk0[:], in_=memt[:, t, :], func=AF.Square, accum_out=mss[:, t:t + 1]),
                      r=[r_memt], w=[r_junk0, r_mss])
                rstd_op(mss[:], 2, D, mtmp[:], mrs[:], [r_mss], r_mtmp, r_mrs)
                for t in range(2):
                    V(lambda e: e.scalar_tensor_tensor(out=memn[:, t, :], in0=memt[:, t, :], scalar=mrs[:, t:t + 1], in1=memw_bc[:],
                                                       op0=ALU.mult, op1=ALU.mult), r=[r_memt, r_mrs, r_memw], w=[r_memn])
                    mm_transpose(pC, lambda b: memn[:, t, b * 128:(b + 1) * 128], 8, [r_memn], r_pC)
                    V(lambda e: e.tensor_copy(out=memT[:, :, t * 128:(t + 1) * 128], in_=pC.rearrange("p (k t) -> p k t", k=8)),
                      r=r_pC, w=[r_memT])
                for fo in range(8):
                    bank = fo % 2
                    PE([(lambda e, kc=kc: e.matmul(pA[:, bank * 512:bank * 512 + 256], lhsT=wk[:, kc, fo * 128:(fo + 1) * 128],
                                                   rhs=memT[:, kc, :], start=(kc == 0), stop=(kc == 7))) for kc in range(8)],
                       r=[r_wk, r_memT], w=[r_pA[bank]])
                    A(lambda e: e.activation(out=kTm[:, fo, :], in_=pA[:, bank * 512:bank * 512 + 256], func=AF.Copy),
                      r=[r_pA[bank]], w=[r_kTm])
                for mt in range(2):
                    for half in range(2):
                        PE([(lambda e, kc=kc: e.matmul(pB[:, half * 512:(half + 1) * 512], lhsT=memT[:, kc, mt * 128:(mt + 1) * 128],
                                                       rhs=wv[:, kc, half * 512:(half + 1) * 512], start=(kc == 0), stop=(kc == 7)))
                            for kc in range(8)], r=[r_wv, r_memT], w=[r_pB[half]])
                        A(lambda e: e.activation(out=vm[:, mt, half * 512:(half + 1) * 512], in_=pB[:, half * 512:(half + 1) * 512],
                                                 func=AF.Copy), r=[r_pB[half]], w=[r_vm])
                tk.barrier()

            tk.dma("pool", "bd", lambda e: e.dma_start(out=bd[:], in_=bd_d), w=[r_bd])
            tk.dma("pool", "bm", lambda e: e.dma_start(out=bm[:], in_=bm_d), w=[r_bm])
            tk.dma("pool", "wr", lambda e: e.dma_start(out=wr[:], in_=wview(wr_d)), w=[r_wr])
            tk.dma("pool", "w_out", lambda e: e.dma_start(out=w_out[:], in_=wview(w_out_d)), w=[r_w_out])
            tk.dma("pool", "wq", lambda e: e.dma_start(out=wq[:], in_=wview(wq_d)), w=[r_wq])
            tk.dma("pool", "wo", lambda e: e.dma_start(out=wo[:], in_=wview(wo_d)), w=[r_wo])
            V(lambda e: e.memset(vring[:].rearrange("p a b c -> p (a b c)"), 1.0), w=r_vring)

            xt = [sb("xt%d" % j, [128, D], F32, sa) for j in range(TPG)]
            r_xt = [Res() for _ in range(TPG)]
            xn = [sb("xn0", [128, D], BF16, sa)] * 2
            r_xn = [Res()] * 2
            junk = xn[0]; r_junk = r_xn[0]
            hT = sb("hT", [128, 8, GT], BF16, sa)
            r_hT = [Res() for _ in range(TPG)]
            mix = sb("mix", [128, 12, GT], BF16, sa)
            r_mix = [Res() for _ in range(12)]
            qT = mix[:, 0:8, :]
            r_qT = r_mix[0:8]
            yrT = mix[:, 8:12, :]
            r_yrT = r_mix[8:12]
            yaT = sb("yaT", [128, 4, GT], BF16, sa)
            r_yaT = [Res() for _ in range(TPG)]
            qxT = mix[:, 0:8, :]
            r_qxT = r_mix[0:8]
            ssq = sb("ssq", [128, 4], F32, sa); r_ssq = Res()
            stmp = sb("stmp", [128, 8], F32, sa); r_stmp = Res()
            rstd = sb("rstd", [128, 4], F32, sa); r_rstd = Res()
            ssqr = sb("ssqr", [128, 4], F32, sa); r_ssqr = Res()
            ssqa = sb("ssqa", [128, 4], F32, sa); r_ssqa = Res()
            rstdg = sb("rstdg", [128, 8], F32, sa); r_rstdg = Res()
            xrp = sb("xrp", [128, GT + 3], F32, sa); r_xrp = Res()
            xc = sb("xc", [128, GT], F32, sa); r_xc = Res()
            xcb = sb("xcb", [128, GT], BF16, sa); r_xcb = Res()
            tr_ = sb("tr_", [128, GT], F32, sa); r_tr = Res()
            ti_ = sb("ti_", [128, GT], F32, sa); r_ti = Res()
            av = sb("av", [128, GT], F32, sa); r_av = Res()
            vv = sb("vv", [128, GT], F32, sa); r_vv = Res()
            hs = sb("hs", [128, GT], F32, sa); r_hs = Res()
            xgs = sb("xgs", [128, GT], F32, sa); r_xgs = Res()
            g1 = sb("g1", [128, GT], F32, sa); r_g1 = Res()
            g2 = sb("g2", [128, GT], F32, sa); r_g2 = Res()
            a2 = hs; r_a2 = r_hs
            gl = av; r_gl = r_av
            yraw = vv; r_yraw = r_vv
            ysq = sb("ysq", [128, GT], BF16, sa); r_ysq = Res()
            Eb = [sb("Eb%d" % k, [128, 640], BF16, sa) for k in range(2)]
            r_Eb = [Res(), Res()]
            rden8 = sb("rden8", [128, 8], F32, sa); r_rden8 = Res()
            ex = [sb("ex0", [128, 2, GT], BF16, sa)] * 2
            r_ex = [Res()] * 2
            rdn = tr_; r_rdn = r_tr
            h3 = [sb("h3_%d" % k, [128, D], BF16, sa) for k in range(2)]
            r_h3 = [Res(), Res()]
            h3T = sb("h3T", [128, 8, 128], BF16, sa); r_h3T = Res()
            yat = h3[0][:].bitcast(F32); r_yat = r_h3[0]
            yab = Eb[0][:, 0:512]; r_yab = r_Eb[0]
            rt = sb("rt", [128, 772], F32, sa)
            r_rt = Res()
            Ab4 = xcb[:, 0:4 * NE].rearrange("p (j e) -> p j e", j=4); r_Ab = r_xcb

            def rtv(a, b, **kw):
                v = rt[:, a:b]
                return v.rearrange(kw.pop("pat"), **kw) if kw else v

            lg4 = rtv(0, 144, pat="p (j c) -> p j c", j=4)
            gmax4 = rt[:, 144:148]
            gsum4 = rt[:, 148:152]
            gp4 = rt[:, 152:156]
            m1_4 = rt[:, 156:160]
            m2_4 = rt[:, 160:164]
            dd4 = rt[:, 164:168]
            edd4 = rt[:, 168:172]
            w1_4 = rt[:, 172:176]
            w2_4 = rt[:, 176:180]
            gsh4 = rtv(180, 196, pat="p (j c) -> p j c", j=4)
            gex4 = rtv(196, 212, pat="p (j c) -> p j c", j=4)
            goh4 = rtv(212, 228, pat="p (j c) -> p j c", j=4)
            el4 = rtv(228, 260, pat="p (j c) -> p j c", j=4)
            oh1_4 = rtv(260, 292, pat="p (j c) -> p j c", j=4)
            el2_4 = rtv(292, 324, pat="p (j c) -> p j c", j=4)
            oh2_4 = rtv(324, 356, pat="p (j c) -> p j c", j=4)
            pk4 = rtv(356, 364, pat="p (j k) -> p j k", j=4)
            ek4 = rtv(364, 372, pat="p (j k) -> p j k", j=4)
            ok4 = rtv(372, 380, pat="p (j k) -> p j k", j=4)
            sd4 = rtv(380, 388, pat="p (j k) -> p j k", j=4)
            prod4 = rtv(388, 516, pat="p (j g e) -> p j g e", j=4, g=4)
            A1_4 = rtv(516, 644, pat="p (j c) -> p j c", j=4)
            A2_4 = rtv(644, 772, pat="p (j c) -> p j c", j=4)
            posf4 = rtv(0, 128, pat="p (j c) -> p j c", j=4)
            prodE = rtv(388, 516, pat="p (j c) -> p j c", j=4)

            def load_x(g):
                for j in range(TPG):
                    i = g * TPG + j
                    tk.dma("sp", "x%d" % j, lambda e: e.dma_start(out=xt[j][:], in_=x_d[i * 128:(i + 1) * 128, :]), w=[r_xt[j]])

            def norm_stats(j):
                A(lambda e: e.activation(out=junk[:], in_=xt[j][:], func=AF.Square, accum_out=ssq[:, j:j + 1]),
                  r=[r_xt[j]], w=[r_junk, r_ssq])

            def norm_to_hT(j, lnw):
                k = j % 2
                A(lambda e: e.activation(out=xn[k][:], in_=xt[j][:], func=AF.Copy, scale=rstd[:, j:j + 1]),
                  r=[r_xt[j], r_rstd], w=[r_xn[k]])
                pq, r_pq = (pB, r_pB) if j % 2 == 0 else (pC, r_pC)
                mm_transpose(pq, lambda b: xn[k][:, b * 128:(b + 1) * 128], 8, [r_xn[k]], r_pq)
                V(lambda e: e.tensor_tensor(out=hT[:, :, j * 128:(j + 1) * 128], in0=pq.rearrange("p (k t) -> p k t", k=8),
                                            in1=lnw.unsqueeze(2).broadcast_to([128, 8, 128]), op=ALU.mult),
                  r=r_pq + [r_vecs], w=[r_hT[j]])

            def proj_fm(wt, r_w, col0, bank_ap, r_bank):
                PE([(lambda e, kc=kc: e.matmul(bank_ap, lhsT=wt[:, kc, col0:col0 + 128], rhs=hT[:, kc, :],
                                               start=(kc == 0), stop=(kc == 7))) for kc in range(8)],
                   r=list(r_w) + r_hT, w=[r_bank])

            def rnn_steps(g, c):
                C0 = pD
                rC0 = r_pD
                cw0 = 40 + c * 4
                M_ = []
                G_ = []
                T_ = []
                G_.append(lambda: proj_fm(w_in, r_w_in, 512 + c * 128, C0, rC0))
                G_.append(lambda: V(lambda e: e.tensor_copy(out=xgs[:], in_=C0), r=[rC0], w=[r_xgs]))
                G_.append(lambda: A(lambda e: e.activation(out=g1[:], in_=xgs[:], func=AF.Square), r=[r_xgs], w=[r_g1]))
                G_.append(lambda: GP(lambda e: e.tensor_scalar(out=g1[:], in0=g1[:], scalar1=0.044715, scalar2=1.0, op0=ALU.mult, op1=ALU.add),
                                     r=[r_g1], w=[r_g1]))
                G_.append(lambda: GP(lambda e: e.tensor_tensor(out=g1[:], in0=xgs[:], in1=g1[:], op=ALU.mult), r=[r_xgs, r_g1], w=[r_g1]))
                G_.append(lambda: A(lambda e: e.activation(out=g2[:], in_=g1[:], func=AF.Tanh, scale=GELU_K), r=[r_g1], w=[r_g2]))
                G_.append(lambda: V(lambda e: e.scalar_tensor_tensor(out=g2[:], in0=g2[:], scalar=1.0, in1=xgs[:], op0=ALU.add, op1=ALU.mult),
                                    r=[r_g2, r_xgs], w=[r_g2]))
                M_.append(lambda: proj_fm(w_in, r_w_in, c * 128, C0, rC0))
                M_.append(lambda: V(lambda e: e.tensor_copy(out=xrp[:, 3:GT + 3], in_=C0), r=[rC0], w=[r_xrp]))
                M_.append(lambda: V(lambda e: e.tensor_copy(out=xrp[:, 0:3], in_=halo[:, c, :]), r=[r_halo[c]], w=[r_xrp]))
                M_.append(lambda: V(lambda e: e.tensor_scalar(out=xc[:], in0=xrp[:, 3:GT + 3], scalar1=vecs[:, cw0 + 3:cw0 + 4],
                                                               scalar2=convb[:, c:c + 1], op0=ALU.mult, op1=ALU.add),
                                    r=[r_xrp, r_vecs], w=[r_xc]))
                for jj in range(3):
                    M_.append(lambda jj=jj: V(lambda e: e.scalar_tensor_tensor(out=xc[:], in0=xrp[:, jj:jj + GT],
                                                                               scalar=vecs[:, cw0 + jj:cw0 + jj + 1], in1=xc[:],
                                                                               op0=ALU.mult, op1=ALU.add),
                                              r=[r_xrp, r_vecs, r_xc], w=[r_xc]))
                M_.append(lambda: V(lambda e: e.tensor_copy(out=halo[:, c, :], in_=xrp[:, GT:GT + 3]), r=[r_xrp], w=[r_halo[c]]))
                M_.append(lambda: GP(lambda e: e.tensor_copy(out=xcb[:], in_=xc[:]), r=[r_xc], w=[r_xcb]))
                M_.append(lambda: PE([lambda e: e.matmul(C0, lhsT=bd[:, 0, c, :], rhs=xcb[:], start=True, stop=True)],
                                     r=[r_bd, r_xcb], w=[rC0]))
                M_.append(lambda: A(lambda e: e.activation(out=tr_[:], in_=C0, func=AF.Tanh, scale=0.5, bias=hb[:, c:c + 1]),
                                    r=[rC0, r_hb], w=[r_tr]))
                M_.append(lambda: PE([lambda e: e.matmul(C0, lhsT=bd[:, 1, c, :], rhs=xcb[:], start=True, stop=True)],
                                     r=[r_bd, r_xcb], w=[rC0]))
                M_.append(lambda: A(lambda e: e.activation(out=ti_[:], in_=C0, func=AF.Tanh, scale=0.5, bias=hb[:, 4 + c:5 + c]),
                                    r=[rC0, r_hb], w=[r_ti]))
                M_.append(lambda: A(lambda e: e.activation(out=av[:], in_=tr_[:], func=AF.Exp, scale=hc[:, c:c + 1], bias=hc[:, c:c + 1]),
                                    r=[r_tr, r_hc], w=[r_av]))
                M_.append(lambda: A(lambda e: e.activation(out=a2[:], in_=tr_[:], func=AF.Exp, scale=hc[:, 4 + c:5 + c],
                                                           bias=hc[:, 4 + c:5 + c]), r=[r_tr, r_hc], w=[r_a2]))
                M_.append(lambda: V(lambda e: e.scalar_tensor_tensor(out=vv[:], in0=ti_[:], scalar=1.0, in1=xc[:], op0=ALU.add, op1=ALU.mult),
                                    r=[r_ti, r_xc], w=[r_vv]))
                M_.append(lambda: A(lambda e: e.activation(out=a2[:], in_=a2[:], func=AF.Sqrt, scale=-0.25, bias=qtr[:, 0:1]),
                                    r=[r_a2, r_neg], w=[r_a2]))
                M_.append(lambda: V(lambda e: e.tensor_tensor(out=vv[:], in0=vv[:], in1=a2[:], op=ALU.mult), r=[r_vv, r_a2], w=[r_vv]))
                M_.append(lambda: V(lambda e: e.tensor_tensor_scan(out=hs[:], data0=av[:], data1=vv[:], initial=hstate[:, c:c + 1],
                                                                    op0=ALU.mult, op1=ALU.add), r=[r_av, r_vv, r_hstate[c]], w=[r_hs]))
                M_.append(lambda: V(lambda e: e.tensor_copy(out=hstate[:, c:c + 1], in_=hs[:, GT - 1:GT]), r=[r_hs], w=[r_hstate[c]]))
                T_.append(lambda: V(lambda e: e.scalar_tensor_tensor(out=yraw[:], in0=g2[:], scalar=0.5, in1=hs[:], op0=ALU.mult, op1=ALU.mult),
                                    r=[r_g2, r_hs], w=[r_yraw]))
                T_.append(lambda: V(lambda e: e.tensor_scalar(out=yrT[:, c, :], in0=yraw[:], scalar1=gnrw[:, c:c + 1], scalar2=None, op0=ALU.mult),
                                    r=[r_yraw, r_vecs], w=[r_yrT[c]]))
                T_.append(lambda: GP(lambda e: e.tensor_tensor(out=ysq[:], in0=yraw[:], in1=yraw[:], op=ALU.mult), r=[r_yraw], w=[r_ysq]))
                T_.append(lambda: PE([(lambda e, j=j: e.matmul(C0[:, j:j + 1], lhsT=ysq[:, j * 128:(j + 1) * 128], rhs=ones_bf[:, 0:1],
                                                               start=True, stop=True)) for j in range(TPG)], r=[r_ysq, r_ones], w=[rC0]))
                if c == 0:
                    T_.append(lambda: V(lambda e: e.tensor_copy(out=ssqr[:], in_=C0[:, 0:4]), r=[rC0], w=[r_ssqr]))
                else:
                    T_.append(lambda: V(lambda e: e.tensor_tensor(out=ssqr[:], in0=C0[:, 0:4], in1=ssqr[:], op=ALU.add),
                                        r=[rC0, r_ssqr], w=[r_ssqr]))
                out_ = []
                gi = 0
                for mi, m_ in enumerate(M_):
                    out_.append(m_)
                    if mi % 3 == 1 and gi < len(G_):
                        out_.append(G_[gi]); gi += 1
                out_.extend(G_[gi:])
                out_.extend(T_)
                return out_

            def att_tile(g, j, steps):
                i = g * TPG + j
                ms = [m for m in range(5) if i - m >= 0]
                nb = len(ms)
                pBv = pC.rearrange("p (b x) -> p b x", b=2)[:, :, 0:260].rearrange("p b (h d) -> p b h d", d=65)
                kstep = 3

                def emit_S(h):
                    ch = h // 2
                    bi = h % 2
                    fns = []
                    for m in ms:
                        slot = (i - m) % 8
                        fns.append(lambda e, m=m: e.matmul(SX[bi][:, m * 128:(m + 1) * 128], lhsT=ident[:],
                                                           rhs=bm[:, h, m * 128:(m + 1) * 128], start=True, stop=False))
                        fns.append(lambda e, m=m, slot=slot: e.matmul(
                            SX[bi][:, m * 128:(m + 1) * 128], lhsT=kring[:, ch, slot * 128:(slot + 1) * 128],
                            rhs=qT[:, h, j * 128:(j + 1) * 128], start=False, stop=True))
                    PE(fns, r=[r_kring[0][ch], r_kring[1][ch], r_qT[h], r_bm, r_ident], w=[r_SX[bi]])

                emit_S(0)
                for h in range(8):
                    bi = h % 2
                    if h + 1 < 8:
                        emit_S(h + 1)
                    A(lambda e: e.activation(out=Eb[bi][:, 0:nb * 128], in_=SX[bi][:, 0:nb * 128], func=AF.Exp),
                      r=[r_SX[bi]], w=[r_Eb[bi]])
                    fns = []
                    for idx, m in enumerate(ms):
                        slot = (i - m) % 8
                        fns.append(lambda e, m=m, slot=slot, idx=idx: e.matmul(
                            pC[:, (h // 4) * 512 + (h % 4) * 65:(h // 4) * 512 + (h % 4) * 65 + 65],
                            lhsT=Eb[bi][:, m * 128:(m + 1) * 128], rhs=vring[:, slot, h, :], start=(idx == 0), stop=(idx == nb - 1)))
                    PE(fns, r=[r_Eb[bi]] + r_vring, w=[r_pC[h // 4]])
                    for _ in range(kstep):
                        if steps:
                            steps.pop(0)()
                if j == TPG - 1:
                    while steps:
                        steps.pop(0)()
                V(lambda e: e.reciprocal(out=rden8[:].rearrange("p (b h) -> p b h", b=2), in_=pBv[:, :, :, 64]),
                  r=r_pC, w=[r_rden8])
                V(lambda e: e.tensor_tensor(out=yat[:].rearrange("p (b h d) -> p b h d", b=2, h=4), in0=pBv[:, :, :, 0:64],
                                            in1=rden8[:].rearrange("p (b h) -> p b h", b=2).unsqueeze(3).broadcast_to([128, 2, 4, 64]),
                                            op=ALU.mult), r=r_pC + [r_rden8], w=[r_yat])
                A(lambda e: e.activation(out=junk[:, 0:512], in_=yat[:], func=AF.Square, accum_out=ssqa[:, j:j + 1]),
                  r=[r_yat], w=[r_junk, r_ssqa])
                A(lambda e: e.activation(out=yab[:], in_=yat[:], func=AF.Copy), r=[r_yat], w=[r_yab])
                mm_transpose(pE, lambda b: yab[:, b * 128:(b + 1) * 128], 4, [r_yab], [r_pE])
                V(lambda e: e.tensor_tensor(out=yaT[:, :, j * 128:(j + 1) * 128], in0=pE.rearrange("p (k t) -> p k t", k=4),
                                            in1=gnaw.unsqueeze(2).broadcast_to([128, 4, 128]), op=ALU.mult),
                  r=[r_pE, r_vecs], w=[r_yaT[j]])

            def out_proj(j):
                for half in range(2):
                    PE([(lambda e, c=c: e.matmul(pA[:, half * 512:(half + 1) * 512], lhsT=yrT[:, c, j * 128:(j + 1) * 128],
                                                 rhs=w_out[:, c, half * 512:(half + 1) * 512], start=(c == 0), stop=(c == 3)))
                        for c in range(4)], r=r_yrT + [r_w_out], w=[r_pA[half]])
                for half in range(2):
                    PE([(lambda e, c=c: e.matmul(pB[:, half * 512:(half + 1) * 512], lhsT=yaT[:, c, j * 128:(j + 1) * 128],
                                                 rhs=w_out[:, 4 + c, half * 512:(half + 1) * 512], start=(c == 0), stop=(c == 3)))
                        for c in range(4)], r=[r_yaT[j], r_w_out], w=[r_pB[half]])
                V(lambda e: e.scalar_tensor_tensor(out=xt[j][:], in0=pA[:], scalar=rstdg[:, j:j + 1], in1=xt[j][:],
                                                   op0=ALU.mult, op1=ALU.add), r=r_pA + [r_rstdg, r_xt[j]], w=[r_xt[j]])
                V(lambda e: e.scalar_tensor_tensor(out=xt[j][:], in0=pB[:], scalar=rstdg[:, 4 + j:5 + j], in1=xt[j][:],
                                                   op0=ALU.mult, op1=ALU.add), r=r_pB + [r_rstdg, r_xt[j]], w=[r_xt[j]])

            def cross_attn():
                for hh in range(4):
                    k = hh % 2
                    for mt in range(2):
                        PE([(lambda e, dc=dc: e.matmul(pB[:, mt * 512:(mt + 1) * 512], lhsT=kTm[:, 2 * hh + dc, mt * 128:(mt + 1) * 128],
                                                       rhs=qxT[:, 2 * hh + dc, :], start=(dc == 0), stop=(dc == 1))) for dc in range(2)],
                           r=[r_kTm, r_qxT[2 * hh], r_qxT[2 * hh + 1]], w=[r_pB[mt]])
                        A(lambda e: e.activation(out=ex[k][:, mt, :], in_=pB[:, mt * 512:(mt + 1) * 512], func=AF.Exp, scale=0.0625),
                          r=[r_pB[mt]], w=[r_ex[k]])
                    PE([(lambda e, mt=mt: e.matmul(pD[:], lhsT=ones_bf[:], rhs=ex[k][:, mt, :], start=(mt == 0), stop=(mt == 1)))
                        for mt in range(2)], r=[r_ones, r_ex[k]], w=[r_pD])
                    V(lambda e: e.reciprocal(out=rdn[:], in_=pD[:]), r=[r_pD], w=[r_rdn])
                    for dc in range(2):
                        PE([(lambda e, mt=mt: e.matmul(pC[:, dc * 512:(dc + 1) * 512],
                                                       lhsT=vm[:, mt, (2 * hh + dc) * 128:(2 * hh + dc + 1) * 128],
                                                       rhs=ex[k][:, mt, :], start=(mt == 0), stop=(mt == 1))) for mt in range(2)],
                           r=[r_vm, r_ex[k]], w=[r_pC[dc]])
                        V(lambda e: e.tensor_tensor(out=hT[:, 2 * hh + dc, :], in0=pC[:, dc * 512:(dc + 1) * 512], in1=rdn[:], op=ALU.mult),
                          r=[r_pC[dc], r_rdn], w=r_hT)

            def wo_proj(j):
                pw, r_pw = (pA, r_pA) if j % 2 == 0 else (pB, r_pB)
                for half in range(2):
                    PE([(lambda e, kc=kc: e.matmul(pw[:, half * 512:(half + 1) * 512], lhsT=hT[:, kc, j * 128:(j + 1) * 128],
                                                   rhs=wo[:, kc, half * 512:(half + 1) * 512], start=(kc == 0), stop=(kc == 7)))
                        for kc in range(8)], r=r_hT + [r_wo], w=[r_pw[half]])
                V(lambda e: e.tensor_tensor(out=xt[j][:], in0=pw, in1=xt[j][:], op=ALU.add), r=r_pw + [r_xt[j]], w=[r_xt[j]])

            def route_group(g):
                R = [r_rt]
                h3r = [mix[:, 2 * j:2 * j + 2, :].rearrange("p a t -> p (a t)") for j in range(TPG)]
                r_h3r = [[r_mix[2 * j], r_mix[2 * j + 1]] for j in range(TPG)]
                for j in range(TPG):
                    i = g * TPG + j
                    A(lambda e: e.activation(out=h3r[j], in_=xt[j][:], func=AF.Copy, scale=rstd[:, j:j + 1]),
                      r=[r_xt[j], r_rstd], w=r_h3r[j])
                    tk.dma("sp", "x2st%d" % j, lambda e: e.dma_start(out=x2s_d[i * 128:(i + 1) * 128, :], in_=xt[j][:]), r=[r_xt[j]], w=[r_x2s[i]])
                    pq, r_pq = (pB, r_pB) if j % 2 == 0 else (pC, r_pC)
                    mm_transpose(pq, lambda b: h3r[j][:, b * 128:(b + 1) * 128], 8, r_h3r[j], r_pq)
                    V(lambda e: e.tensor_tensor(out=h3T[:], in0=pq.rearrange("p (k t) -> p k t", k=8),
                                                in1=ln3w.unsqueeze(2).broadcast_to([128, 8, 128]), op=ALU.mult), r=r_pq + [r_vecs], w=[r_h3T])
                    PE([(lambda e, kc=kc: e.matmul(pD[:, j * 64:j * 64 + 36], lhsT=h3T[:, kc, :], rhs=wr[:, kc, :], start=(kc == 0), stop=(kc == 7)))
                        for kc in range(8)], r=[r_h3T, r_wr], w=[r_pD])
                pDl = pD[:, 0:256].rearrange("p (j c) -> p j c", j=4)[:, :, 0:36]
                V(lambda e: e.tensor_tensor(out=lg4, in0=pDl, in1=rb_bc[:].unsqueeze(1).broadcast_to([128, 4, 36]), op=ALU.add),
                  r=[r_pD, r_rb], w=R)
                lgG = lg4[:, :, 0:4]
                lgE = lg4[:, :, 4:36].rearrange("p j (g e) -> p j g e", g=4)
                V(lambda e: e.tensor_reduce(out=gmax4, in_=lgG, axis=AX.X, op=ALU.max), r=R, w=R)
                V(lambda e: e.tensor_tensor(out=gsh4, in0=lgG, in1=gmax4.unsqueeze(2).broadcast_to([128, 4, 4]), op=ALU.subtract), r=R, w=R)
                A(lambda e: e.activation(out=gex4, in_=gsh4, func=AF.Exp), r=R, w=R)
                V(lambda e: e.tensor_reduce(out=gsum4, in_=gex4, axis=AX.X, op=ALU.add), r=R, w=R)
                V(lambda e: e.reciprocal(out=gp4, in_=gsum4), r=R, w=R)
                V(lambda e: e.tensor_tensor(out=goh4, in0=lgG, in1=gmax4.unsqueeze(2).broadcast_to([128, 4, 4]), op=ALU.is_ge), r=R, w=R)
                V(lambda e: e.tensor_tensor(out=prod4, in0=lgE, in1=goh4.unsqueeze(3).broadcast_to([128, 4, 4, 8]), op=ALU.mult), r=R, w=R)
                V(lambda e: e.tensor_reduce(out=el4, in_=prod4.rearrange("p j g e -> p j e g"), axis=AX.X, op=ALU.add), r=R, w=R)
                V(lambda e: e.tensor_reduce(out=m1_4, in_=el4, axis=AX.X, op=ALU.max), r=R, w=R)
                V(lambda e: e.tensor_tensor(out=oh1_4, in0=el4, in1=m1_4.unsqueeze(2).broadcast_to([128, 4, 8]), op=ALU.is_ge), r=R, w=R)
                V(lambda e: e.scalar_tensor_tensor(out=el2_4, in0=oh1_4, scalar=-1e30, in1=el4, op0=ALU.mult, op1=ALU.add), r=R, w=R)
                V(lambda e: e.tensor_reduce(out=m2_4, in_=el2_4, axis=AX.X, op=ALU.max), r=R, w=R)
                V(lambda e: e.tensor_tensor(out=oh2_4, in0=el2_4, in1=m2_4.unsqueeze(2).broadcast_to([128, 4, 8]), op=ALU.is_ge), r=R, w=R)
                V(lambda e: e.tensor_tensor(out=dd4, in0=m2_4, in1=m1_4, op=ALU.subtract), r=R, w=R)
                A(lambda e: e.activation(out=edd4, in_=dd4, func=AF.Exp), r=R, w=R)
                V(lambda e: e.tensor_scalar(out=w1_4, in0=edd4, scalar1=1.0, scalar2=None, op0=ALU.add), r=R, w=R)
                V(lambda e: e.reciprocal(out=w1_4, in_=w1_4), r=R, w=R)
                V(lambda e: e.tensor_tensor(out=w2_4, in0=edd4, in1=w1_4, op=ALU.mult), r=R, w=R)
                cwv = cw_all[:, 8 * g:8 * g + 8].rearrange("p (j k) -> p j k", k=2)
                r_cwg = [r_cw[g * TPG + j] for j in range(TPG)]
                V(lambda e: e.tensor_tensor(out=cwv[:, :, 0], in0=w1_4, in1=gp4, op=ALU.mult), r=R, w=r_cwg)
                V(lambda e: e.tensor_tensor(out=cwv[:, :, 1], in0=w2_4, in1=gp4, op=ALU.mult), r=R + r_cwg, w=r_cwg)
                gohb = goh4.unsqueeze(3).broadcast_to([128, 4, 4, 8])
                V(lambda e: e.tensor_tensor(out=A1_4.rearrange("p j (g e) -> p j g e", g=4), in0=gohb,
                                            in1=oh1_4.unsqueeze(2).broadcast_to([128, 4, 4, 8]), op=ALU.mult), r=R, w=R)
                V(lambda e: e.tensor_tensor(out=A2_4.rearrange("p j (g e) -> p j g e", g=4), in0=gohb,
                                            in1=oh2_4.unsqueeze(2).broadcast_to([128, 4, 4, 8]), op=ALU.mult), r=R, w=R)
                V(lambda e: e.tensor_tensor(out=Ab4[:], in0=A1_4, in1=A2_4, op=ALU.add), r=R, w=[r_Ab])
                for j in range(TPG):
                    fns = [lambda e: e.matmul(pD[:, 256 + j * 32:256 + (j + 1) * 32], lhsT=tri_bf[:], rhs=Ab4[:, j, :], start=True, stop=(j == 0))]
                    for jp in range(j):
                        fns.append(lambda e, jp=jp: e.matmul(pD[:, 256 + j * 32:256 + (j + 1) * 32], lhsT=ones_bf[:], rhs=Ab4[:, jp, :],
                                                             start=False, stop=(jp == j - 1)))
                    PE(fns, r=[r_tri, r_ones, r_Ab], w=[r_pD])
                PE([(lambda e, j=j: e.matmul(pD[:, 384:416], lhsT=ones_bf[:], rhs=Ab4[:, j, :], start=(j == 0), stop=(j == TPG - 1)))
                    for j in range(TPG)], r=[r_ones, r_Ab], w=[r_pD])
                V(lambda e: e.tensor_tensor(out=posf4, in0=pD[:, 256:384].rearrange("p (j e) -> p j e", j=4),
                                            in1=cnt[:].unsqueeze(1).broadcast_to([128, 4, NE]), op=ALU.add), r=[r_pD, r_cnt], w=R)
                V(lambda e: e.tensor_tensor(out=cnt[:], in0=pD[:, 384:416], in1=cnt[:], op=ALU.add), r=[r_pD, r_cnt], w=[r_cnt])
                eoffb = eoff[:].unsqueeze(1).broadcast_to([128, 4, NE])
                for kk, Af in enumerate((A1_4, A2_4)):
                    V(lambda e: e.tensor_tensor(out=prodE, in0=Af, in1=posf4, op=ALU.mult), r=R, w=R)
                    V(lambda e: e.tensor_reduce(out=pk4[:, :, kk], in_=prodE, axis=AX.X, op=ALU.add), r=R, w=R)
                    V(lambda e: e.tensor_tensor(out=prodE, in0=Af, in1=eoffb, op=ALU.mult), r=R + [r_eoff], w=R)
                    V(lambda e: e.tensor_reduce(out=ek4[:, :, kk], in_=prodE, axis=AX.X, op=ALU.add), r=R, w=R)
                V(lambda e: e.tensor_scalar(out=ok4, in0=pk4, scalar1=float(CAP), scalar2=None, op0=ALU.is_lt), r=R, w=R)
                V(lambda e: e.tensor_tensor(out=sd4, in0=pk4, in1=ek4, op=ALU.add), r=R, w=R)
                V(lambda e: e.scalar_tensor_tensor(out=sd4, in0=sd4, scalar=-float(TRASH), in1=ok4, op0=ALU.add, op1=ALU.mult), r=R, w=R)
                V(lambda e: e.tensor_scalar(out=sd4, in0=sd4, scalar1=float(TRASH), scalar2=None, op0=ALU.add), r=R, w=R)
                r_dg = [r_dest[g * TPG + j] for j in range(TPG)]
                V(lambda e: e.tensor_copy(out=dest_all[:, 8 * g:8 * g + 8], in_=sd4.rearrange("p j k -> p (j k)")), r=R, w=r_dg)
                def do_scatter():
                    for j in range(TPG):
                        i = g * TPG + j
                        for kk in range(2):
                            tk.dma("pool", "sc%d" % j, lambda e: e.indirect_dma_start(
                                out=xs_d, out_offset=bass.IndirectOffsetOnAxis(ap=dest_all[:, 2 * i + kk:2 * i + kk + 1], axis=0),
                                in_=h3r[j], in_offset=None), r=r_h3r[j] + [r_dest[i], r_xsz], w=[Res()])
                return do_scatter

            pending = None
            for g in range(NG):
                par = g % 2
                load_x(g)
                if g == 0:
                    xs_z = xs_d.rearrange("(p r) d -> p r d", p=128)
                    RZ = NROWS // 128
                    ZC = 34
                    for zi in range(RZ // ZC):
                        tk.dma("sp", "xsz", lambda e: e.dma_start(out=xs_z[:, zi * ZC:(zi + 1) * ZC, :],
                                                                  in_=zt2[:].unsqueeze(1).broadcast_to([128, ZC, D])),
                               r=[r_zt2, r_w_out, r_wq, r_wo, r_bm, r_bd, r_wr], w=[r_xsz])
                for j in range(TPG):
                    norm_stats(j)
                rstd_op(ssq[:], 4, D, stmp[:, 0:4], rstd[:], [r_ssq], r_stmp, r_rstd)
                if pending is not None:
                    pending()
                for j in range(TPG):
                    norm_to_hT(j, ln1w)
                rnn_all = []
                for c_ in range(4):
                    rnn_all.extend(rnn_steps(g, c_))

                def pump(n):
                    for _ in range(n):
                        if rnn_all:
                            rnn_all.pop(0)()

                GP(lambda e: e.memset(mix[:, 0:8, :].rearrange("p a t -> p (a t)"), 0.0), w=r_mix[0:8])
                for c in range(4):
                    proj_fm(w_in, r_w_in, 1024 + c * 128, pA[:, (c % 2) * 512:(c % 2 + 1) * 512], r_pA[c % 2])
                    for hh_ in range(2):
                        A(lambda e: e.activation(out=qT[hh_ * 64:(hh_ + 1) * 64, 2 * c + hh_, :],
                                                 in_=pA[hh_ * 64:(hh_ + 1) * 64, (c % 2) * 512:(c % 2 + 1) * 512], func=AF.Copy, scale=0.125),
                          r=[r_pA[c % 2]], w=[r_qT[2 * c + hh_]])
                    pump(3)
                for c in range(4):
                    proj_fm(w_in, r_w_in, 1536 + c * 128, pA[:, (c % 2) * 512:(c % 2 + 1) * 512], r_pA[c % 2])
                    A(lambda e: e.activation(out=kring[:, c, par * 512:(par + 1) * 512], in_=pA[:, (c % 2) * 512:(c % 2 + 1) * 512],
                                             func=AF.Copy), r=[r_pA[c % 2]], w=[r_kring[par][c]])
                    pump(3)
                for j in range(TPG):
                    slot = (g * TPG + j) % 8
                    PE([(lambda e, kc=kc: e.matmul(pC[:, (j % 2) * 512:(j % 2 + 1) * 512], lhsT=hT[:, kc, j * 128:(j + 1) * 128],
                                                   rhs=w_in[:, kc, 2048:2560], start=(kc == 0), stop=(kc == 7))) for kc in range(8)],
                       r=r_w_in + [r_hT[j]], w=[r_pC[j % 2]])
                    V(lambda e: e.tensor_copy(out=vring[:, slot, :, 0:64],
                                              in_=pC[:, (j % 2) * 512:(j % 2 + 1) * 512].rearrange("p (h d) -> p h d", h=8)),
                      r=[r_pC[j % 2]], w=[r_vring[slot]])
                    pump(3)
                for j in range(TPG):
                    att_tile(g, j, rnn_all)
                V(lambda e: e.tensor_copy(out=stmp[:, 0:4], in_=ssqr[:]), r=[r_ssqr], w=[r_stmp])
                V(lambda e: e.tensor_copy(out=stmp[:, 4:8], in_=ssqa[:]), r=[r_ssqa], w=[r_stmp])
                V(lambda e: e.tensor_scalar(out=stmp[:], in0=stmp[:], scalar1=1.0 / 512, scalar2=EPS, op0=ALU.mult, op1=ALU.add),
                  r=[r_stmp], w=[r_stmp])
                GP(lambda e: e.tensor_tensor(out=rstdg[:], in0=stmp[:], in1=neg05[:], op=ALU.pow), r=[r_stmp, r_neg], w=[r_rstdg])
                for j in range(TPG):
                    out_proj(j)
                    norm_stats(j)
                rstd_op(ssq[:], 4, D, stmp[:, 0:4], rstd[:], [r_ssq], r_stmp, r_rstd)
                for j in range(TPG):
                    norm_to_hT(j, ln2w)
                for fo in range(8):
                    proj_fm(wq, [r_wq], fo * 128, pA[:, (fo % 2) * 512:(fo % 2 + 1) * 512], r_pA[fo % 2])
                    A(lambda e: e.activation(out=qxT[:, fo, :], in_=pA[:, (fo % 2) * 512:(fo % 2 + 1) * 512], func=AF.Copy),
                      r=[r_pA[fo % 2]], w=[r_qxT[fo]])
                cross_attn()
                for j in range(TPG):
                    wo_proj(j)
                    norm_stats(j)
                rstd_op(ssq[:], 4, D, stmp[:, 0:4], rstd[:], [r_ssq], r_stmp, r_rstd)
                pending = route_group(g)
            pending()
            tk.barrier()

        with ExitStack() as sbk:
            NB = 3
            wg = [sb("wg%d" % k, [128, 8, 512], BF16, sbk) for k in range(NB)]
            wu = [sb("wu%d" % k, [128, 8, 512], BF16, sbk) for k in range(NB)]
            wd = [sb("wd%d" % k, [128, 4, D], BF16, sbk) for k in range(NB)]
            r_wg = [Res() for _ in range(NB)]; r_wu = [Res() for _ in range(NB)]; r_wd = [Res() for _ in range(NB)]
            xsl = [sb("xsl%d" % k, [128, CAP // 128, D], BF16, sbk) for k in range(NB)]
            r_xsl = [Res() for _ in range(NB)]
            xsT = sb("xsT", [128, 8, CAP], BF16, sbk); r_xsT = Res()
            sg = [sb("sg%d" % k, [128, CAP], F32, sbk) for k in range(2)]
            r_sg = [Res(), Res()]
            aT = sb("aT", [128, 4, CAP], BF16, sbk)
            r_aT = [Res() for _ in range(4)]
            yb = [sb("yb%d" % k, [128, D], F32, sbk) for k in range(2)]
            r_yb = [Res(), Res()]

            def load_expert(e_):
                k = e_ % NB
                tk.dma("pool", "ewg%d" % k, lambda e: e.dma_start(out=wg[k][:], in_=eg_d[e_].rearrange("(k p) n -> p k n", p=128)), w=[r_wg[k]])
                tk.dma("pool", "ewu%d" % k, lambda e: e.dma_start(out=wu[k][:], in_=eu_d[e_].rearrange("(k p) n -> p k n", p=128)), w=[r_wu[k]])
                tk.dma("pool", "ewd%d" % k, lambda e: e.dma_start(out=wd[k][:], in_=ed_d[e_].rearrange("(k p) n -> p k n", p=128)), w=[r_wd[k]])
                tk.dma("sp", "xsl%d" % k, lambda e: e.dma_start(out=xsl[k][:], in_=xs_d[e_ * CAP:(e_ + 1) * CAP, :].rearrange("(t p) d -> p t d", p=128)),
                       w=[r_xsl[k]])

            xsT2 = [xsT, sb("xsT_b", [128, 8, CAP], BF16, sbk)]
            r_xsT2 = [r_xsT, Res()]

            def tr_steps(e_):
                k = e_ % NB
                xo, r_xo = xsT2[e_ % 2], r_xsT2[e_ % 2]
                st_ = []
                n = 0
                for t in range(CAP // 128):
                    for hf in range(2):
                        pq, r_pq = (pD, r_pD) if n % 2 == 0 else (pE, r_pE)
                        n += 1
                        st_.append(lambda t=t, hf=hf, pq=pq, r_pq=r_pq: mm_transpose(
                            pq, lambda b: xsl[k][:, t, (hf * 4 + b) * 128:(hf * 4 + b + 1) * 128], 4, [r_xsl[k]], [r_pq]))
                        st_.append(lambda t=t, hf=hf, pq=pq, r_pq=r_pq: V(lambda e: e.tensor_tensor(
                            out=xo[:, hf * 4:(hf + 1) * 4, t * 128:(t + 1) * 128], in0=pq.rearrange("p (k t) -> p k t", k=4),
                            in1=ln3w[:, hf * 4:(hf + 1) * 4].unsqueeze(2).broadcast_to([128, 4, 128]), op=ALU.mult),
                            r=[r_pq, r_vecs], w=[r_xo]))
                return st_

            def main_steps(e_):
                k = e_ % NB
                xo, r_xo = xsT2[e_ % 2], r_xsT2[e_ % 2]
                st_ = []
                for fc in range(4):
                    b2 = fc % 2
                    st_.append(lambda fc=fc, b2=b2: PE([(lambda e, kc=kc: e.matmul(pA[:, b2 * 512:b2 * 512 + CAP], lhsT=wg[k][:, kc, fc * 128:(fc + 1) * 128],
                                                                                 rhs=xo[:, kc, :], start=(kc == 0), stop=(kc == 7))) for kc in range(8)],
                                                       r=[r_wg[k], r_xo], w=[r_pA[b2]]))
                    st_.append(lambda fc=fc, b2=b2: PE([(lambda e, kc=kc: e.matmul(pB[:, b2 * 512:b2 * 512 + CAP], lhsT=wu[k][:, kc, fc * 128:(fc + 1) * 128],
                                                                                 rhs=xo[:, kc, :], start=(kc == 0), stop=(kc == 7))) for kc in range(8)],
                                                       r=[r_wu[k], r_xo], w=[r_pB[b2]]))
                    st_.append(lambda b2=b2: A(lambda e: e.activation(out=sg[b2][:], in_=pA[:, b2 * 512:b2 * 512 + CAP], func=AF.Silu),
                                               r=[r_pA[b2]], w=[r_sg[b2]]))
                    st_.append(lambda fc=fc, b2=b2: V(lambda e: e.tensor_tensor(out=aT[:, fc, :], in0=pB[:, b2 * 512:b2 * 512 + CAP], in1=sg[b2][:], op=ALU.mult),
                                                      r=[r_pB[b2], r_sg[b2]], w=[r_aT[fc]]))
                for t in range(CAP // 128):
                    yk = (e_ * (CAP // 128) + t) % 2
                    for half in range(2):
                        st_.append(lambda t=t, half=half: PE([(lambda e, fc=fc: e.matmul(pC[:, half * 512:(half + 1) * 512], lhsT=aT[:, fc, t * 128:(t + 1) * 128],
                                                                                         rhs=wd[k][:, fc, half * 512:(half + 1) * 512], start=(fc == 0), stop=(fc == 3)))
                                                              for fc in range(4)], r=r_aT + [r_wd[k]], w=[r_pC[half]]))
                    st_.append(lambda yk=yk: A(lambda e: e.activation(out=yb[yk][:, 0:512], in_=pC[:, 0:512], func=AF.Copy), r=[r_pC[0], r_yb[yk]], w=[r_yb[yk]]))
                    st_.append(lambda yk=yk: V(lambda e: e.tensor_copy(out=yb[yk][:, 512:1024], in_=pC[:, 512:1024]), r=[r_pC[1], r_yb[yk]], w=[r_yb[yk]]))
                    row0 = e_ * CAP + t * 128
                    st_.append(lambda yk=yk, row0=row0: tk.dma("sp", "ys%d" % yk, lambda e: e.dma_start(out=ys_d[row0:row0 + 128, :], in_=yb[yk][:]),
                                                               r=[r_yb[yk]], w=[Res()]))
                return st_

            load_expert(0)
            load_expert(1)
            for f_ in tr_steps(0):
                f_()
            for e_ in range(NE):
                if e_ + 2 < NE:
                    load_expert(e_ + 2)
                ms_ = main_steps(e_)
                ts_ = tr_steps(e_ + 1) if e_ + 1 < NE else []
                while ms_ or ts_:
                    for _ in range(2):
                        if ms_:
                            ms_.pop(0)()
                    if ts_:
                        ts_.pop(0)()
            tk.barrier()

        with ExitStack() as sc:
            NBC = 4
            x2t = [sb("x2t%d" % k, [128, D], F32, sc) for k in range(NBC)]
            y1t = [sb("y1t%d" % k, [128, D], F32, sc) for k in range(NBC)]
            y2t = [sb("y2t%d" % k, [128, D], F32, sc) for k in range(NBC)]
            r_x2t = [Res() for _ in range(NBC)]; r_y1t = [Res() for _ in range(NBC)]; r_y2t = [Res() for _ in range(NBC)]
            junkc = [sb("junkc%d" % k, [128, D], BF16, sc) for k in range(2)]; r_junkc = [Res(), Res()]
            fin_bc = sb("fin_bc", [128, D], F32, sc); r_fin = Res()
            tk.dma("sp", "fin", lambda e: e.dma_start(out=fin_bc[:], in_=fin_d.partition_broadcast(128)), w=[r_fin])
            fss = sb("fss", [128, NBC], F32, sc); r_fss = [Res() for _ in range(NBC)]
            ftmp = sb("ftmp", [128, NBC], F32, sc); r_ftmp = [Res() for _ in range(NBC)]
            frs = sb("frs", [128, NBC], F32, sc); r_frs = [Res() for _ in range(NBC)]

            def load_c(i):
                k = i % NBC
                tk.dma("sp", "cx%d" % k, lambda e: e.dma_start(out=x2t[k][:], in_=x2s_d[i * 128:(i + 1) * 128, :]), r=[r_x2s[i]], w=[r_x2t[k]])
                tk.dma("pool", "cy1%d" % k, lambda e: e.indirect_dma_start(
                    out=y1t[k][:], out_offset=None, in_=ys_d, in_offset=bass.IndirectOffsetOnAxis(ap=dest_all[:, 2 * i:2 * i + 1], axis=0)),
                    r=[r_dest[i]], w=[r_y1t[k]])
                tk.dma("pool", "cy2%d" % k, lambda e: e.indirect_dma_start(
                    out=y2t[k][:], out_offset=None, in_=ys_d, in_offset=bass.IndirectOffsetOnAxis(ap=dest_all[:, 2 * i + 1:2 * i + 2], axis=0)),
                    r=[r_dest[i]], w=[r_y2t[k]])

            def c_steps(i):
                k = i % NBC
                jk = i % 2
                st_ = []
                st_.append(lambda: V(lambda e: e.scalar_tensor_tensor(out=x2t[k][:], in0=y1t[k][:], scalar=cw_all[:, 2 * i:2 * i + 1], in1=x2t[k][:],
                                                                      op0=ALU.mult, op1=ALU.add), r=[r_y1t[k], r_cw[i], r_x2t[k]], w=[r_x2t[k]]))
                st_.append(lambda: V(lambda e: e.scalar_tensor_tensor(out=x2t[k][:], in0=y2t[k][:], scalar=cw_all[:, 2 * i + 1:2 * i + 2], in1=x2t[k][:],
                                                                      op0=ALU.mult, op1=ALU.add), r=[r_y2t[k], r_cw[i], r_x2t[k]], w=[r_x2t[k]]))
                st_.append(lambda: A(lambda e: e.activation(out=junkc[jk][:], in_=x2t[k][:], func=AF.Square, accum_out=fss[:, k:k + 1]),
                                     r=[r_x2t[k]], w=[r_junkc[jk], r_fss[k]]))
                st_.append(lambda: V(lambda e: e.tensor_scalar(out=ftmp[:, k:k + 1], in0=fss[:, k:k + 1], scalar1=1.0 / D, scalar2=EPS,
                                                               op0=ALU.mult, op1=ALU.add), r=[r_fss[k]], w=[r_ftmp[k]]))
                st_.append(lambda: GP(lambda e: e.tensor_tensor(out=frs[:, k:k + 1], in0=ftmp[:, k:k + 1], in1=neg05[:, 0:1], op=ALU.pow),
                                      r=[r_ftmp[k], r_neg], w=[r_frs[k]]))
                st_.append(lambda: A(lambda e: e.activation(out=y2t[k][:], in_=x2t[k][:], func=AF.Copy, scale=frs[:, k:k + 1]),
                                     r=[r_x2t[k], r_frs[k]], w=[r_y2t[k]]))
                st_.append(lambda: V(lambda e: e.tensor_tensor(out=y1t[k][:], in0=y2t[k][:], in1=fin_bc[:], op=ALU.mult),
                                     r=[r_y2t[k], r_fin], w=[r_y1t[k]]))
                st_.append(lambda: tk.dma("sp", "out%d" % k, lambda e: e.dma_start(out=out_d[i * 128:(i + 1) * 128, :], in_=y1t[k][:]), r=[r_y1t[k]]))
                return st_

            load_c(0)
            load_c(1)
            for i in range(0, NT, 2):
                for t_ in (i + 2, i + 3):
                    if t_ < NT:
                        load_c(t_)
                a_ = c_steps(i)
                b_ = c_steps(i + 1)
                while a_ or b_:
                    if a_:
                        a_.pop(0)()
                    if b_:
                        b_.pop(0)()
            for k in range(NBC):
                ds = tk.dsem("out%d" % k)
                nc.sync.wait_ge(ds.sem, ds.cnt)
    return nc


def _prep_shared(inp):
    f = np.float32
    vecs = np.zeros((128, NV), f)

    def pc(v, n):
        return np.ascontiguousarray(np.asarray(v, f).reshape(n, 128).T)

    vecs[:, 0:8] = pc(inp["ln1_w"][0], 8)
    vecs[:, 8:16] = pc(inp["ln2_w"][0], 8)
    vecs[:, 16:20] = pc(inp["gn_rnn_w"][0], 4)
    vecs[:, 20:24] = pc(inp["gn_att_w"][0], 4)
    vecs[:, 24:28] = pc(inp["conv_b"][0], 4)
    vecs[:, 28:32] = pc(inp["rnn_ba"][0], 4)
    vecs[:, 32:36] = pc(inp["rnn_bx"][0], 4)
    vecs[:, 36:40] = pc(inp["rnn_lambda"][0], 4)
    vecs[:, 56:64] = pc(inp["ln3_w"][0], 8)
    cwt = np.asarray(inp["conv_w"][0], f)
    for c in range(4):
        for j in range(4):
            vecs[:, 40 + c * 4 + j] = cwt[j, c * 128:(c + 1) * 128]
    bd = np.zeros((128, 2, 4, 128), f)
    for gi, name in enumerate(("rnn_wa", "rnn_wx")):
        w = np.asarray(inp[name][0], f)
        for c in range(4):
            for b in range(2):
                bd[b * 64:(b + 1) * 64, gi, c, b * 64:(b + 1) * 64] = w[2 * c + b]
    rb = np.asarray(inp["rel_bias"][0], f)
    kk = np.arange(128)[:, None]
    qq = np.arange(640)[None, :]
    idx = np.clip(qq - kk, -128, 128) + 128
    valid = np.where(kk < 64, (qq < 576), (qq >= 64))
    bm = np.empty((128, 8, 640), f)
    for h in range(8):
        bm[:, h, :] = np.where(valid, rb[h][idx], f(-30000.0))
    wr = np.concatenate([np.asarray(inp["router_group_w"][0], f),
                         np.asarray(inp["router_expert_w"][0], f).transpose(1, 0, 2).reshape(D, 32)], axis=1)
    rbias = np.concatenate([np.asarray(inp["router_group_b"][0], f), np.asarray(inp["router_expert_b"][0], f).reshape(32)])
    shared = {
        "vecs": vecs, "bm": bm, "bd": bd, "wr": np.ascontiguousarray(wr), "rb": np.ascontiguousarray(rbias),
        "w_in": np.ascontiguousarray(inp["w_in"][0], f), "w_out": np.ascontiguousarray(inp["w_out"][0], f),
        "wq": np.ascontiguousarray(inp["xq_w"][0], f), "wk": np.ascontiguousarray(inp["xk_w"][0], f),
        "wv": np.ascontiguousarray(inp["xv_w"][0], f), "wo": np.ascontiguousarray(inp["xo_w"][0], f),
        "memw": np.ascontiguousarray(inp["mem_norm_w"], f),
        "fin": np.ascontiguousarray(inp["final_norm_w"], f),
        "eg": np.ascontiguousarray(inp["expert_gate_w"][0], f), "eu": np.ascontiguousarray(inp["expert_up_w"][0], f),
        "ed": np.ascontiguousarray(inp["expert_down_w"][0], f),
    }
    return shared


def kernel(**inputs):
    inp = {k: np.asarray(v) for k, v in inputs.items()}
    shared = _prep_shared(inp)
    nc = build_nc()
    in_maps = []
    for b in range(8):
        m = dict(shared)
        m["x"] = np.ascontiguousarray(inp["x"][b], np.float32)
        m["mem"] = np.ascontiguousarray(inp["mem"][b], np.float32)
        in_maps.append(m)
    res = run_bass_kernel_spmd(nc, in_maps, core_ids=list(range(8)))
    return np.stack([np.asarray(r["out"], np.float32) for r in res.results], axis=0)
```
